# Optimizing a Trainium2 kernel written in Bass

```python
import jax, jax.numpy as jnp
from jax import lax
import numpy as np

D_MODEL = 2048
BATCH = 8
SEQ = 2048
DEPTH = 1

CONV_CHANNELS = D_MODEL // 2
DW_TAPS = 31
N_HEADS = 16
HEAD_DIM = 128
N_KV_GROUPS = 4
GROUP_SIZE = N_HEADS // N_KV_GROUPS
ROT_DIM = HEAD_DIM // 4
ROPE_THETA = 500000.0
CMP_BLOCK = 32
CMP_STRIDE = 16
CMP_HIDDEN = 256
SEL_BLOCK = 64
N_SELECT = 16
N_FORCED_LOCAL = 2
WINDOW = 512
WIN_QBLOCK = 128
SEL_QCHUNK = 16
N_GROUPS = 8
EXPERTS_PER_GROUP = 8
N_EXPERTS = N_GROUPS * EXPERTS_PER_GROUP
TOP_K = 2
EXPERT_FF = 512
MOE_ROWBLOCK = 128

Q_COLS = N_HEADS * HEAD_DIM
KV_COLS = N_KV_GROUPS * HEAD_DIM
IN_COLS = 2 * CONV_CHANNELS + Q_COLS + 6 * KV_COLS + 3 * N_HEADS
NORM_EPS = 1e-6
NEG_INF = -1e30
FORCE_BONUS = 1e4

kernel_name = "hybrid_conv_nsa_hiermoe"


def rms_norm(x, g):
    xf = x.astype(jnp.float32)
    y = xf * lax.rsqrt(jnp.mean(xf * xf, axis=-1, keepdims=True) + NORM_EPS)
    return y.astype(x.dtype) * g


def layer_norm(x, g, b):
    xf = x.astype(jnp.float32)
    mu = jnp.mean(xf, axis=-1, keepdims=True)
    var = jnp.mean(jnp.square(xf - mu), axis=-1, keepdims=True)
    return ((xf - mu) * lax.rsqrt(var + NORM_EPS)).astype(x.dtype) * g + b


def rope_tables(seq):
    pos = jnp.arange(seq, dtype=jnp.float32)
    inv = ROPE_THETA ** (-jnp.arange(0, ROT_DIM, 2, dtype=jnp.float32) / ROT_DIM)
    ang = pos[:, None] * inv[None, :]
    return jnp.cos(ang), jnp.sin(ang)


def partial_rope(x, cos, sin):
    half = ROT_DIM // 2
    c = cos[:, None, :].astype(x.dtype)
    s = sin[:, None, :].astype(x.dtype)
    x1, x2, rest = x[..., :half], x[..., half:ROT_DIM], x[..., ROT_DIM:]
    return jnp.concatenate([x1 * c - x2 * s, x1 * s + x2 * c, rest], axis=-1)


def masked_softmax(s, mask):
    s = jnp.where(mask, s.astype(jnp.float32), NEG_INF)
    return jax.nn.softmax(s, axis=-1) * mask


def conformer_conv(u_a, u_b, dw, dw_b, ln_g, ln_b):
    glu = u_a * jax.nn.sigmoid(u_b)
    padded = jnp.pad(glu, ((0, 0), (DW_TAPS - 1, 0), (0, 0)))
    y = lax.conv_general_dilated(padded, dw[:, None, :], window_strides=(1,), padding='VALID',
                                 dimension_numbers=('NWC', 'WIO', 'NWC'),
                                 feature_group_count=CONV_CHANNELS) + dw_b
    return jax.nn.silu(layer_norm(y, ln_g, ln_b))


def compress(kv, pos_emb, w1, w2):
    seq = kv.shape[2]
    n_cmp = (seq - CMP_BLOCK) // CMP_STRIDE + 1
    idx = np.arange(n_cmp)[:, None] * CMP_STRIDE + np.arange(CMP_BLOCK)[None, :]
    blocks = kv[:, :, idx] + pos_emb
    flat = blocks.reshape(blocks.shape[0], blocks.shape[1], n_cmp, CMP_BLOCK * HEAD_DIM)
    return jax.nn.gelu(flat @ w1) @ w2


def block_importance_matrix(n_cmp, n_blk):
    r_sel, r_cmp = SEL_BLOCK // CMP_STRIDE, CMP_BLOCK // CMP_STRIDE
    a = np.zeros((n_cmp, n_blk), np.float32)
    j = np.arange(n_blk)
    for m in range(r_sel):
        for n in range(r_cmp):
            i = r_sel * j + m - n
            ok = (i >= 0) & (i < n_cmp)
            np.add.at(a, (i[ok], j[ok]), 1.0)
    return jnp.asarray(a)


def nsa_attention(q, k_cmp, v_cmp, k_slc, v_slc, k_win, v_win, gates,
                  pos_k, k_w1, k_w2, pos_v, v_w1, v_w2):
    B, G, R, S, dh = q.shape
    t = jnp.arange(S)

    kc = compress(k_cmp, pos_k, k_w1, k_w2)
    vc = compress(v_cmp, pos_v, v_w1, v_w2)
    n_cmp = kc.shape[2]
    cmp_end = jnp.arange(n_cmp) * CMP_STRIDE + CMP_BLOCK - 1
    s_cmp = jnp.einsum('bgrsd,bgcd->bgrsc', q, kc)
    p_cmp = masked_softmax(s_cmp, cmp_end[None, :] <= t[:, None])
    o_cmp = jnp.einsum('bgrsc,bgcd->bgrsd', p_cmp.astype(vc.dtype), vc)

    n_blk = S // SEL_BLOCK
    imp = jnp.einsum('bgrsc,cj->bgsj', p_cmp, block_importance_matrix(n_cmp, n_blk))
    blk = jnp.arange(n_blk)[None, :]
    cur = (t // SEL_BLOCK)[:, None]
    forced = (blk == 0) | ((blk <= cur) & (blk > cur - N_FORCED_LOCAL))
    imp = jnp.where(forced, imp + FORCE_BONUS, imp)
    imp = jnp.where(blk * SEL_BLOCK <= t[:, None], imp, NEG_INF)
    n_sel = min(N_SELECT, n_blk)
    _, sel_idx = lax.top_k(imp, n_sel)

    n_chunk = S // SEL_QCHUNK
    q_chunks = q.reshape(B, G, R, n_chunk, SEL_QCHUNK, dh).transpose(3, 0, 1, 2, 4, 5)
    idx_chunks = sel_idx.reshape(B, G, n_chunk, SEL_QCHUNK, n_sel).transpose(2, 0, 1, 3, 4)
    k_blk = k_slc.reshape(B, G, n_blk, SEL_BLOCK, dh)
    v_blk = v_slc.reshape(B, G, n_blk, SEL_BLOCK, dh)
    b_ix = jnp.arange(B)[:, None, None, None]
    g_ix = jnp.arange(G)[None, :, None, None]
    n_keys = n_sel * SEL_BLOCK

    def sel_chunk(args):
        qc, ic, start = args
        kg = k_blk[b_ix, g_ix, ic]
        vg = v_blk[b_ix, g_ix, ic].reshape(B, G, SEL_QCHUNK, n_keys, dh)
        tq = start + jnp.arange(SEL_QCHUNK)
        kpos = ic[..., None] * SEL_BLOCK + jnp.arange(SEL_BLOCK)
        mask = (kpos <= tq[None, None, :, None, None]).reshape(B, G, 1, SEL_QCHUNK, n_keys)
        s = jnp.einsum('bgrqd,bgqnkd->bgrqnk', qc, kg).reshape(B, G, R, SEL_QCHUNK, n_keys)
        p = masked_softmax(s, mask)
        return jnp.einsum('bgrqj,bgqjd->bgrqd', p.astype(vg.dtype), vg)

    o_slc = lax.map(sel_chunk, (q_chunks, idx_chunks, jnp.arange(n_chunk) * SEL_QCHUNK))
    o_slc = o_slc.transpose(1, 2, 3, 0, 4, 5).reshape(B, G, R, S, dh)

    n_wb = S // WIN_QBLOCK
    span = WIN_QBLOCK + WINDOW
    q_blocks = q.reshape(B, G, R, n_wb, WIN_QBLOCK, dh).transpose(3, 0, 1, 2, 4, 5)
    k_pad = jnp.pad(k_win, ((0, 0), (0, 0), (WINDOW, 0), (0, 0)))
    v_pad = jnp.pad(v_win, ((0, 0), (0, 0), (WINDOW, 0), (0, 0)))

    def win_block(args):
        qb, start = args
        kb = lax.dynamic_slice_in_dim(k_pad, start, span, axis=2)
        vb = lax.dynamic_slice_in_dim(v_pad, start, span, axis=2)
        tq = start + jnp.arange(WIN_QBLOCK)
        kpos = start - WINDOW + jnp.arange(span)
        diff = tq[:, None] - kpos[None, :]
        mask = (kpos[None, :] >= 0) & (diff >= 0) & (diff < WINDOW)
        s = jnp.einsum('bgrqd,bgkd->bgrqk', qb, kb)
        p = masked_softmax(s, mask)
        return jnp.einsum('bgrqk,bgkd->bgrqd', p.astype(vb.dtype), vb)

    o_win = lax.map(win_block, (q_blocks, jnp.arange(n_wb) * WIN_QBLOCK))
    o_win = o_win.transpose(1, 2, 3, 0, 4, 5).reshape(B, G, R, S, dh)

    o = gates[0] * o_cmp + gates[1] * o_slc + gates[2] * o_win
    return o.transpose(0, 3, 1, 2, 4).reshape(B, S, N_HEADS * HEAD_DIM)


def hier_moe(h, w_grp, b_grp, w_exp, b_exp, w1, w3, w2):
    B, S, D = h.shape
    T = B * S
    hf = h.reshape(T, D)
    grp_logits = (hf @ w_grp).astype(jnp.float32) + b_grp.astype(jnp.float32)
    p_grp = jax.nn.softmax(grp_logits, axis=-1)
    g_sel = jnp.argmax(grp_logits, axis=-1)
    p_g = jnp.take_along_axis(p_grp, g_sel[:, None], axis=-1)
    exp_logits = ((hf @ w_exp).astype(jnp.float32) + b_exp.astype(jnp.float32)).reshape(T, N_GROUPS, EXPERTS_PER_GROUP)
    in_grp = jnp.take_along_axis(exp_logits, g_sel[:, None, None], axis=1)[:, 0]
    top_val, top_loc = lax.top_k(in_grp, TOP_K)
    gate = p_g * jax.nn.softmax(top_val, axis=-1)
    expert = g_sel[:, None] * EXPERTS_PER_GROUP + top_loc

    n_assign = T * TOP_K
    flat_e = expert.reshape(-1)
    flat_tok = jnp.repeat(jnp.arange(T, dtype=jnp.int32), TOP_K)
    flat_w = gate.reshape(-1)
    order = jnp.argsort(flat_e)
    e_s, tok_s, w_s = flat_e[order], flat_tok[order], flat_w[order]
    counts = jnp.zeros((N_EXPERTS,), jnp.int32).at[flat_e].add(1)
    padded = (counts + MOE_ROWBLOCK - 1) // MOE_ROWBLOCK * MOE_ROWBLOCK
    start = jnp.cumsum(counts) - counts
    pend = jnp.cumsum(padded)
    pstart = pend - padded
    dest = pstart[e_s] + (jnp.arange(n_assign, dtype=jnp.int32) - start[e_s])
    n_blocks = -(-(n_assign + N_EXPERTS * (MOE_ROWBLOCK - 1)) // MOE_ROWBLOCK)
    P = n_blocks * MOE_ROWBLOCK
    row_tok = jnp.full((P,), T, jnp.int32).at[dest].set(tok_s)
    row_w = jnp.zeros((P,), jnp.float32).at[dest].set(w_s)
    block_e = jnp.minimum(jnp.searchsorted(pend, jnp.arange(n_blocks) * MOE_ROWBLOCK, side='right'), N_EXPERTS - 1)
    x_rows = jnp.concatenate([hf, jnp.zeros((1, D), hf.dtype)], axis=0)[row_tok].reshape(n_blocks, MOE_ROWBLOCK, D)

    def expert_block(args):
        xb, e = args
        return (jax.nn.silu(xb @ w1[e]) * (xb @ w3[e])) @ w2[e]

    y_rows = lax.map(expert_block, (x_rows, block_e)).reshape(P, D)
    y = jax.ops.segment_sum(y_rows * row_w[:, None].astype(y_rows.dtype), row_tok, num_segments=T + 1)[:T]
    return y.reshape(B, S, D)


def setup_inputs(seed: int = 0) -> dict:
    key = jax.random.key(seed)
    ks = jax.random.split(key, 27)
    f32 = jnp.float32

    def nrm(k, shape, scale):
        return jax.random.normal(k, shape, f32) * scale

    D, C, L = D_MODEL, CONV_CHANNELS, DEPTH
    return {
        "x": nrm(ks[0], (BATCH, SEQ, D), 1.0),
        "attn_norm": 1.0 + nrm(ks[1], (L, D), 0.01),
        "w_in": nrm(ks[2], (L, D, IN_COLS), D ** -0.5),
        "conv_dw": nrm(ks[3], (L, DW_TAPS, C), DW_TAPS ** -0.5),
        "conv_dw_b": nrm(ks[4], (L, C), 0.01),
        "conv_ln_g": 1.0 + nrm(ks[5], (L, C), 0.01),
        "conv_ln_b": nrm(ks[6], (L, C), 0.01),
        "cmp_pos_k": nrm(ks[7], (L, CMP_BLOCK, HEAD_DIM), 0.02),
        "cmp_k_w1": nrm(ks[8], (L, CMP_BLOCK * HEAD_DIM, CMP_HIDDEN), (CMP_BLOCK * HEAD_DIM) ** -0.5),
        "cmp_k_w2": nrm(ks[9], (L, CMP_HIDDEN, HEAD_DIM), CMP_HIDDEN ** -0.5),
        "cmp_pos_v": nrm(ks[10], (L, CMP_BLOCK, HEAD_DIM), 0.02),
        "cmp_v_w1": nrm(ks[11], (L, CMP_BLOCK * HEAD_DIM, CMP_HIDDEN), (CMP_BLOCK * HEAD_DIM) ** -0.5),
        "cmp_v_w2": nrm(ks[12], (L, CMP_HIDDEN, HEAD_DIM), CMP_HIDDEN ** -0.5),
        "w_proj_conv": nrm(ks[13], (L, C, D), C ** -0.5),
        "w_proj_nsa": nrm(ks[14], (L, Q_COLS, D), Q_COLS ** -0.5),
        "w_merge": nrm(ks[15], (L, D, 2 * D), D ** -0.5),
        "b_merge": nrm(ks[16], (L, 2 * D), 0.01),
        "w_out": nrm(ks[17], (L, D, D), D ** -0.5),
        "ffn_norm": 1.0 + nrm(ks[18], (L, D), 0.01),
        "w_grp": nrm(ks[19], (L, D, N_GROUPS), D ** -0.5),
        "b_grp": nrm(ks[20], (L, N_GROUPS), 0.01),
        "w_exp": nrm(ks[21], (L, D, N_EXPERTS), D ** -0.5),
        "b_exp": nrm(ks[22], (L, N_EXPERTS), 0.01),
        "exp_w1": nrm(ks[23], (L, N_EXPERTS, D, EXPERT_FF), D ** -0.5),
        "exp_w3": nrm(ks[24], (L, N_EXPERTS, D, EXPERT_FF), D ** -0.5),
        "exp_w2": nrm(ks[25], (L, N_EXPERTS, EXPERT_FF, D), EXPERT_FF ** -0.5),
        "final_norm": 1.0 + nrm(ks[26], (D,), 0.01),
    }


def reference(x, attn_norm, w_in, conv_dw, conv_dw_b, conv_ln_g, conv_ln_b,
              cmp_pos_k, cmp_k_w1, cmp_k_w2, cmp_pos_v, cmp_v_w1, cmp_v_w2,
              w_proj_conv, w_proj_nsa, w_merge, b_merge, w_out, ffn_norm,
              w_grp, b_grp, w_exp, b_exp, exp_w1, exp_w3, exp_w2, final_norm):
    B, S, _ = x.shape
    cos, sin = rope_tables(S)
    scale = HEAD_DIM ** -0.5
    sizes = [CONV_CHANNELS, CONV_CHANNELS, Q_COLS] + [KV_COLS] * 6
    split_points = [int(v) for v in np.cumsum(sizes)]

    def kv_heads(u, rotate):
        u = u.reshape(B, S, N_KV_GROUPS, HEAD_DIM)
        if rotate:
            u = partial_rope(u, cos, sin)
        return u.transpose(0, 2, 1, 3)

    for l in range(DEPTH):
        h = rms_norm(x, attn_norm[l])
        proj = h @ w_in[l]
        conv_a, conv_b, q, kc, vc, ksl, vsl, kw, vw, gl = jnp.split(proj, split_points, axis=-1)

        y_conv = conformer_conv(conv_a, conv_b, conv_dw[l], conv_dw_b[l], conv_ln_g[l], conv_ln_b[l]) @ w_proj_conv[l]

        qh = partial_rope(q.reshape(B, S, N_HEADS, HEAD_DIM), cos, sin) * scale
        qh = qh.reshape(B, S, N_KV_GROUPS, GROUP_SIZE, HEAD_DIM).transpose(0, 2, 3, 1, 4)
        gates = jax.nn.sigmoid(gl).reshape(B, S, 3, N_KV_GROUPS, GROUP_SIZE).transpose(2, 0, 3, 4, 1)[..., None]
        o_nsa = nsa_attention(qh, kv_heads(kc, True), kv_heads(vc, False), kv_heads(ksl, True), kv_heads(vsl, False),
                              kv_heads(kw, True), kv_heads(vw, False), gates,
                              cmp_pos_k[l], cmp_k_w1[l], cmp_k_w2[l], cmp_pos_v[l], cmp_v_w1[l], cmp_v_w2[l])
        y_nsa = o_nsa @ w_proj_nsa[l]

        g_conv, g_nsa = jnp.split(jax.nn.sigmoid(h @ w_merge[l] + b_merge[l]), 2, axis=-1)
        x = x + (g_conv * y_conv + g_nsa * y_nsa) @ w_out[l]

        x = x + hier_moe(rms_norm(x, ffn_norm[l]), w_grp[l], b_grp[l], w_exp[l], b_exp[l],
                         exp_w1[l], exp_w3[l], exp_w2[l])
    return rms_norm(x, final_norm)
```

```python
import numpy as np
import ml_dtypes
from contextlib import ExitStack
import concourse.bass as bass
import concourse.mybir as mybir
from concourse.bass_utils import run_bass_kernel_spmd

F32 = mybir.dt.float32
BF16 = mybir.dt.bfloat16
I32 = mybir.dt.int32
AF = mybir.ActivationFunctionType
ALU = mybir.AluOpType

D = 2048
S = 2048
NT = 16
CCH = 1024
TAPS = 31
NH = 16
NG = 4
INC = 7216
NEXP = 64
CAP = 192
R2 = CAP - 128
FF = 512
EPS = 1e-6
SCALE = 128 ** -0.5
NEG = -1.0e5
NSLOT = NEXP * CAP


class Op:
    __slots__ = ("eng", "fn", "deps", "signal", "val", "sem", "is_dma", "phase")


class Sched:
    ENG = ("pe", "act", "dve", "pool", "sp")
    BLK = {"pe": "tensor", "act": "scalar", "dve": "vector", "pool": "gpsimd", "sp": "sync"}
    PSUM_KEYS = {"B0", "B1", "OA", "OB", "OA1", "OB1", "TB", "TB2", "MISC"}

    def __init__(self, nc, stack):
        self.nc = nc
        self.stack = stack
        self.sems = {e: stack.enter_context(nc.semaphore("c_" + e)) for e in self.ENG}
        self.count = {e: 0 for e in self.ENG}
        self.ops = {e: [] for e in self.ENG}
        self.lastw = {}
        self.readers = {}
        self.dsems = {}
        self.dcount = {}
        self.waited = {e: {} for e in self.ENG}
        self.phase = 0
        self.phase_last = {}
        self.barrier = {e: [] for e in self.ENG}
        self.phase_map = {}

    def _mk(self, eng, fn, reads, writes):
        op = Op()
        op.eng = eng; op.fn = fn; op.signal = False; op.val = None; op.sem = None
        op.is_dma = False; op.phase = self.phase
        deps = []
        for k in reads:
            w = self.lastw.get(k)
            if w is not None:
                deps.append(w)
            if k in self.PSUM_KEYS:
                deps.extend(r for r in self.readers.get(k, []) if r.eng != eng)
            self.readers.setdefault(k, []).append(op)
        for k in writes:
            w = self.lastw.get(k)
            if w is not None:
                deps.append(w)
            deps.extend(self.readers.get(k, []))
            self.lastw[k] = op
            self.readers[k] = []
        deps.extend(self.barrier[eng])
        self.barrier[eng] = []
        out = []
        seen = set()
        for d in deps:
            if d is op:
                continue
            if (not d.is_dma) and d.phase < self.phase:
                d = self.phase_last[d.phase][d.eng]
            if id(d) in seen:
                continue
            seen.add(id(d))
            if (not d.is_dma) and d.eng == eng and eng == "pe":
                continue
            d.signal = True
            out.append(d)
        op.deps = out
        self.ops[eng].append(op)
        return op

    def add(self, eng, fn, reads=(), writes=()):
        return self._mk(eng, fn, reads, writes)

    def dma(self, eng, fn, sem, reads=(), writes=()):
        op = self._mk(eng, fn, reads, writes)
        op.is_dma = True
        pm = self.phase_map.setdefault(eng, {})
        sem = "g%s%d" % (eng, pm.setdefault(sem, len(pm)))
        if sem not in self.dsems:
            self.dsems[sem] = self.stack.enter_context(self.nc.semaphore("d_" + sem))
            self.dcount[sem] = 0
        self.dcount[sem] += 16
        op.sem = sem
        op.val = self.dcount[sem]
        return op

    def flush(self):
        nc = self.nc
        last = {}
        for e in self.ENG:
            comp = [o for o in self.ops[e] if not o.is_dma]
            if comp:
                comp[-1].signal = True
                last[e] = comp[-1]
        self.phase_last[self.phase] = last
        for e in self.ENG:
            c = self.count[e]
            for o in self.ops[e]:
                if not o.is_dma and o.signal:
                    c += 1
                    o.val = c
            self.count[e] = c
        ops_by_eng = {e: list(self.ops[e]) for e in self.ENG}
        outstanding_dma = [o for e in self.ENG for o in self.ops[e] if o.is_dma]
        with nc.Block() as block:
            for e in self.ENG:
                ops = ops_by_eng[e]
                if not ops:
                    continue

                def runner(h, ops=ops, e=e):
                    waited = self.waited[e]
                    for o in ops:
                        need = {}
                        for d in o.deps:
                            if d.is_dma:
                                s = self.dsems[d.sem]; key = "d_" + d.sem
                            else:
                                s = self.sems[d.eng]; key = "c_" + d.eng
                            if key not in need or need[key][1] < d.val:
                                need[key] = (s, d.val)
                        for key, (s, val) in need.items():
                            if waited.get(key, 0) >= val:
                                continue
                            h.wait_ge(s, val)
                            waited[key] = val
                        ins = o.fn(h)
                        if o.is_dma:
                            ins.then_inc(self.dsems[o.sem], 16)
                        elif o.signal:
                            ins.then_inc(self.sems[o.eng], 1)

                getattr(block, self.BLK[e])(runner)
        bl = list(last.values()) + outstanding_dma
        for e in self.ENG:
            self.barrier[e] = self.barrier[e] + bl
        self.ops = {e: [] for e in self.ENG}
        self.phase_map = {}
        self.phase += 1

    def bound_reg(self, h, val):
        if getattr(self, "_breg", None) is None:
            self._breg = h.alloc_register("bc")
            h.reg_mov(self._breg, val)
        return self._breg

    def finish(self):
        nc = self.nc
        with nc.Block() as block:
            def runner(h):
                for name, sem in self.dsems.items():
                    if self.dcount[name] > 0:
                        h.wait_ge(sem, self.dcount[name])
                for e in self.ENG:
                    if self.count[e] > 0:
                        h.wait_ge(self.sems[e], self.count[e])
            block.sync(runner)


def _consts():
    c = {}
    pos = np.arange(S, dtype=np.float32)
    inv = (500000.0 ** (-np.arange(0, 32, 2, dtype=np.float32) / 32)).astype(np.float32)
    ang = pos[:, None] * inv[None, :]
    c["rope_cs"] = np.concatenate([np.cos(ang), np.sin(ang)], axis=1).astype(np.float32)
    a = np.zeros((128, 32), np.float32)
    j = np.arange(32)
    for m in range(4):
        for n in range(2):
            i = 4 * j + m - n
            ok = (i >= 0) & (i < 127)
            np.add.at(a, (i[ok], j[ok]), 1.0)
    c["cmpA"] = a
    cc = np.arange(128)[:, None, None]; ii = np.arange(16)[None, :, None]; qq = np.arange(128)[None, None, :]
    c["cmpmask"] = np.where(16 * cc + 31 <= 128 * ii + qq, 0.0, NEG).astype(np.float32).reshape(128, 16 * 128)
    k = np.arange(128)[:, None]; q = np.arange(128)[None, :]
    tri = np.stack([np.where(k <= q, 0.0, NEG), np.where(k > q, 0.0, NEG)], axis=1).astype(np.float32)
    c["tri"] = tri.reshape(128, 256)
    e = np.zeros((32, 16, 128), np.float32)
    for jj in range(16):
        e[2 * jj, jj, :64] = 1.0
        e[2 * jj + 1, jj, 64:] = 1.0
    c["esel"] = e.reshape(32, 16 * 128)
    addm = np.zeros((128, 8, 32), np.float32)
    for i in range(8, 16):
        t = 128 * i + np.arange(128)[:, None]
        blk = np.arange(32)[None, :]
        cur = t // 64
        forced = (blk == 0) | ((blk <= cur) & (blk > cur - 2))
        avail = blk * 64 <= t
        addm[:, i - 8, :] = np.where(avail, np.where(forced, 1e4, 0.0), -1e30)
    c["addm"] = addm.reshape(128, 256)
    c["identf"] = np.eye(128, dtype=np.float32)
    c["upper"] = (np.arange(128)[:, None] < np.arange(128)[None, :]).astype(np.float32)
    c["eoff"] = np.broadcast_to((np.arange(64, dtype=np.float32) * CAP)[None, :], (128, 64)).copy()
    return c


def build(stop_after=None, nexp=NEXP):
    nc = bass.Bass("TRN2", target_bir_lowering=False)

    def din(name, shape, dt=F32):
        return nc.dram_tensor(name, list(shape), dt, kind="ExternalInput").ap()

    x = din("x", [S, D]); attn_norm = din("attn_norm", [16, 128]); w_in = din("w_in", [D, INC])
    conv_dw = din("conv_dw", [TAPS, CCH]); conv_dw_b = din("conv_dw_b", [8, 128])
    conv_ln_g = din("conv_ln_g", [8, 128]); conv_ln_b = din("conv_ln_b", [8, 128])
    cmp_pos = [din("cmp_pos_k", [32, 128]), din("cmp_pos_v", [32, 128])]
    cmp_w1 = [din("cmp_k_w1", [4096, 256]), din("cmp_v_w1", [4096, 256])]
    cmp_w2 = [din("cmp_k_w2", [256, 128]), din("cmp_v_w2", [256, 128])]
    w_proj_conv = din("w_proj_conv", [CCH, D]); w_proj_nsa = din("w_proj_nsa", [D, D])
    w_merge = din("w_merge", [D, 2 * D]); b_merge = din("b_merge", [32, 128]); w_out = din("w_out", [D, D])
    ffn_norm = din("ffn_norm", [1, D]); w_gr = din("w_gr", [D, 72]); b_gr = din("b_gr", [1, 72])
    exp_w1 = din("exp_w1", [nexp, D, FF]); exp_w3 = din("exp_w3", [nexp, D, FF]); exp_w2 = din("exp_w2", [nexp, FF, D])
    final_norm = din("final_norm", [1, D])
    rope_cs = din("rope_cs", [S, 32]); cmpA = din("cmpA", [128, 32]); cmpmask_d = din("cmpmask", [128, 2048])
    tri_d = din("tri", [128, 256]); esel_d = din("esel", [32, 2048]); addm_d = din("addm", [128, 256])
    identf_d = din("identf", [128, 128]); upper_d = din("upper", [128, 128]); eoff_d = din("eoff", [128, 64])

    dbg = stop_after is not None
    out = nc.dram_tensor("out", [S, D], F32, kind="ExternalOutput").ap()

    dbg_out = {"P1": ["HT"], "P2": ["UT"], "P3": ["OT"], "P4": ["X1", "RT"], "P5": []}.get(stop_after, [])

    def scratch(name, shape, dt):
        kind = "ExternalOutput" if name in dbg_out else "Internal"
        return nc.dram_tensor(name, list(shape), dt, kind=kind).ap()

    HT = scratch("HT", [D, S], BF16)
    UT = scratch("UT", [CCH, S], BF16)
    OT = scratch("OT", [D, S], BF16)
    X1 = scratch("X1", [S, D], F32)
    XS = scratch("XS", [NSLOT, D], BF16)
    YS = scratch("YS", [NSLOT, D], BF16)
    RT = scratch("RT", [S, 4], F32)
    import os as _os
    _skip = tuple(int(v) for v in _os.environ.get("DBG_SKIP", "3,7,11,13,15").split(",") if v != "")
    PRE = [e for e in range(nexp) if e % 16 not in _skip] if nexp == NEXP else []
    pre_idx = {e: j for j, e in enumerate(PRE)}
    npre = max(1, len(PRE))
    EB1 = nc.dram_tensor("EB1", [npre, 128, 16 * FF], BF16).ap()
    EB3 = nc.dram_tensor("EB3", [npre, 128, 16 * FF], BF16).ap()
    EB2 = nc.dram_tensor("EB2", [npre, 128, 4 * D], BF16).ap()
    bg_list = [(e, w) for e in PRE for w in range(3)]
    bg_pos = [0]

    with ExitStack() as top:
        Sc = Sched(nc, top)

        def sb(st, name, shape, dt):
            return st.enter_context(nc.sbuf_tensor("s_" + name, list(shape), dt))

        def ps(st, name, shape, dt):
            return st.enter_context(nc.psum_tensor("p_" + name, list(shape), dt))

        identf = sb(top, "identf", [128, 128], F32)
        identb = sb(top, "identb", [128, 128], BF16)
        onesb = sb(top, "onesb", [128, 128], BF16)
        Sc.dma("sp", lambda h: h.dma_start(out=identf[:], in_=identf_d[:, :]), "c0", writes=["identf"])
        Sc.dma("pool", lambda h: h.dma_start(out=identb[:], in_=identf_d[:, :]), "c1", writes=["identb"])
        Sc.add("pool", lambda h: h.memset(onesb[:], 1.0), writes=["onesb"])

        def bg(n=1):
            for _ in range(n):
                if bg_pos[0] >= len(bg_list):
                    return
                e, w = bg_list[bg_pos[0]]
                bg_pos[0] += 1
                j = pre_idx[e]
                src = [exp_w1, exp_w3, exp_w2][w][e].rearrange("(k p) n -> p k n", p=128)
                dst = [EB1, EB3, EB2][w][j].rearrange("p (k n) -> p k n", n=(D if w == 2 else FF))
                Sc.dma("pool", lambda h, src=src, dst=dst: h.dma_start(out=dst, in_=src), "bg", writes=["EB%d_%d" % (e, w)])

        def load_T(st, name, dram_ap, R, dst_ap, MISC):
            stg = sb(st, "stg_" + name, [32, 128], F32)
            Sc.dma("sp", lambda h: h.dma_start(out=stg[0:R, :], in_=dram_ap), "ld_" + name, writes=["stg_" + name])
            Sc.add("pe", lambda h: h.transpose(out=MISC[:, 0:R], in_=stg[0:R, :], identity=identf[0:R, 0:R]),
                   reads=["stg_" + name, "identf"], writes=["MISC"])
            Sc.add("dve", lambda h: h.tensor_copy(out=dst_ap, in_=MISC[:, 0:R]), reads=["MISC"], writes=[name])

        with ExitStack() as stA:
            hT = sb(stA, "hT", [128, 16, S], BF16)
            with ExitStack() as st:
                TB = ps(st, "TB_1", [128, 8, 128], BF16); TB2 = ps(st, "TB2_1", [128, 8, 128], BF16); MISC = ps(st, "MISC_1", [128, 512], F32)
                TBS = [TB, TB2]
                gT = sb(st, "gT", [128, 16], F32)
                load_T(st, "gT", attn_norm[:, :], 16, gT[:, :], MISC)
                xt = [sb(st, "xt%d" % i, [128, D], F32) for i in range(2)]
                xb = [sb(st, "xb%d" % i, [128, D], BF16) for i in range(2)]
                junk = sb(st, "junk", [128, D], BF16)
                ss = [sb(st, "ss%d" % i, [128, 1], F32) for i in range(2)]
                import os
                for i in range(int(os.environ.get("DBG_NT", NT))):
                    s = i % 2
                    Sc.dma("sp", lambda h, i=i, s=s: h.dma_start(out=xt[s][:], in_=x[i * 128:(i + 1) * 128, :]), "xt%d" % s, writes=["xt%d" % s])
                    Sc.add("act", lambda h, s=s: h.activation(out=junk[:], in_=xt[s][:], func=AF.Square, accum_out=ss[s][:]),
                           reads=["xt%d" % s], writes=["junk", "ss%d" % s])
                    Sc.add("act", lambda h, s=s: h.activation(out=ss[s][:], in_=ss[s][:], func=AF.Sqrt, scale=1.0 / D, bias=EPS),
                           reads=["ss%d" % s], writes=["ss%d" % s])
                    Sc.add("dve", lambda h, s=s: h.reciprocal(out=ss[s][:], in_=ss[s][:]), reads=["ss%d" % s], writes=["ss%d" % s])
                    Sc.add("dve", lambda h, s=s: h.tensor_scalar(out=xb[s][:], in0=xt[s][:], scalar1=ss[s][:, 0:1], scalar2=None, op0=ALU.mult),
                           reads=["xt%d" % s, "ss%d" % s], writes=["xb%d" % s])
                    for c4 in range(4):
                        for j in range(4):
                            c = 4 * c4 + j
                            Sc.add("pe", lambda h, s=s, c=c, j=j, c4=c4: h.transpose(out=TBS[c4 % 2][:, j, :], in_=xb[s][:, c * 128:(c + 1) * 128], identity=identb[:]),
                                   reads=["xb%d" % s, "identb"], writes=[["TB", "TB2"][c4 % 2]])
                        Sc.add("dve", lambda h, i=i, c4=c4: h.tensor_tensor(out=hT[:, 4 * c4:4 * c4 + 4, i * 128:(i + 1) * 128], in0=TBS[c4 % 2][:, 0:4, :],
                                                                   in1=gT[:, 4 * c4:4 * c4 + 4].unsqueeze(2).to_broadcast([128, 4, 128]), op=ALU.mult),
                               reads=[["TB", "TB2"][c4 % 2], "gT"], writes=["hT"])
                for kc in range(16):
                    Sc.dma("sp", lambda h, kc=kc: h.dma_start(out=HT[kc * 128:(kc + 1) * 128, :], in_=hT[:, kc, :]), "HTw", reads=["hT"], writes=["HT"])
                Sc.flush()
            if stop_after == "P1":
                Sc.finish(); return nc

            with ExitStack() as st:
                B0 = ps(st, "B0_2", [128, 512], F32); B1 = ps(st, "B1_2", [128, 512], F32); MISC = ps(st, "MISC_2", [128, 512], F32)
                dwT = sb(st, "dwT", [128, 8, 32], F32)
                cb = sb(st, "cb", [128, 3, 8], F32)
                dws = sb(st, "dws", [32, CCH], F32)
                Sc.dma("sp", lambda h: h.dma_start(out=dws[0:TAPS, :], in_=conv_dw[:, :]), "dws", writes=["dws"])
                for cc in range(8):
                    Sc.add("pe", lambda h, cc=cc: h.transpose(out=MISC[:, 0:TAPS], in_=dws[0:TAPS, cc * 128:(cc + 1) * 128], identity=identf[0:TAPS, 0:TAPS]),
                           reads=["dws", "identf"], writes=["MISC"])
                    Sc.add("dve", lambda h, cc=cc: h.tensor_copy(out=dwT[:, cc, 0:TAPS], in_=MISC[:, 0:TAPS]), reads=["MISC"], writes=["dwT"])
                load_T(st, "cb0", conv_dw_b[:, :], 8, cb[:, 0, :], MISC)
                load_T(st, "cb1", conv_ln_g[:, :], 8, cb[:, 1, :], MISC)
                load_T(st, "cb2", conv_ln_b[:, :], 8, cb[:, 2, :], MISC)
                zt = sb(st, "zt", [128, 1, D], BF16)
                Sc.add("pool", lambda h: h.memset(zt[:], 0.0), writes=["zt"])
                for zi in range(NSLOT // 2048):
                    Sc.dma("sp", lambda h, zi=zi: h.dma_start(out=XS[zi * 2048:(zi + 1) * 2048, :].rearrange("(a p) d -> p a d", p=128), in_=zt[:, 0:1, :].to_broadcast([128, 16, D])), "xsz%d" % zi, reads=["zt"], writes=["XSz%d" % zi])
                y_all = sb(st, "y_all", [128, 8, S], BF16)
                w_in_v = w_in.rearrange("(k p) n -> p k n", p=128)
                with ExitStack() as sc:
                    CA = [ps(sc, "CA0_2", [128, 512], F32), ps(sc, "CA1_2", [128, 512], F32)]
                    wa = sb(sc, "wa", [128, 16, 512], BF16); wb = sb(sc, "wb", [128, 16, 512], BF16)
                    glu2 = [sb(sc, "glu%d" % i, [128, 30 + S], BF16) for i in range(2)]
                    diag2 = [sb(sc, "diag%d" % i, [128, TAPS, 128], BF16) for i in range(2)]
                    sig = sb(sc, "sig", [128, 512], F32)
                    for i in range(2):
                        Sc.add("pool", lambda h, i=i: h.memset(glu2[i][:, 0:30], 0.0), writes=["glu%d" % i])

                    def c_proj(cc, tt):
                        c4 = cc % 4
                        glu = glu2[cc % 2]; kg = "glu%d" % (cc % 2)
                        for k in range(16):
                            Sc.add("pe", lambda h, k=k: h.matmul(B0[:, :], lhsT=wa[:, k, c4 * 128:(c4 + 1) * 128], rhs=hT[:, k, tt * 512:(tt + 1) * 512], start=(k == 0), stop=(k == 15)),
                                   reads=["wa", "hT"], writes=["B0"])
                        for k in range(16):
                            Sc.add("pe", lambda h, k=k: h.matmul(B1[:, :], lhsT=wb[:, k, c4 * 128:(c4 + 1) * 128], rhs=hT[:, k, tt * 512:(tt + 1) * 512], start=(k == 0), stop=(k == 15)),
                                   reads=["wb", "hT"], writes=["B1"])
                        Sc.add("act", lambda h: h.activation(out=sig[:], in_=B1[:, :], func=AF.Sigmoid), reads=["B1"], writes=["sig"])
                        Sc.add("dve", lambda h: h.tensor_tensor(out=glu[:, 30 + tt * 512:30 + (tt + 1) * 512], in0=B0[:, :], in1=sig[:], op=ALU.mult),
                               reads=["B0", "sig"], writes=[kg])

                    def c_diag(cc):
                        dg = diag2[cc % 2]
                        for k in range(TAPS):
                            Sc.add("pool", lambda h, k=k: h.tensor_scalar(out=dg[:, k, :], in0=identb[:], scalar1=dwT[:, cc, k:k + 1], scalar2=None, op0=ALU.mult),
                                   reads=["identb", "dwT"], writes=["diag%d" % (cc % 2)])

                    def c_conv(cc, tt):
                        glu = glu2[cc % 2]; kg = "glu%d" % (cc % 2); dg = diag2[cc % 2]
                        bank = CA[tt % 2]; bkey = ["OA", "OA1"][tt % 2]
                        for k in range(TAPS):
                            Sc.add("pe", lambda h, k=k: h.matmul(bank[:, :], lhsT=dg[:, k, :], rhs=glu[:, k + tt * 512:k + (tt + 1) * 512], start=(k == 0), stop=(k == TAPS - 1)),
                                   reads=[kg, "diag%d" % (cc % 2)], writes=[bkey])
                        Sc.add("act", lambda h: h.activation(out=y_all[:, cc, tt * 512:(tt + 1) * 512], in_=bank[:, :], func=AF.Identity, bias=cb[:, 0, cc:cc + 1]),
                               reads=[bkey, "cb0"], writes=["y_all"])

                    def c_loadw(gi):
                        Sc.dma("pool", lambda h: h.dma_start(out=wa[:], in_=w_in_v[:, :, gi * 512:(gi + 1) * 512]), "wa", writes=["wa"])
                        Sc.dma("pool", lambda h: h.dma_start(out=wb[:], in_=w_in_v[:, :, CCH + gi * 512:CCH + (gi + 1) * 512]), "wb", writes=["wb"])

                    c_loadw(0)
                    c_diag(0); c_diag(1)
                    for tt in range(4):
                        c_proj(0, tt)
                    for cc in range(8):
                        if cc + 1 < 8 and (cc + 1) % 4 == 0:
                            c_loadw((cc + 1) // 4)
                            for tt in range(4):
                                c_conv(cc, tt)
                            for tt in range(4):
                                c_proj(cc + 1, tt)
                        else:
                            for tt in range(4):
                                if cc + 1 < 8:
                                    c_proj(cc + 1, tt)
                                c_conv(cc, tt)
                        if cc + 2 < 8:
                            c_diag(cc + 2)
                        bg(2)
                    Sc.flush()
                acc = sb(st, "acc", [128, S], F32)
                mean_b = sb(st, "mean_b", [128, S], F32); rstd_b = sb(st, "rstd_b", [128, S], F32)
                ysq = sb(st, "ysq", [128, 512], BF16); msq = sb(st, "msq", [128, 512], F32)
                for tt in range(4):
                    tsl = slice(tt * 512, (tt + 1) * 512)
                    for cc in range(8):
                        Sc.add("pe", lambda h, cc=cc, tsl=tsl: h.matmul(B0[:, :], lhsT=onesb[:, :], rhs=y_all[:, cc, tsl], start=(cc == 0), stop=(cc == 7)),
                               reads=["y_all", "onesb"], writes=["B0"])
                    for cc in range(8):
                        Sc.add("act", lambda h, cc=cc, tsl=tsl: h.activation(out=ysq[:], in_=y_all[:, cc, tsl], func=AF.Square), reads=["y_all"], writes=["ysq"])
                        Sc.add("pe", lambda h, cc=cc: h.matmul(B1[:, :], lhsT=onesb[:, :], rhs=ysq[:], start=(cc == 0), stop=(cc == 7)),
                               reads=["ysq", "onesb"], writes=["B1"])
                    Sc.add("dve", lambda h, tsl=tsl: h.tensor_scalar(out=mean_b[:, tsl], in0=B0[:, :], scalar1=1.0 / CCH, scalar2=None, op0=ALU.mult), reads=["B0"], writes=["mean_b"])
                    Sc.add("dve", lambda h, tsl=tsl: h.tensor_tensor(out=msq[:], in0=mean_b[:, tsl], in1=mean_b[:, tsl], op=ALU.mult), reads=["mean_b"], writes=["msq"])
                    Sc.add("dve", lambda h, tsl=tsl: h.scalar_tensor_tensor(out=rstd_b[:, tsl], in0=B1[:, :], scalar=1.0 / CCH, in1=msq[:], op0=ALU.mult, op1=ALU.subtract),
                           reads=["B1", "msq"], writes=["rstd_b"])
                    Sc.add("act", lambda h, tsl=tsl: h.activation(out=rstd_b[:, tsl], in_=rstd_b[:, tsl], func=AF.Sqrt, bias=EPS), reads=["rstd_b"], writes=["rstd_b"])
                    Sc.add("dve", lambda h, tsl=tsl: h.reciprocal(out=rstd_b[:, tsl], in_=rstd_b[:, tsl]), reads=["rstd_b"], writes=["rstd_b"])
                ub = [sb(st, "ub%d" % i, [128, S], BF16) for i in range(2)]
                for cc in range(8):
                    s = cc % 2
                    Sc.add("dve", lambda h, cc=cc: h.tensor_tensor(out=acc[:], in0=y_all[:, cc, :], in1=mean_b[:], op=ALU.subtract), reads=["y_all", "mean_b"], writes=["acc"])
                    Sc.add("dve", lambda h: h.tensor_tensor(out=acc[:], in0=acc[:], in1=rstd_b[:], op=ALU.mult), reads=["acc", "rstd_b"], writes=["acc"])
                    Sc.add("act", lambda h, cc=cc, s=s: h.activation(out=ub[s][:], in_=acc[:], func=AF.Silu, scale=cb[:, 1, cc:cc + 1], bias=cb[:, 2, cc:cc + 1]),
                           reads=["acc", "cb1", "cb2"], writes=["ub%d" % s])
                    Sc.dma("sp", lambda h, cc=cc, s=s: h.dma_start(out=UT[cc * 128:(cc + 1) * 128, :], in_=ub[s][:]), "UTw%d" % s, reads=["ub%d" % s], writes=["UT"])
                Sc.flush()
            if stop_after == "P2":
                Sc.finish(); return nc

            with ExitStack() as st:
                B0 = ps(st, "B0_3", [128, 512], F32); B1 = ps(st, "B1_3", [128, 512], F32); MISC = ps(st, "MISC_3", [128, 512], F32)
                OA0 = ps(st, "OA0_3", [128, 512], F32); OA1 = ps(st, "OA1_3", [128, 512], F32)
                OB0 = ps(st, "OB0_3", [128, 512], F32); OB1 = ps(st, "OB1_3", [128, 512], F32)
                TB = ps(st, "TB_3", [128, 8, 128], BF16)
                cs = sb(st, "cs", [128, NT, 32], F32)
                Sc.dma("sp", lambda h: h.dma_start(out=cs[:], in_=rope_cs.rearrange("(i p) c -> p i c", p=128)), "cs", writes=["cs"])
                cmpmask = sb(st, "cmpmask", [128, NT, 128], BF16)
                Sc.dma("pool", lambda h: h.dma_start(out=cmpmask[:], in_=cmpmask_d.rearrange("p (i q) -> p i q", q=128)), "cmpmask", writes=["cmpmask"])
                tri = sb(st, "tri", [128, 2, 128], BF16)
                Sc.dma("pool", lambda h: h.dma_start(out=tri[:], in_=tri_d.rearrange("p (i q) -> p i q", q=128)), "tri", writes=["tri"])
                esel = sb(st, "esel", [32, 16, 128], BF16)
                Sc.dma("pool", lambda h: h.dma_start(out=esel[:], in_=esel_d.rearrange("p (i q) -> p i q", q=128)), "esel", writes=["esel"])
                addm = sb(st, "addm", [128, 8, 32], F32)
                Sc.dma("sp", lambda h: h.dma_start(out=addm[:], in_=addm_d.rearrange("p (i q) -> p i q", q=32)), "addm", writes=["addm"])
                wg = sb(st, "wg", [128, 16, 48], BF16)
                w_in_v = w_in.rearrange("(k p) n -> p k n", p=128)
                Sc.dma("pool", lambda h: h.dma_start(out=wg[:], in_=w_in_v[:, :, 7168:7216]), "wg", writes=["wg"])
                w2 = [sb(st, "w2_%d" % i, [128, 2, 128], BF16) for i in range(2)]
                posT = [sb(st, "posT%d" % i, [128, 32], BF16) for i in range(2)]
                posf2 = [sb(st, "posf%d" % i, [128, 32], F32) for i in range(2)]
                for kv in range(2):
                    Sc.dma("pool", lambda h, kv=kv: h.dma_start(out=w2[kv][:], in_=cmp_w2[kv].rearrange("(b p) d -> p b d", p=128)), "w2_%d" % kv, writes=["w2_%d" % kv])
                    load_T(st, "posf%d" % kv, cmp_pos[kv][:, :], 32, posf2[kv][:, :], MISC)
                    Sc.add("dve", lambda h, kv=kv: h.tensor_copy(out=posT[kv][:], in_=posf2[kv][:]), reads=["posf%d" % kv], writes=["posT%d" % kv])
                gates = sb(st, "gates", [128, NT, 48], F32)
                arena = sb(st, "arena", [128, 20480], BF16)
                wq = arena[:, 0:8192].rearrange("p (k n) -> p k n", n=512)
                wkv = arena[:, 8192:20480].rearrange("p (k s n) -> p k s n", s=6, n=128)
                w1 = [arena[:, 0:8192].rearrange("p (j n) -> p j n", n=256), arena[:, 8192:16384].rearrange("p (j n) -> p j n", n=256)]
                qT = sb(st, "qT", [128, 4, S], BF16)
                kT = sb(st, "kT", [128, 3, S], BF16)
                vcT = sb(st, "vcT", [128, S], BF16)
                vslc = sb(st, "vslc", [128, NT, 132], BF16)
                vwin = sb(st, "vwin", [128, NT, 132], BF16)
                vcx = sb(st, "vcx", [128, 164], BF16)
                kcT = sb(st, "kcT", [128, 128], BF16)
                hid = [sb(st, "hid%d" % i, [128, 2, 128], BF16) for i in range(2)]
                qtok2 = [sb(st, "qtok%d" % i, [128, 4, 128], BF16) for i in range(2)]
                ktok2 = [sb(st, "ktok%d" % i, [128, 4, 128], BF16) for i in range(2)]
                rt = [sb(st, "rt%d" % i, [128, 4, 16], F32) for i in range(2)]
                cmpAf = sb(st, "cmpAf", [128, 32], F32)
                Sc.dma("sp", lambda h: h.dma_start(out=cmpAf[:], in_=cmpA[:, :]), "cmpAf", writes=["cmpAf"])
                Sc.add("pool", lambda h: h.memset(vcx[:], 0.0), writes=["vcx"])
                Sc.add("pool", lambda h: h.memset(vcx[:, 128:129], 1.0), reads=["vcx"], writes=["vcx"])
                Sc.add("dve", lambda h: h.tensor_copy(out=vcx[:, 129:161], in_=cmpAf[:]), reads=["cmpAf", "vcx"], writes=["vcx"])
                Sc.add("pool", lambda h: h.memset(vslc[:, :, 128:129], 1.0), writes=["vslc"])
                Sc.add("pool", lambda h: h.memset(vwin[:, :, 128:129], 1.0), writes=["vwin"])
                pT = [sb(st, "pT%d" % i, [128, 512], BF16) for i in range(3)]
                gsq = sb(st, "gsq", [128, 128], F32); gu = sb(st, "gu", [128, 128], F32)
                den = sb(st, "den", [128, 3, 4], F32); fac = sb(st, "fac", [128, 3, 4], F32)
                imp = sb(st, "imp", [128, 32], F32); impw = sb(st, "impw", [128, 32], F32); m8 = sb(st, "m8", [128, 16], F32)
                negsel = sb(st, "negsel", [128, 32], BF16); negselT = sb(st, "negselT", [32, 128], BF16)
                oacc = sb(st, "oacc", [128, 4, 128], F32); otok = sb(st, "otok", [128, 4, 128], BF16)
                oTb = [sb(st, "oTb%d" % i, [128, 4, 512], BF16) for i in range(2)]
                SB = [B0, B1, MISC]
                OAv = [OA0[:, :].rearrange("p (a b) -> p a b", b=256), OA1[:, :].rearrange("p (a b) -> p a b", b=256)]
                OBv = [OB0[:, :].rearrange("p (a b) -> p a b", b=256), OB1[:, :].rearrange("p (a b) -> p a b", b=256)]
                pcount = [0]

                def rope(src, nh, dst, i, tag, wkey):
                    cosb = cs[:, i, 0:16].unsqueeze(1).to_broadcast([128, nh, 16])
                    sinb = cs[:, i, 16:32].unsqueeze(1).to_broadcast([128, nh, 16])
                    x1 = src[:, :, 0:16]; x2 = src[:, :, 16:32]
                    t0 = rt[0][:, 0:nh, :]; t1 = rt[1][:, 0:nh, :]
                    rd = [tag]
                    Sc.add("dve", lambda h: h.tensor_tensor(out=t0, in0=x1, in1=cosb, op=ALU.mult), reads=rd + ["cs"], writes=["rt0"])
                    Sc.add("dve", lambda h: h.tensor_tensor(out=t1, in0=x2, in1=sinb, op=ALU.mult), reads=rd + ["cs"], writes=["rt1"])
                    Sc.add("dve", lambda h: h.tensor_tensor(out=dst[:, :, 0:16], in0=t0, in1=t1, op=ALU.subtract), reads=["rt0", "rt1"], writes=[wkey])
                    Sc.add("dve", lambda h: h.tensor_tensor(out=t0, in0=x1, in1=sinb, op=ALU.mult), reads=rd + ["cs"], writes=["rt0"])
                    Sc.add("dve", lambda h: h.tensor_tensor(out=t1, in0=x2, in1=cosb, op=ALU.mult), reads=rd + ["cs"], writes=["rt1"])
                    Sc.add("dve", lambda h: h.tensor_tensor(out=dst[:, :, 16:32], in0=t0, in1=t1, op=ALU.add), reads=["rt0", "rt1"], writes=[wkey])

                for g in range(NG):
                    Sc.dma("pool", lambda h, g=g: h.dma_start(out=wq, in_=w_in_v[:, :, 2048 + g * 512:2048 + (g + 1) * 512]), "wq", writes=["arena0"])
                    for s6 in range(6):
                        c0 = 4096 + s6 * 512 + g * 128
                        Sc.dma("pool", lambda h, s6=s6, c0=c0: h.dma_start(out=wkv[:, :, s6, :], in_=w_in_v[:, :, c0:c0 + 128]), "wkv", writes=["arena1"])
                    def proj_mm(i):
                        p = i % 2
                        tsl = slice(i * 128, (i + 1) * 128)
                        bq = [B0, MISC][p]; kq = ["B0", "MISC"][p]
                        b1 = [B1, OB1][p]; k1 = ["B1", "OB1"][p]
                        b2 = [OA0, OA1][p]; k2 = ["OA", "OA1"][p]
                        for k in range(16):
                            Sc.add("pe", lambda h, k=k: h.matmul(bq[:, :], lhsT=hT[:, k, tsl], rhs=wq[:, k, :], start=(k == 0), stop=(k == 15)), reads=["hT", "arena0"], writes=[kq])
                        for k in range(16):
                            Sc.add("pe", lambda h, k=k: h.matmul(b1[:, :], lhsT=hT[:, k, tsl], rhs=wkv[:, k, 0:4, :], start=(k == 0), stop=(k == 15)), reads=["hT", "arena1"], writes=[k1])
                        for k in range(16):
                            Sc.add("pe", lambda h, k=k: h.matmul(b2[:, 0:256], lhsT=hT[:, k, tsl], rhs=wkv[:, k, 4:6, :], start=(k == 0), stop=(k == 15)), reads=["hT", "arena1"], writes=[k2])
                        if g == 0:
                            for k in range(16):
                                Sc.add("pe", lambda h, k=k: h.matmul(OB0[:, 0:48], lhsT=hT[:, k, tsl], rhs=wg[:, k, :], start=(k == 0), stop=(k == 15)), reads=["hT", "wg"], writes=["OB"])

                    def proj_evac(i):
                        p = i % 2
                        bq = [B0, MISC][p]; kq = ["B0", "MISC"][p]
                        b1 = [B1, OB1][p]; k1 = ["B1", "OB1"][p]
                        b2 = [OA0, OA1][p]; k2 = ["OA", "OA1"][p]
                        qtk = qtok2[p]; ktk = ktok2[p]; sq = "qtok%d" % p; sk = "ktok%d" % p
                        if g == 0:
                            Sc.add("act", lambda h: h.activation(out=gates[:, i, :], in_=OB0[:, 0:48], func=AF.Sigmoid), reads=["OB"], writes=["gates"])
                        B0v = bq[:, :].rearrange("p (a b) -> p a b", b=128)
                        B1v = b1[:, :].rearrange("p (a b) -> p a b", b=128)
                        OAp = b2[:, 0:256].rearrange("p (a b) -> p a b", b=128)
                        Sc.add("act", lambda h: h.activation(out=qtk[:, :, 32:128], in_=B0v[:, :, 32:128], func=AF.Copy), reads=[kq], writes=[sq + "a"])
                        rope(B0v, 4, qtk[:, :, :], i, kq, sq + "r")
                        Sc.add("act", lambda h: h.activation(out=ktk[:, 0:2, 32:128], in_=B1v[:, 0:4:2, 32:128], func=AF.Copy), reads=[k1], writes=[sk + "a"])
                        rope(B1v[:, 0:4:2, :], 2, ktk[:, 0:2, :], i, k1, sk + "r1")
                        Sc.add("act", lambda h: h.activation(out=ktk[:, 2:3, 32:128], in_=OAp[:, 0:1, 32:128], func=AF.Copy), reads=[k2], writes=[sk + "b"])
                        rope(OAp[:, 0:1, :], 1, ktk[:, 2:3, :], i, k2, sk + "r2")
                        Sc.add("act", lambda h: h.activation(out=ktk[:, 3, :], in_=B1v[:, 1, :], func=AF.Copy), reads=[k1], writes=[sk + "c"])
                        Sc.add("act", lambda h: h.activation(out=vslc[:, i, 0:128], in_=B1v[:, 3, :], func=AF.Copy), reads=[k1], writes=["vslc"])
                        Sc.add("act", lambda h: h.activation(out=vwin[:, i, 0:128], in_=OAp[:, 1, :], func=AF.Copy), reads=[k2], writes=["vwin"])

                    def proj_tr(i):
                        p = i % 2
                        tsl = slice(i * 128, (i + 1) * 128)
                        qtk = qtok2[p]; ktk = ktok2[p]; sq = "qtok%d" % p; sk = "ktok%d" % p
                        for r in range(4):
                            Sc.add("pe", lambda h, r=r: h.transpose(out=TB[:, r, :], in_=qtk[:, r, :], identity=identb[:]), reads=[sq + "a", sq + "r", "identb"], writes=["TB"])
                        for r in range(4):
                            Sc.add("pe", lambda h, r=r: h.transpose(out=TB[:, 4 + r, :], in_=ktk[:, r, :], identity=identb[:]),
                                   reads=[sk + "a", sk + "b", sk + "c", sk + "r1", sk + "r2", "identb"], writes=["TB"])
                        Sc.add("act", lambda h: h.activation(out=qT[:, :, tsl], in_=TB[:, 0:4, :], func=AF.Copy), reads=["TB"], writes=["qT"])
                        Sc.add("act", lambda h: h.activation(out=kT[:, :, tsl], in_=TB[:, 4:7, :], func=AF.Copy), reads=["TB"], writes=["kT"])
                        Sc.add("act", lambda h: h.activation(out=vcT[:, tsl], in_=TB[:, 7, :], func=AF.Copy), reads=["TB"], writes=["vcT"])

                    proj_mm(0); proj_evac(0)
                    for i in range(NT):
                        if i + 1 < NT:
                            proj_mm(i + 1); proj_evac(i + 1)
                        proj_tr(i)
                        if i % 2 == 1:
                            bg(1)
                    for kv in range(2):
                        Sc.dma("pool", lambda h, kv=kv: h.dma_start(out=w1[kv], in_=cmp_w1[kv].rearrange("(j d) n -> d j n", d=128)), "w1_%d" % kv, writes=["arena%d" % kv])
                    for kv in range(2):
                        srcT = kT[:, 0, :] if kv == 0 else vcT[:, :]
                        skey = "kT" if kv == 0 else "vcT"
                        for blk in range(2):
                            for j in range(32):
                                rhs = bass.AP(srcT.tensor, srcT.offset + j, [[srcT.ap[0][0], 128], [16, 127]])
                                Sc.add("pe", lambda h, kv=kv, blk=blk, j=j, rhs=rhs: h.matmul(MISC[:, 0:127], lhsT=w1[kv][:, j, blk * 128:(blk + 1) * 128], rhs=rhs, start=(j == 0), stop=False),
                                       reads=["arena%d" % kv, skey], writes=["MISC"])
                                Sc.add("pe", lambda h, kv=kv, blk=blk, j=j: h.matmul(MISC[:, 0:127], lhsT=w1[kv][:, j, blk * 128:(blk + 1) * 128], rhs=posT[kv][:, j:j + 1].to_broadcast([128, 127]), start=False, stop=(j == 31)),
                                       reads=["arena%d" % kv, "posT%d" % kv], writes=["MISC"])
                            Sc.add("act", lambda h: h.activation(out=gsq[:, 0:127], in_=MISC[:, 0:127], func=AF.Square), reads=["MISC"], writes=["gsq"])
                            Sc.add("dve", lambda h: h.tensor_scalar(out=gsq[:, 0:127], in0=gsq[:, 0:127], scalar1=0.044715, scalar2=1.0, op0=ALU.mult, op1=ALU.add), reads=["gsq"], writes=["gsq"])
                            Sc.add("dve", lambda h: h.tensor_tensor(out=gu[:, 0:127], in0=gsq[:, 0:127], in1=MISC[:, 0:127], op=ALU.mult), reads=["gsq", "MISC"], writes=["gu"])
                            Sc.add("act", lambda h: h.activation(out=gu[:, 0:127], in_=gu[:, 0:127], func=AF.Tanh, scale=0.7978845608), reads=["gu"], writes=["gu"])
                            Sc.add("dve", lambda h: h.tensor_scalar(out=gu[:, 0:127], in0=gu[:, 0:127], scalar1=1.0, scalar2=0.5, op0=ALU.add, op1=ALU.mult), reads=["gu"], writes=["gu"])
                            Sc.add("dve", lambda h, kv=kv, blk=blk: h.tensor_tensor(out=hid[kv][:, blk, 0:127], in0=gu[:, 0:127], in1=MISC[:, 0:127], op=ALU.mult), reads=["gu", "MISC"], writes=["hid%d" % kv])
                    for blk in range(2):
                        Sc.add("pe", lambda h, blk=blk: h.matmul(MISC[:, 0:127], lhsT=w2[0][:, blk, :], rhs=hid[0][:, blk, 0:127], start=(blk == 0), stop=(blk == 1)), reads=["w2_0", "hid0"], writes=["MISC"])
                    Sc.add("dve", lambda h: h.tensor_copy(out=kcT[:, 0:127], in_=MISC[:, 0:127]), reads=["MISC"], writes=["kcT"])
                    for blk in range(2):
                        Sc.add("pe", lambda h, blk=blk: h.matmul(MISC[0:127, 0:128], lhsT=hid[1][:, blk, 0:127], rhs=w2[1][:, blk, :], start=(blk == 0), stop=(blk == 1)), reads=["w2_1", "hid1"], writes=["MISC"])
                    Sc.add("dve", lambda h: h.tensor_copy(out=vcx[0:127, 0:128], in_=MISC[0:127, 0:128]), reads=["MISC"], writes=["vcx"])

                    OAK = ["OA", "OA1"]; OBK = ["OB", "OB1"]
                    def emit_qk(job):
                        sidx = pcount[0] % 3
                        pcount[0] += 1
                        job["sidx"] = sidx
                        bank = SB[sidx]; bkey = ["B0", "B1", "MISC"][sidx]
                        i = job["i"]; npart = job["npart"]; lhsT = job["lhsT"]; extra = job["extra"]
                        qs = qT[:, :, i * 128:(i + 1) * 128]
                        n = len(extra)
                        Sc.add("pe", lambda h: h.matmul(bank[0:npart, :], lhsT=lhsT, rhs=qs, start=True, stop=(n == 0)), reads=["qT"] + job["lkeys"], writes=[bkey])
                        for xi, (l2, r2, k2) in enumerate(extra):
                            Sc.add("pe", lambda h, l2=l2, r2=r2, xi=xi: h.matmul(bank[0:npart, :], lhsT=l2, rhs=r2, start=False, stop=(xi == n - 1)), reads=k2, writes=[bkey])
                        Sc.add("act", lambda h: h.activation(out=pT[sidx][0:npart, :], in_=bank[0:npart, :], func=AF.Exp, scale=SCALE), reads=[bkey], writes=["pT%d" % sidx])

                    def bc4(ap2, npart):
                        return ap2.unsqueeze(1).to_broadcast([npart, 4, 128])

                    def denfac(branch, Ov, okey, i, g):
                        for b in range(2):
                            Sc.add("dve", lambda h, b=b: h.tensor_scalar(out=den[:, branch, 2 * b:2 * b + 2], in0=Ov[b][:, :, 128], scalar1=1e-30, scalar2=None, op0=ALU.max), reads=okey, writes=["den%d" % branch])
                        Sc.add("dve", lambda h: h.reciprocal(out=den[:, branch, :], in_=den[:, branch, :]), reads=["den%d" % branch], writes=["den%d" % branch])
                        gc0 = branch * 16 + g * 4
                        Sc.add("dve", lambda h: h.tensor_tensor(out=fac[:, branch, :], in0=den[:, branch, :], in1=gates[:, i, gc0:gc0 + 4], op=ALU.mult), reads=["den%d" % branch, "gates"], writes=["fac%d" % branch])

                    def make_jobs(i, g):
                        jobs = []

                        def pv_cmp(sidx):
                            for r in range(4):
                                Sc.add("pe", lambda h, r=r: h.matmul(OBv[r // 2][:, r % 2, 0:161], lhsT=pT[sidx][0:127, r * 128:(r + 1) * 128], rhs=vcx[0:127, 0:161], start=True, stop=True),
                                       reads=["pT%d" % sidx, "vcx"], writes=OBK)

                        def post_cmp():
                            denfac(0, OBv, OBK, i, g)
                            if i >= 8:
                                for r in range(4):
                                    if r == 0:
                                        Sc.add("dve", lambda h: h.tensor_scalar(out=imp[:], in0=OBv[0][:, 0, 129:161], scalar1=den[:, 0, 0:1], scalar2=None, op0=ALU.mult), reads=OBK + ["den0"], writes=["imp"])
                                    else:
                                        Sc.add("dve", lambda h, r=r: h.scalar_tensor_tensor(out=imp[:], in0=OBv[r // 2][:, r % 2, 129:161], scalar=den[:, 0, r:r + 1], in1=imp[:], op0=ALU.mult, op1=ALU.add), reads=OBK + ["den0", "imp"], writes=["imp"])
                            for r in range(4):
                                Sc.add("dve", lambda h, r=r: h.tensor_scalar(out=oacc[:, r, :], in0=OBv[r // 2][:, r % 2, 0:128], scalar1=fac[:, 0, r:r + 1], scalar2=None, op0=ALU.mult), reads=OBK + ["fac0"], writes=["oacc"])
                            if i >= 8:
                                Sc.add("dve", lambda h: h.tensor_tensor(out=imp[:], in0=imp[:], in1=addm[:, i - 8, :], op=ALU.add), reads=["imp", "addm"], writes=["imp"])
                                Sc.add("dve", lambda h: h.max(out=m8[:, 0:8], in_=imp[:]), reads=["imp"], writes=["m8"])
                                Sc.add("dve", lambda h: h.match_replace(out=impw[:], in_to_replace=m8[:, 0:8], in_values=imp[:], imm_value=-3e38), reads=["imp", "m8"], writes=["impw"])
                                Sc.add("dve", lambda h: h.max(out=m8[:, 8:16], in_=impw[:]), reads=["impw"], writes=["m8"])
                                Sc.add("dve", lambda h: h.tensor_scalar(out=impw[:], in0=imp[:], scalar1=m8[:, 15:16], scalar2=None, op0=ALU.is_ge), reads=["imp", "m8"], writes=["impw"])
                                Sc.add("dve", lambda h: h.tensor_scalar(out=negsel[:], in0=impw[:], scalar1=-1.0, scalar2=-NEG, op0=ALU.add, op1=ALU.mult), reads=["impw"], writes=["negsel"])
                                Sc.add("pe", lambda h: h.transpose(out=TB[0:32, 4, :], in_=negsel[:, :], identity=identb[:]), reads=["negsel", "identb"], writes=["TB"])
                                Sc.add("dve", lambda h: h.tensor_copy(out=negselT[:, :], in_=TB[0:32, 4, :]), reads=["TB"], writes=["negselT"])

                        jobs.append(dict(i=i, lhsT=kcT[:, 0:127], lkeys=["kcT"], npart=127, pv=pv_cmp, post=post_cmp,
                                         extra=[(identb[0:127, 0:127], bc4(cmpmask[0:127, i, :], 127), ["identb", "cmpmask"])]))

                        j0 = max(0, i - 4)
                        for j in range(j0, i + 1):
                            extra = []
                            if j == i:
                                extra.append((identb[:, :], bc4(tri[:, 0, :], 128), ["identb", "tri"]))
                            if j == i - 4:
                                extra.append((identb[:, :], bc4(tri[:, 1, :], 128), ["identb", "tri"]))

                            def pv_win(sidx, j=j):
                                for r in range(4):
                                    Sc.add("pe", lambda h, r=r: h.matmul(OAv[r // 2][:, r % 2, 0:129], lhsT=pT[sidx][:, r * 128:(r + 1) * 128], rhs=vwin[:, j, 0:129], start=(j == j0 and r % 2 == 0), stop=(j == i), skip_group_check=True),
                                           reads=["pT%d" % sidx, "vwin"], writes=OAK)

                            jobs.append(dict(i=i, lhsT=kT[:, 2, j * 128:(j + 1) * 128], lkeys=["kT"], npart=128, pv=pv_win, post=None, extra=extra))

                        def post_combine():
                            denfac(2, OAv, OAK, i, g)
                            denfac(1, OBv, OBK, i, g)
                            for r in range(4):
                                Sc.add("dve", lambda h, r=r: h.scalar_tensor_tensor(out=oacc[:, r, :], in0=OAv[r // 2][:, r % 2, 0:128], scalar=fac[:, 2, r:r + 1], in1=oacc[:, r, :], op0=ALU.mult, op1=ALU.add), reads=OAK + ["fac2", "oacc"], writes=["oacc"])
                            for r in range(4):
                                Sc.add("dve", lambda h, r=r: h.scalar_tensor_tensor(out=otok[:, r, :], in0=OBv[r // 2][:, r % 2, 0:128], scalar=fac[:, 1, r:r + 1], in1=oacc[:, r, :], op0=ALU.mult, op1=ALU.add), reads=OBK + ["fac1", "oacc"], writes=["otok"])
                            for r in range(4):
                                Sc.add("pe", lambda h, r=r: h.transpose(out=TB[:, r, :], in_=otok[:, r, :], identity=identb[:]), reads=["otok", "identb"], writes=["TB"])
                            bg(1)
                            ob = (i // 4) % 2
                            Sc.add("act", lambda h: h.activation(out=oTb[ob][:, :, (i % 4) * 128:(i % 4 + 1) * 128], in_=TB[:, 0:4, :], func=AF.Copy), reads=["TB"], writes=["oTb%d" % ob])
                            if i % 4 == 3:
                                t0 = (i // 4) * 512
                                Sc.dma("sp", lambda h: h.dma_start(out=OT.rearrange("(hh d) t -> d hh t", d=128)[:, g * 4:(g + 1) * 4, t0:t0 + 512], in_=oTb[ob][:]), "OTw%d" % ob, reads=["oTb%d" % ob], writes=["OT"])

                        for j in range(0, i + 1):
                            extra = []
                            if j == i:
                                extra.append((identb[:, :], bc4(tri[:, 0, :], 128), ["identb", "tri"]))
                            if i >= 8:
                                extra.append((esel[0:32, j, :], bc4(negselT[0:32, :], 32), ["esel", "negselT"]))

                            def pv_slc(sidx, j=j):
                                for r in range(4):
                                    Sc.add("pe", lambda h, r=r: h.matmul(OBv[r // 2][:, r % 2, 0:129], lhsT=pT[sidx][:, r * 128:(r + 1) * 128], rhs=vslc[:, j, 0:129], start=(j == 0 and r % 2 == 0), stop=(j == i), skip_group_check=True),
                                           reads=["pT%d" % sidx, "vslc"], writes=OBK)

                            jobs.append(dict(i=i, lhsT=kT[:, 1, j * 128:(j + 1) * 128], lkeys=["kT"], npart=128, pv=pv_slc, post=(post_combine if j == i else None), extra=extra))
                        return jobs

                    jobs = []
                    for i in range(NT):
                        jobs += make_jobs(i, g)
                    LOOK = 2
                    for idx in range(min(LOOK, len(jobs))):
                        emit_qk(jobs[idx])
                    for idx, job in enumerate(jobs):
                        job["pv"](job["sidx"])
                        if job["post"] is not None:
                            job["post"]()
                        if idx + LOOK < len(jobs):
                            emit_qk(jobs[idx + LOOK])
                Sc.flush()
        if stop_after == "P3":
            Sc.finish(); return nc

        dest_i = sb(top, "dest_i", [128, NT, 2], I32)
        gate2 = sb(top, "gate2", [128, NT, 2], F32)
        with ExitStack() as st:
            B0 = ps(st, "B0_4", [128, 512], F32); B1 = ps(st, "B1_4", [128, 512], F32); MISC = ps(st, "MISC_4", [128, 512], F32)
            OA0 = ps(st, "OA0_4", [128, 512], F32); OB0 = ps(st, "OB0_4", [128, 512], F32)
            TB = ps(st, "TB_4", [128, 8, 128], BF16); TB2 = ps(st, "TB2_4", [128, 8, 128], BF16)
            TBS = [TB, TB2]
            bmT = sb(st, "bmT", [128, 32], F32)
            load_T(st, "bmT", b_merge[:, :], 32, bmT[:, :], MISC)
            fng = sb(st, "fng", [128, D], F32)
            Sc.dma("sp", lambda h: h.dma_start(out=fng[:], in_=ffn_norm[0:1, :].broadcast_to([128, D])), "fng", writes=["fng"])
            bgr = sb(st, "bgr", [128, 72], F32)
            Sc.dma("sp", lambda h: h.dma_start(out=bgr[:], in_=b_gr[0:1, :].broadcast_to([128, 72])), "bgr", writes=["bgr"])
            wgr = sb(st, "wgr", [128, 16, 72], BF16)
            Sc.dma("pool", lambda h: h.dma_start(out=wgr[:], in_=w_gr.rearrange("(k p) n -> p k n", p=128)), "wgr", writes=["wgr"])
            upper = sb(st, "upper", [128, 128], BF16)
            Sc.dma("pool", lambda h: h.dma_start(out=upper[:], in_=upper_d[:, :]), "upper", writes=["upper"])
            eoff = sb(st, "eoff", [128, 64], F32)
            Sc.dma("sp", lambda h: h.dma_start(out=eoff[:], in_=eoff_d[:, :]), "eoff", writes=["eoff"])
            base = sb(st, "base", [128, 64], F32)
            Sc.add("pool", lambda h: h.memset(base[:], 0.0), writes=["base"])

            HALF = 1024
            mT = sb(st, "mT", [128, 16, HALF], BF16)
            lg = sb(st, "lg", [128, 72], F32)
            r8 = sb(st, "r8", [128, 8], F32); r1 = sb(st, "r1", [128, 8], F32)
            oh = [sb(st, "oh%d" % i, [128, 64], F32) for i in range(2)]
            msk = sb(st, "msk", [128, 64], F32); Ab = sb(st, "Ab", [128, 64], BF16)
            cnt = sb(st, "cnt", [128, 64], F32); tmp64 = sb(st, "tmp64", [128, 64], F32)
            dst_f = sb(st, "dst_f", [128, 4], F32)
            ss = sb(st, "ss4", [128, 1], F32)
            w_merge_v = w_merge.rearrange("(k p) n -> p k n", p=128)
            w_pc_v = w_proj_conv.rearrange("(k p) n -> p k n", p=128)
            w_pn_v = w_proj_nsa.rearrange("(k p) n -> p k n", p=128)
            w_out_v = w_out.rearrange("(k p) n -> p k n", p=128)
            for hf in range(2):
              h0 = hf * HALF
              with ExitStack() as sa:
                hTh = sb(sa, "hTh%d" % hf, [128, 16, HALF], BF16)
                uTh = sb(sa, "uTh%d" % hf, [128, 8, HALF], BF16)
                oTh = sb(sa, "oTh%d" % hf, [128, 16, HALF], BF16)
                wm = [sb(sa, "wm%d_%d" % (hf, i), [128, 16, 2, 256], BF16) for i in range(2)]
                wpc = [sb(sa, "wpc%d_%d" % (hf, i), [128, 8, 256], BF16) for i in range(2)]
                wpn = [sb(sa, "wpn%d_%d" % (hf, i), [128, 16, 256], BF16) for i in range(2)]
                gcs = sb(sa, "gcs%d" % hf, [128, 512], F32); gns = sb(sa, "gns%d" % hf, [128, 512], F32); tmpm = sb(sa, "tmpm%d" % hf, [128, 512], F32)
                for kc in range(16):
                    Sc.dma("sp", lambda h, kc=kc, h0=h0: h.dma_start(out=hTh[:, kc, :], in_=HT[kc * 128:(kc + 1) * 128, h0:h0 + HALF]), "hTh%d" % kc, reads=["HT"], writes=["hTh%d" % kc])
                    Sc.dma("sp", lambda h, kc=kc, h0=h0: h.dma_start(out=oTh[:, kc, :], in_=OT[kc * 128:(kc + 1) * 128, h0:h0 + HALF]), "oTh%d" % kc, reads=["OT"], writes=["oTh%d" % kc])
                for kc in range(8):
                    Sc.dma("sp", lambda h, kc=kc, h0=h0: h.dma_start(out=uTh[:, kc, :], in_=UT[kc * 128:(kc + 1) * 128, h0:h0 + HALF]), "uTh%d" % kc, reads=["UT"], writes=["uTh%d" % kc])

                def load_w(d8):
                    wbi = d8 % 2
                    c0 = d8 * 256
                    Sc.dma("pool", lambda h: h.dma_start(out=wm[wbi][:, :, 0, :], in_=w_merge_v[:, :, c0:c0 + 256]), "wm%d" % wbi, writes=["wm%d" % wbi])
                    Sc.dma("pool", lambda h: h.dma_start(out=wm[wbi][:, :, 1, :], in_=w_merge_v[:, :, D + c0:D + c0 + 256]), "wm%d" % wbi, writes=["wm%d" % wbi])
                    Sc.dma("pool", lambda h: h.dma_start(out=wpc[wbi][:], in_=w_pc_v[:, :, c0:c0 + 256]), "wpc%d" % wbi, writes=["wpc%d" % wbi])
                    Sc.dma("pool", lambda h: h.dma_start(out=wpn[wbi][:], in_=w_pn_v[:, :, c0:c0 + 256]), "wpn%d" % wbi, writes=["wpn%d" % wbi])

                def stage_a(d8, db, t2):
                    wbi = d8 % 2
                    dblk = d8 * 2 + db
                    csl = slice(db * 128, (db + 1) * 128)
                    tsl = slice(t2 * 512, (t2 + 1) * 512)
                    kwm = "wm%d" % wbi; kpc = "wpc%d" % wbi; kpn = "wpn%d" % wbi
                    for k in range(16):
                        Sc.add("pe", lambda h, k=k: h.matmul(OA0[:, :], lhsT=wm[wbi][:, k, 0, csl], rhs=hTh[:, k, tsl], start=(k == 0), stop=(k == 15)), reads=[kwm, "hTh%d" % k], writes=["OA"])
                    for k in range(16):
                        Sc.add("pe", lambda h, k=k: h.matmul(OB0[:, :], lhsT=wm[wbi][:, k, 1, csl], rhs=hTh[:, k, tsl], start=(k == 0), stop=(k == 15)), reads=[kwm, "hTh%d" % k], writes=["OB"])
                    for k in range(8):
                        Sc.add("pe", lambda h, k=k: h.matmul(B0[:, :], lhsT=wpc[wbi][:, k, csl], rhs=uTh[:, k, tsl], start=(k == 0), stop=(k == 7)), reads=[kpc, "uTh%d" % k], writes=["B0"])
                    for k in range(16):
                        Sc.add("pe", lambda h, k=k: h.matmul(B1[:, :], lhsT=wpn[wbi][:, k, csl], rhs=oTh[:, k, tsl], start=(k == 0), stop=(k == 15)), reads=[kpn, "oTh%d" % k], writes=["B1"])
                    Sc.add("act", lambda h: h.activation(out=gcs[:], in_=OA0[:, :], func=AF.Sigmoid, bias=bmT[:, dblk:dblk + 1]), reads=["OA", "bmT"], writes=["gcs"])
                    Sc.add("act", lambda h: h.activation(out=gns[:], in_=OB0[:, :], func=AF.Sigmoid, bias=bmT[:, 16 + dblk:17 + dblk]), reads=["OB", "bmT"], writes=["gns"])
                    Sc.add("dve", lambda h: h.tensor_tensor(out=tmpm[:], in0=gcs[:], in1=B0[:, :], op=ALU.mult), reads=["gcs", "B0"], writes=["tmpm"])
                    Sc.add("dve", lambda h: h.tensor_tensor(out=gns[:], in0=gns[:], in1=B1[:, :], op=ALU.mult), reads=["gns", "B1"], writes=["gns"])
                    Sc.add("dve", lambda h: h.tensor_tensor(out=mT[:, dblk, tsl], in0=tmpm[:], in1=gns[:], op=ALU.add), reads=["tmpm", "gns"], writes=["mT"])

                load_w(0)
                for d8 in range(8):
                    if d8 + 1 < 8:
                        load_w(d8 + 1)
                    bg(1)
                    for db in range(2):
                        for t2 in range(2):
                            stage_a(d8, db, t2)
                Sc.flush()
              with ExitStack() as sk:
                wo4 = sb(sk, "wo4_%d" % hf, [128, 4, 16, 512], BF16)
                xin2 = [sb(sk, "xin%d_%d" % (hf, i), [128, D], F32) for i in range(2)]
                x1t2 = [sb(sk, "x1t%d_%d" % (hf, i), [128, D], F32) for i in range(2)]
                xnb = [sb(sk, "xnb%d_%d" % (i, hf), [128, D], BF16) for i in range(2)]
                junk = sb(sk, "junk4_%d" % hf, [128, D], BF16)
                xnT = sb(sk, "xnT%d" % hf, [128, 16, 128], BF16)
                for d4 in range(4):
                    Sc.dma("pool", lambda h, d4=d4: h.dma_start(out=wo4[:, d4, :, :], in_=w_out_v[:, :, d4 * 512:(d4 + 1) * 512]), "wo4_%d" % d4, writes=["wo4_%d" % d4])
                def sb_mm(t8):
                    tt = hf * 8 + t8
                    tsl = slice(t8 * 128, (t8 + 1) * 128)
                    xin = xin2[t8 % 2]; x1t = x1t2[t8 % 2]; kxin = "xin%d" % (t8 % 2); kx1t = "x1t%d" % (t8 % 2)
                    Sc.dma("sp", lambda h, tt=tt, xin=xin: h.dma_start(out=xin[:], in_=x[tt * 128:(tt + 1) * 128, :]), kxin, writes=[kxin])
                    for d4 in range(4):
                        dsl = slice(d4 * 512, (d4 + 1) * 512)
                        bank = [B0, B1, OA0, OB0][d4]; bkey = ["B0", "B1", "OA", "OB"][d4]
                        for k in range(16):
                            Sc.add("pe", lambda h, k=k, tsl=tsl, bank=bank, d4=d4: h.matmul(bank[:, :], lhsT=mT[:, k, tsl], rhs=wo4[:, d4, k, :], start=(k == 0), stop=(k == 15)), reads=["mT", "wo4_%d" % d4], writes=[bkey])
                        Sc.add("dve", lambda h, dsl=dsl, bank=bank, x1t=x1t, xin=xin: h.tensor_tensor(out=x1t[:, dsl], in0=bank[:, :], in1=xin[:, dsl], op=ALU.add), reads=[bkey, kxin], writes=[kx1t])

                def sb_post(t8):
                    tt = hf * 8 + t8
                    tsl = slice(t8 * 128, (t8 + 1) * 128)
                    xin = xin2[t8 % 2]; x1t = x1t2[t8 % 2]; kxin = "xin%d" % (t8 % 2); kx1t = "x1t%d" % (t8 % 2)
                    Sc.dma("sp", lambda h, tt=tt, x1t=x1t: h.dma_start(out=X1[tt * 128:(tt + 1) * 128, :], in_=x1t[:]), "X1w%d" % (t8 % 2), reads=[kx1t], writes=["X1"])
                    s = tt % 2
                    Sc.add("act", lambda h, x1t=x1t: h.activation(out=junk[:], in_=x1t[:], func=AF.Square, accum_out=ss[:]), reads=[kx1t], writes=["junk4", "ss4"])
                    Sc.add("act", lambda h: h.activation(out=ss[:], in_=ss[:], func=AF.Sqrt, scale=1.0 / D, bias=EPS), reads=["ss4"], writes=["ss4"])
                    Sc.add("dve", lambda h: h.reciprocal(out=ss[:], in_=ss[:]), reads=["ss4"], writes=["ss4"])
                    Sc.add("dve", lambda h, s=s, x1t=x1t: h.scalar_tensor_tensor(out=xnb[s][:], in0=x1t[:], scalar=ss[:, 0:1], in1=fng[:], op0=ALU.mult, op1=ALU.mult), reads=[kx1t, "ss4", "fng"], writes=["xnb%d" % s])
                    for c4 in range(4):
                        for j in range(4):
                            c = 4 * c4 + j
                            Sc.add("pe", lambda h, s=s, c=c, j=j, c4=c4: h.transpose(out=TBS[c4 % 2][:, j, :], in_=xnb[s][:, c * 128:(c + 1) * 128], identity=identb[:]), reads=["xnb%d" % s, "identb"], writes=[["TB", "TB2"][c4 % 2]])
                        Sc.add("act", lambda h, c4=c4: h.activation(out=xnT[:, 4 * c4:4 * c4 + 4, :], in_=TBS[c4 % 2][:, 0:4, :], func=AF.Copy), reads=[["TB", "TB2"][c4 % 2]], writes=["xnT"])
                    for k in range(16):
                        Sc.add("pe", lambda h, k=k: h.matmul(MISC[:, 0:72], lhsT=xnT[:, k, :], rhs=wgr[:, k, :], start=(k == 0), stop=(k == 15)), reads=["xnT", "wgr"], writes=["MISC"])
                    Sc.add("dve", lambda h: h.tensor_tensor(out=lg[:], in0=MISC[:, 0:72], in1=bgr[:], op=ALU.add), reads=["MISC", "bgr"], writes=["lg"])
                    Sc.add("dve", lambda h: h.max(out=r8[:], in_=lg[:, 0:8]), reads=["lg"], writes=["r8"])
                    Sc.add("dve", lambda h: h.tensor_scalar(out=r1[:, 0:1], in0=r8[:, 0:1], scalar1=-1.0, scalar2=None, op0=ALU.mult), reads=["r8"], writes=["r1a"])
                    Sc.add("act", lambda h: h.activation(out=r1[:, 0:8], in_=lg[:, 0:8], func=AF.Exp, bias=r1[:, 0:1], accum_out=r1[:, 1:2]) if False else h.activation(out=tmp64[:, 0:8], in_=lg[:, 0:8], func=AF.Exp, bias=r1[:, 0:1], accum_out=r1[:, 1:2]),
                           reads=["lg", "r1a"], writes=["tmp64", "r1b"])
                    Sc.add("dve", lambda h: h.reciprocal(out=r1[:, 2:3], in_=r1[:, 1:2]), reads=["r1b"], writes=["r1c"])
                    Sc.add("dve", lambda h: h.tensor_scalar(out=tmp64[:, 8:16], in0=lg[:, 0:8], scalar1=r8[:, 0:1], scalar2=None, op0=ALU.is_ge), reads=["lg", "r8", "tmp64"], writes=["tmp64"])
                    Sc.add("dve", lambda h: h.tensor_scalar(out=tmp64[:, 8:16], in0=tmp64[:, 8:16], scalar1=-1.0, scalar2=1e9, op0=ALU.add, op1=ALU.mult), reads=["tmp64"], writes=["tmp64"])
                    Sc.add("dve", lambda h: h.tensor_tensor(out=msk[:, :].rearrange("p (g e) -> p g e", e=8), in0=lg[:, 8:72].rearrange("p (g e) -> p g e", e=8),
                                                            in1=tmp64[:, 8:16].unsqueeze(2).to_broadcast([128, 8, 8]), op=ALU.add), reads=["lg", "tmp64"], writes=["msk"])
                    Sc.add("dve", lambda h: h.max(out=r8[:], in_=msk[:]), reads=["msk", "r8"], writes=["r8"])
                    Sc.add("dve", lambda h: h.tensor_scalar(out=oh[0][:], in0=msk[:], scalar1=r8[:, 0:1], scalar2=None, op0=ALU.is_equal), reads=["msk", "r8"], writes=["oh0"])
                    Sc.add("dve", lambda h: h.tensor_scalar(out=oh[1][:], in0=msk[:], scalar1=r8[:, 1:2], scalar2=None, op0=ALU.is_equal), reads=["msk", "r8"], writes=["oh1"])
                    Sc.add("dve", lambda h: h.tensor_tensor(out=r1[:, 3:4], in0=r8[:, 0:1], in1=r8[:, 1:2], op=ALU.subtract), reads=["r8"], writes=["r1d"])
                    Sc.add("act", lambda h: h.activation(out=r1[:, 4:5], in_=r1[:, 3:4], func=AF.Sigmoid), reads=["r1d"], writes=["r1e"])
                    Sc.add("dve", lambda h, tt=tt: h.tensor_tensor(out=gate2[:, tt, 0:1], in0=r1[:, 4:5], in1=r1[:, 2:3], op=ALU.mult), reads=["r1e", "r1c"], writes=["gate2"])
                    Sc.add("dve", lambda h, tt=tt: h.tensor_tensor(out=gate2[:, tt, 1:2], in0=r1[:, 2:3], in1=gate2[:, tt, 0:1], op=ALU.subtract), reads=["r1c", "gate2"], writes=["gate2"])
                    Sc.add("dve", lambda h: h.tensor_tensor(out=Ab[:], in0=oh[0][:], in1=oh[1][:], op=ALU.add), reads=["oh0", "oh1"], writes=["Ab"])
                    Sc.add("pe", lambda h: h.matmul(MISC[:, 128:192], lhsT=upper[:, :], rhs=Ab[:], start=True, stop=True), reads=["upper", "Ab"], writes=["MISC"])
                    Sc.add("pe", lambda h: h.matmul(MISC[:, 192:256], lhsT=onesb[:, :], rhs=Ab[:], start=True, stop=True), reads=["onesb", "Ab"], writes=["MISC"])
                    Sc.add("dve", lambda h: h.tensor_tensor(out=cnt[:], in0=MISC[:, 128:192], in1=base[:], op=ALU.add), reads=["MISC", "base"], writes=["cnt"])
                    Sc.add("dve", lambda h: h.tensor_tensor(out=base[:], in0=MISC[:, 192:256], in1=base[:], op=ALU.add), reads=["MISC", "base"], writes=["base"])
                    for kk in range(2):
                        Sc.add("dve", lambda h, kk=kk: h.tensor_tensor(out=tmp64[:], in0=oh[kk][:], in1=cnt[:], op=ALU.mult), reads=["oh%d" % kk, "cnt"], writes=["tmp64"])
                        Sc.add("dve", lambda h, kk=kk: h.reduce_sum(out=dst_f[:, kk:kk + 1], in_=tmp64[:], axis=mybir.AxisListType.X), reads=["tmp64"], writes=["dst_f"])
                        Sc.add("dve", lambda h, kk=kk: h.tensor_tensor(out=tmp64[:], in0=oh[kk][:], in1=eoff[:], op=ALU.mult), reads=["oh%d" % kk, "eoff"], writes=["tmp64"])
                        Sc.add("dve", lambda h, kk=kk: h.reduce_sum(out=dst_f[:, 2 + kk:3 + kk], in_=tmp64[:], axis=mybir.AxisListType.X), reads=["tmp64"], writes=["dst_f"])
                        Sc.add("dve", lambda h, kk=kk: h.tensor_scalar(out=tmp64[:, 0:1], in0=dst_f[:, kk:kk + 1], scalar1=float(CAP), scalar2=1e6, op0=ALU.is_ge, op1=ALU.mult), reads=["dst_f"], writes=["tmp64"])
                        Sc.add("dve", lambda h, kk=kk: h.tensor_tensor(out=dst_f[:, kk:kk + 1], in0=dst_f[:, kk:kk + 1], in1=dst_f[:, 2 + kk:3 + kk], op=ALU.add), reads=["dst_f"], writes=["dst_f"])
                        Sc.add("dve", lambda h, kk=kk: h.tensor_tensor(out=dst_f[:, kk:kk + 1], in0=dst_f[:, kk:kk + 1], in1=tmp64[:, 0:1], op=ALU.add), reads=["dst_f", "tmp64"], writes=["dst_f"])
                    Sc.add("dve", lambda h, tt=tt: h.tensor_copy(out=dest_i[:, tt, :], in_=dst_f[:, 0:2]), reads=["dst_f"], writes=["dest_i"])
                    for kk in range(2):
                        Sc.dma("pool", lambda h, tt=tt, kk=kk, s=s: h.indirect_dma_start(out=XS[:, :], out_offset=bass.IndirectOffsetOnAxis(ap=dest_i[:, tt, kk:kk + 1], axis=0), in_=xnb[s][:, :], in_offset=None,
                                                                                  bounds_check=Sc.bound_reg(h, NSLOT - 1), oob_is_err=False), "xs_sc%d" % s, reads=["xnb%d" % s, "dest_i"], writes=["XS"])
                    if dbg:
                        Sc.add("dve", lambda h, tt=tt: h.tensor_copy(out=dst_f[:, 2:4], in_=gate2[:, tt, :]), reads=["gate2", "dst_f"], writes=["dst_f"])
                        Sc.dma("sp", lambda h, tt=tt: h.dma_start(out=RT[tt * 128:(tt + 1) * 128, :], in_=dst_f[:]), "RTw", reads=["dst_f"], writes=["RT"])

                sb_mm(0)
                for t8 in range(8):
                    if t8 + 1 < 8:
                        sb_mm(t8 + 1)
                    sb_post(t8)
                    if t8 % 2 == 1:
                        bg(1)
                if hf == 1:
                    bg(len(bg_list))
                Sc.flush()
        if stop_after == "P4":
            Sc.finish(); return nc

        with ExitStack() as st:
            B0 = ps(st, "B0_5", [128, 512], F32); B1 = ps(st, "B1_5", [128, 512], F32)
            OA0 = ps(st, "OA0_5", [128, 512], F32); OA1 = ps(st, "OA1_5", [128, 512], F32)
            OB0 = ps(st, "OB0_5", [128, 512], F32); OB1 = ps(st, "OB1_5", [128, 512], F32)
            TB = ps(st, "TB_5", [128, 8, 128], BF16); TB2 = ps(st, "TB2_5", [128, 8, 128], BF16)
            TBS = [TB, TB2]
            ew1 = [sb(st, "ew1_%d" % i, [128, 16, FF], BF16) for i in range(2)]
            ew3 = [sb(st, "ew3_%d" % i, [128, 16, FF], BF16) for i in range(2)]
            ew2 = [sb(st, "ew2_%d" % i, [128, 4, D], BF16) for i in range(2)]
            xe = [sb(st, "xe%d" % i, [128, 2, D], BF16) for i in range(2)]
            xeT = sb(st, "xeT", [128, 16, 256], BF16)
            sg = sb(st, "sg", [128, 4, 256], F32)
            hTe = sb(st, "hTe", [128, 4, 256], BF16)
            ye = [sb(st, "ye%d" % i, [128, D], BF16) for i in range(4)]
            Av = [B0[:, :].rearrange("p (a b) -> p a b", b=256), B1[:, :].rearrange("p (a b) -> p a b", b=256)]
            Bv = [OA0[:, :].rearrange("p (a b) -> p a b", b=256), OA1[:, :].rearrange("p (a b) -> p a b", b=256)]
            akey = ["B0", "B1"]; bkeys = ["OA", "OA1"]
            ycount = [0]
            xeT2 = [xeT, sb(st, "xeTb", [128, 16, 256], BF16)]

            def e_ld13(e):
                s = e % 2
                if e in pre_idx:
                    j = pre_idx[e]
                    Sc.dma("pool", lambda h: h.dma_start(out=ew1[s][:], in_=EB1[j].rearrange("p (k n) -> p k n", n=FF)), "ew1_%d" % s, reads=["EB%d_0" % e], writes=["ew1_%d" % s])
                    Sc.dma("pool", lambda h: h.dma_start(out=ew3[s][:], in_=EB3[j].rearrange("p (k n) -> p k n", n=FF)), "ew3_%d" % s, reads=["EB%d_1" % e], writes=["ew3_%d" % s])
                else:
                    Sc.dma("pool", lambda h: h.dma_start(out=ew1[s][:], in_=exp_w1[e].rearrange("(k p) n -> p k n", p=128)), "ew1_%d" % s, writes=["ew1_%d" % s])
                    Sc.dma("pool", lambda h: h.dma_start(out=ew3[s][:], in_=exp_w3[e].rearrange("(k p) n -> p k n", p=128)), "ew3_%d" % s, writes=["ew3_%d" % s])

            def e_ld2(e):
                s = e % 2
                if e in pre_idx:
                    j = pre_idx[e]
                    Sc.dma("pool", lambda h: h.dma_start(out=ew2[s][:], in_=EB2[j].rearrange("p (k n) -> p k n", n=D)), "ew2_%d" % s, reads=["EB%d_2" % e], writes=["ew2_%d" % s])
                else:
                    Sc.dma("pool", lambda h: h.dma_start(out=ew2[s][:], in_=exp_w2[e].rearrange("(k p) n -> p k n", p=128)), "ew2_%d" % s, writes=["ew2_%d" % s])

            def e_ldx(e):
                s = e % 2
                Sc.dma("sp", lambda h: h.dma_start(out=xe[s][:, 0, :], in_=XS[e * CAP:e * CAP + 128, :]), "xe%da" % s, reads=["XS"], writes=["xe%da" % s])
                Sc.dma("sp", lambda h: h.dma_start(out=xe[s][0:R2, 1, :], in_=XS[e * CAP + 128:(e + 1) * CAP, :]), "xe%db" % s, reads=["XS"], writes=["xe%db" % s])

            def e_tr(e):
                s = e % 2
                xt = xeT2[s]; kx = "xeT%d" % s
                tcount = 0
                for a2 in range(2):
                    for c4 in range(4):
                        tb = tcount % 2; tcount += 1
                        for j in range(4):
                            c = 4 * c4 + j
                            nr = 128 if a2 == 0 else R2
                            Sc.add("pe", lambda h, c=c, j=j, tb=tb, a2=a2, nr=nr: h.transpose(out=TBS[tb][:, j, 0:nr], in_=xe[s][0:nr, a2, c * 128:(c + 1) * 128], identity=identb[0:nr, 0:nr]), reads=["xe%d%s" % (s, "ab"[a2]), "identb"], writes=[["TB", "TB2"][tb]])
                        nr = 128 if a2 == 0 else R2
                        if tb == 0:
                            Sc.add("act", lambda h, c4=c4, a2=a2, nr=nr: h.activation(out=xt[:, 4 * c4:4 * c4 + 4, a2 * 128:a2 * 128 + nr], in_=TB[:, 0:4, 0:nr], func=AF.Copy), reads=["TB"], writes=[kx])
                        else:
                            Sc.add("dve", lambda h, c4=c4, a2=a2, nr=nr: h.tensor_copy(out=xt[:, 4 * c4:4 * c4 + 4, a2 * 128:a2 * 128 + nr], in_=TB2[:, 0:4, 0:nr]), reads=["TB2"], writes=[kx])

            def e_s1(e):
                s = e % 2
                xt = xeT2[s]; kx = "xeT%d" % s
                for fb in range(4):
                    for k in range(16):
                        Sc.add("pe", lambda h, fb=fb, k=k: h.matmul(Av[fb // 2][:, fb % 2, 0:CAP], lhsT=ew1[s][:, k, fb * 128:(fb + 1) * 128], rhs=xt[:, k, 0:CAP], start=(k == 0), stop=(k == 15)), reads=["ew1_%d" % s, kx], writes=[akey[fb // 2]])
                for fb in range(4):
                    for k in range(16):
                        Sc.add("pe", lambda h, fb=fb, k=k: h.matmul(Bv[fb // 2][:, fb % 2, 0:CAP], lhsT=ew3[s][:, k, fb * 128:(fb + 1) * 128], rhs=xt[:, k, 0:CAP], start=(k == 0), stop=(k == 15)), reads=["ew3_%d" % s, kx], writes=[bkeys[fb // 2]])
                for hb in range(2):
                    Sc.add("act", lambda h, hb=hb: h.activation(out=sg[:, 2 * hb:2 * hb + 2, 0:CAP], in_=Av[hb][:, :, 0:CAP], func=AF.Silu), reads=[akey[hb]], writes=["sg%d" % hb])
                    Sc.add("dve", lambda h, hb=hb: h.tensor_tensor(out=hTe[:, 2 * hb:2 * hb + 2, 0:CAP], in0=sg[:, 2 * hb:2 * hb + 2, 0:CAP], in1=Bv[hb][:, :, 0:CAP], op=ALU.mult), reads=["sg%d" % hb, bkeys[hb]], writes=["hTe"])

            def e_s2(e):
                s = e % 2
                for a2 in range(2):
                    yb = ye[(e % 2) * 2 + a2]; ykey = "ye%d" % ((e % 2) * 2 + a2)
                    nr = 128 if a2 == 0 else R2
                    for d4 in range(4):
                        bank = [OB0, OB1][ycount[0] % 2]; bkey = ["OB", "OB1"][ycount[0] % 2]; ycount[0] += 1
                        for fb in range(4):
                            Sc.add("pe", lambda h, fb=fb, d4=d4, bank=bank, a2=a2, nr=nr: h.matmul(bank[0:nr, :], lhsT=hTe[:, fb, a2 * 128:a2 * 128 + nr], rhs=ew2[s][:, fb, d4 * 512:(d4 + 1) * 512], start=(fb == 0), stop=(fb == 3)), reads=["hTe", "ew2_%d" % s], writes=[bkey])
                        if d4 % 2 == 0:
                            Sc.add("act", lambda h, d4=d4, bank=bank, yb=yb, nr=nr: h.activation(out=yb[0:nr, d4 * 512:(d4 + 1) * 512], in_=bank[0:nr, :], func=AF.Copy), reads=[bkey], writes=[ykey])
                        else:
                            Sc.add("dve", lambda h, d4=d4, bank=bank, yb=yb, nr=nr: h.tensor_copy(out=yb[0:nr, d4 * 512:(d4 + 1) * 512], in_=bank[0:nr, :]), reads=[bkey], writes=[ykey])
                    Sc.dma("sp", lambda h, a2=a2, yb=yb, nr=nr: h.dma_start(out=YS[e * CAP + a2 * 128:e * CAP + a2 * 128 + nr, :], in_=yb[0:nr, :]), "w" + ykey, reads=[ykey], writes=["YS"])

            for e0 in range(min(2, nexp)):
                e_ld13(e0); e_ld2(e0); e_ldx(e0)
            e_tr(0)
            for e in range(nexp):
                e_s1(e)
                if e + 2 < nexp:
                    e_ld13(e + 2)
                if e + 1 < nexp:
                    e_tr(e + 1)
                if e + 2 < nexp:
                    e_ldx(e + 2)
                e_s2(e)
                if e + 2 < nexp:
                    e_ld2(e + 2)
            Sc.flush()
        if stop_after == "P5":
            Sc.finish(); return nc

        with ExitStack() as st:
            fin = sb(st, "fin", [128, D], F32)
            Sc.dma("sp", lambda h: h.dma_start(out=fin[:], in_=final_norm[0:1, :].broadcast_to([128, D])), "fin", writes=["fin"])
            x1b = [sb(st, "x1b%d" % i, [128, D], F32) for i in range(4)]
            y0 = [sb(st, "y0_%d" % i, [128, D], BF16) for i in range(4)]
            y1 = [sb(st, "y1_%d" % i, [128, D], BF16) for i in range(4)]
            junk = sb(st, "junk6", [128, D], BF16)
            ss = sb(st, "ss6", [128, 1], F32)
            for tt in range(NT):
                s = tt % 4
                Sc.dma("sp", lambda h, tt=tt, s=s: h.dma_start(out=x1b[s][:], in_=X1[tt * 128:(tt + 1) * 128, :]), "x1b%d" % s, reads=["X1"], writes=["x1b%d" % s])
                if tt < 4:
                    Sc.add("pool", lambda h, s=s: h.memset(y0[s][:], 0.0), writes=["y0_%d" % s])
                    Sc.add("pool", lambda h, s=s: h.memset(y1[s][:], 0.0), writes=["y1_%d" % s])
                for kk, yy in enumerate((y0, y1)):
                    Sc.dma("pool", lambda h, tt=tt, kk=kk, s=s, yy=yy: h.indirect_dma_start(out=yy[s][:, :], out_offset=None, in_=YS[:, :], in_offset=bass.IndirectOffsetOnAxis(ap=dest_i[:, tt, kk:kk + 1], axis=0),
                                                                                       bounds_check=Sc.bound_reg(h, NSLOT - 1), oob_is_err=False), "yg%d_%d" % (kk, s), reads=["YS", "dest_i"], writes=["y%d_%d" % (kk, s)])
                Sc.add("dve", lambda h, tt=tt, s=s: h.scalar_tensor_tensor(out=x1b[s][:], in0=y0[s][:], scalar=gate2[:, tt, 0:1], in1=x1b[s][:], op0=ALU.mult, op1=ALU.add), reads=["y0_%d" % s, "gate2", "x1b%d" % s], writes=["x1b%d" % s])
                Sc.add("dve", lambda h, tt=tt, s=s: h.scalar_tensor_tensor(out=x1b[s][:], in0=y1[s][:], scalar=gate2[:, tt, 1:2], in1=x1b[s][:], op0=ALU.mult, op1=ALU.add), reads=["y1_%d" % s, "gate2", "x1b%d" % s], writes=["x1b%d" % s])
                Sc.add("act", lambda h, s=s: h.activation(out=junk[:], in_=x1b[s][:], func=AF.Square, accum_out=ss[:]), reads=["x1b%d" % s], writes=["junk6", "ss6"])
                Sc.add("act", lambda h: h.activation(out=ss[:], in_=ss[:], func=AF.Sqrt, scale=1.0 / D, bias=EPS), reads=["ss6"], writes=["ss6"])
                Sc.add("dve", lambda h: h.reciprocal(out=ss[:], in_=ss[:]), reads=["ss6"], writes=["ss6"])
                Sc.add("dve", lambda h, s=s: h.scalar_tensor_tensor(out=x1b[s][:], in0=x1b[s][:], scalar=ss[:, 0:1], in1=fin[:], op0=ALU.mult, op1=ALU.mult), reads=["x1b%d" % s, "ss6", "fin"], writes=["x1b%d" % s])
                Sc.dma("sp", lambda h, tt=tt, s=s: h.dma_start(out=out[tt * 128:(tt + 1) * 128, :], in_=x1b[s][:]), "outw%d" % s, reads=["x1b%d" % s], writes=["out"])
            Sc.flush()
        Sc.finish()
    return nc


def make_in_maps(inputs, cores):
    c = _consts()
    g = lambda k: np.ascontiguousarray(np.asarray(inputs[k], dtype=np.float32))
    shared = {
        "attn_norm": g("attn_norm")[0].reshape(16, 128),
        "w_in": g("w_in")[0],
        "conv_dw": g("conv_dw")[0],
        "conv_dw_b": g("conv_dw_b")[0].reshape(8, 128),
        "conv_ln_g": g("conv_ln_g")[0].reshape(8, 128),
        "conv_ln_b": g("conv_ln_b")[0].reshape(8, 128),
        "cmp_pos_k": g("cmp_pos_k")[0], "cmp_pos_v": g("cmp_pos_v")[0],
        "cmp_k_w1": g("cmp_k_w1")[0], "cmp_v_w1": g("cmp_v_w1")[0],
        "cmp_k_w2": g("cmp_k_w2")[0], "cmp_v_w2": g("cmp_v_w2")[0],
        "w_proj_conv": g("w_proj_conv")[0], "w_proj_nsa": g("w_proj_nsa")[0],
        "w_merge": g("w_merge")[0], "b_merge": g("b_merge")[0].reshape(32, 128), "w_out": g("w_out")[0],
        "ffn_norm": g("ffn_norm")[0].reshape(1, D),
        "w_gr": np.ascontiguousarray(np.concatenate([g("w_grp")[0], g("w_exp")[0]], axis=1)),
        "b_gr": np.concatenate([g("b_grp")[0], g("b_exp")[0]]).reshape(1, 72),
        "exp_w1": g("exp_w1")[0], "exp_w3": g("exp_w3")[0], "exp_w2": g("exp_w2")[0],
        "final_norm": g("final_norm").reshape(1, D),
    }
    shared.update(c)
    xs = g("x")
    return [dict(shared, x=xs[b]) for b in cores]


def kernel(**inputs):
    nc = build()
    cores = list(range(8))
    in_maps = make_in_maps(inputs, cores)
    res = run_bass_kernel_spmd(nc, in_maps, core_ids=cores)
    return np.stack([res.results[i]["out"] for i in range(8)], axis=0).astype(np.float32)
```

```python
import numpy as np
import ml_dtypes
from contextlib import ExitStack
import concourse.bass as bass
import concourse.mybir as mybir
from concourse.bass_utils import run_bass_kernel_spmd

F32 = mybir.dt.float32
BF16 = mybir.dt.bfloat16
I32 = mybir.dt.int32
AF = mybir.ActivationFunctionType
ALU = mybir.AluOpType

D = 2048
S = 2048
NT = 16
CCH = 1024
TAPS = 31
NH = 16
NG = 4
INC = 7216
NEXP = 64
CAP = 192
R2 = CAP - 128
FF = 512
EPS = 1e-6
SCALE = 128 ** -0.5
NEG = -1.0e5
NSLOT = NEXP * CAP


class Op:
    __slots__ = ("eng", "fn", "deps", "signal", "val", "sem", "is_dma", "phase")


class Sched:
    ENG = ("pe", "act", "dve", "pool", "sp")
    BLK = {"pe": "tensor", "act": "scalar", "dve": "vector", "pool": "gpsimd", "sp": "sync"}
    PSUM_KEYS = {"B0", "B1", "OA", "OB", "OA1", "OB1", "TB", "TB2", "MISC"}

    def __init__(self, nc, stack):
        self.nc = nc
        self.stack = stack
        self.sems = {e: stack.enter_context(nc.semaphore("c_" + e)) for e in self.ENG}
        self.count = {e: 0 for e in self.ENG}
        self.ops = {e: [] for e in self.ENG}
        self.lastw = {}
        self.readers = {}
        self.dsems = {}
        self.dcount = {}
        self.waited = {e: {} for e in self.ENG}
        self.phase = 0
        self.phase_last = {}
        self.barrier = {e: [] for e in self.ENG}
        self.phase_map = {}

    def _mk(self, eng, fn, reads, writes):
        op = Op()
        op.eng = eng; op.fn = fn; op.signal = False; op.val = None; op.sem = None
        op.is_dma = False; op.phase = self.phase
        deps = []
        for k in reads:
            w = self.lastw.get(k)
            if w is not None:
                deps.append(w)
            if k in self.PSUM_KEYS:
                deps.extend(r for r in self.readers.get(k, []) if r.eng != eng)
            self.readers.setdefault(k, []).append(op)
        for k in writes:
            w = self.lastw.get(k)
            if w is not None:
                deps.append(w)
            deps.extend(self.readers.get(k, []))
            self.lastw[k] = op
            self.readers[k] = []
        deps.extend(self.barrier[eng])
        self.barrier[eng] = []
        out = []
        seen = set()
        for d in deps:
            if d is op:
                continue
            if (not d.is_dma) and d.phase < self.phase:
                d = self.phase_last[d.phase][d.eng]
            if id(d) in seen:
                continue
            seen.add(id(d))
            if (not d.is_dma) and d.eng == eng and eng == "pe":
                continue
            d.signal = True
            out.append(d)
        op.deps = out
        self.ops[eng].append(op)
        return op

    def add(self, eng, fn, reads=(), writes=()):
        return self._mk(eng, fn, reads, writes)

    def dma(self, eng, fn, sem, reads=(), writes=()):
        op = self._mk(eng, fn, reads, writes)
        op.is_dma = True
        pm = self.phase_map.setdefault(eng, {})
        sem = "g%s%d" % (eng, pm.setdefault(sem, len(pm)))
        if sem not in self.dsems:
            self.dsems[sem] = self.stack.enter_context(self.nc.semaphore("d_" + sem))
            self.dcount[sem] = 0
        self.dcount[sem] += 16
        op.sem = sem
        op.val = self.dcount[sem]
        return op

    def flush(self):
        nc = self.nc
        last = {}
        for e in self.ENG:
            comp = [o for o in self.ops[e] if not o.is_dma]
            if comp:
                comp[-1].signal = True
                last[e] = comp[-1]
        self.phase_last[self.phase] = last
        for e in self.ENG:
            c = self.count[e]
            for o in self.ops[e]:
                if not o.is_dma and o.signal:
                    c += 1
                    o.val = c
            self.count[e] = c
        ops_by_eng = {e: list(self.ops[e]) for e in self.ENG}
        outstanding_dma = [o for e in self.ENG for o in self.ops[e] if o.is_dma]
        with nc.Block() as block:
            for e in self.ENG:
                ops = ops_by_eng[e]
                if not ops:
                    continue

                def runner(h, ops=ops, e=e):
                    waited = self.waited[e]
                    for o in ops:
                        need = {}
                        for d in o.deps:
                            if d.is_dma:
                                s = self.dsems[d.sem]; key = "d_" + d.sem
                            else:
                                s = self.sems[d.eng]; key = "c_" + d.eng
                            if key not in need or need[key][1] < d.val:
                                need[key] = (s, d.val)
                        for key, (s, val) in need.items():
                            if waited.get(key, 0) >= val:
                                continue
                            h.wait_ge(s, val)
                            waited[key] = val
                        ins = o.fn(h)
                        if o.is_dma:
                            ins.then_inc(self.dsems[o.sem], 16)
                        elif o.signal:
                            ins.then_inc(self.sems[o.eng], 1)

                getattr(block, self.BLK[e])(runner)
        bl = list(last.values()) + outstanding_dma
        for e in self.ENG:
            self.barrier[e] = self.barrier[e] + bl
        self.ops = {e: [] for e in self.ENG}
        self.phase_map = {}
        self.phase += 1

    def bound_reg(self, h, val):
        if getattr(self, "_breg", None) is None:
            self._breg = h.alloc_register("bc")
            h.reg_mov(self._breg, val)
        return self._breg

    def finish(self):
        nc = self.nc
        with nc.Block() as block:
            def runner(h):
                for name, sem in self.dsems.items():
                    if self.dcount[name] > 0:
                        h.wait_ge(sem, self.dcount[name])
                for e in self.ENG:
                    if self.count[e] > 0:
                        h.wait_ge(self.sems[e], self.count[e])
            block.sync(runner)


def _consts():
    c = {}
    pos = np.arange(S, dtype=np.float32)
    inv = (500000.0 ** (-np.arange(0, 32, 2, dtype=np.float32) / 32)).astype(np.float32)
    ang = pos[:, None] * inv[None, :]
    c["rope_cs"] = np.concatenate([np.cos(ang), np.sin(ang)], axis=1).astype(np.float32)
    a = np.zeros((128, 32), np.float32)
    j = np.arange(32)
    for m in range(4):
        for n in range(2):
            i = 4 * j + m - n
            ok = (i >= 0) & (i < 127)
            np.add.at(a, (i[ok], j[ok]), 1.0)
    c["cmpA"] = a
    cc = np.arange(128)[:, None, None]; ii = np.arange(16)[None, :, None]; qq = np.arange(128)[None, None, :]
    c["cmpmask"] = np.where(16 * cc + 31 <= 128 * ii + qq, 0.0, NEG).astype(np.float32).reshape(128, 16 * 128)
    k = np.arange(128)[:, None]; q = np.arange(128)[None, :]
    tri = np.stack([np.where(k <= q, 0.0, NEG), np.where(k > q, 0.0, NEG)], axis=1).astype(np.float32)
    c["tri"] = tri.reshape(128, 256)
    e = np.zeros((32, 16, 128), np.float32)
    for jj in range(16):
        e[2 * jj, jj, :64] = 1.0
        e[2 * jj + 1, jj, 64:] = 1.0
    c["esel"] = e.reshape(32, 16 * 128)
    addm = np.zeros((128, 8, 32), np.float32)
    for i in range(8, 16):
        t = 128 * i + np.arange(128)[:, None]
        blk = np.arange(32)[None, :]
        cur = t // 64
        forced = (blk == 0) | ((blk <= cur) & (blk > cur - 2))
        avail = blk * 64 <= t
        addm[:, i - 8, :] = np.where(avail, np.where(forced, 1e4, 0.0), -1e30)
    c["addm"] = addm.reshape(128, 256)
    c["identf"] = np.eye(128, dtype=np.float32)
    c["upper"] = (np.arange(128)[:, None] < np.arange(128)[None, :]).astype(np.float32)
    c["eoff"] = np.broadcast_to((np.arange(64, dtype=np.float32) * CAP)[None, :], (128, 64)).copy()
    return c


def build(stop_after=None, nexp=NEXP):
    nc = bass.Bass("TRN2", target_bir_lowering=False)

    def din(name, shape, dt=F32):
        return nc.dram_tensor(name, list(shape), dt, kind="ExternalInput").ap()

    x = din("x", [S, D]); attn_norm = din("attn_norm", [16, 128]); w_in = din("w_in", [D, INC])
    conv_dw = din("conv_dw", [TAPS, CCH]); conv_dw_b = din("conv_dw_b", [8, 128])
    conv_ln_g = din("conv_ln_g", [8, 128]); conv_ln_b = din("conv_ln_b", [8, 128])
    cmp_pos = [din("cmp_pos_k", [32, 128]), din("cmp_pos_v", [32, 128])]
    cmp_w1 = [din("cmp_k_w1", [4096, 256]), din("cmp_v_w1", [4096, 256])]
    cmp_w2 = [din("cmp_k_w2", [256, 128]), din("cmp_v_w2", [256, 128])]
    w_proj_conv = din("w_proj_conv", [CCH, D]); w_proj_nsa = din("w_proj_nsa", [D, D])
    w_merge = din("w_merge", [D, 2 * D]); b_merge = din("b_merge", [32, 128]); w_out = din("w_out", [D, D])
    ffn_norm = din("ffn_norm", [1, D]); w_gr = din("w_gr", [D, 72]); b_gr = din("b_gr", [1, 72])
    exp_w1 = din("exp_w1", [nexp, D, FF]); exp_w3 = din("exp_w3", [nexp, D, FF]); exp_w2 = din("exp_w2", [nexp, FF, D])
    final_norm = din("final_norm", [1, D])
    rope_cs = din("rope_cs", [S, 32]); cmpA = din("cmpA", [128, 32]); cmpmask_d = din("cmpmask", [128, 2048])
    tri_d = din("tri", [128, 256]); esel_d = din("esel", [32, 2048]); addm_d = din("addm", [128, 256])
    identf_d = din("identf", [128, 128]); upper_d = din("upper", [128, 128]); eoff_d = din("eoff", [128, 64])

    dbg = stop_after is not None
    out = nc.dram_tensor("out", [S, D], F32, kind="ExternalOutput").ap()

    dbg_out = {"P1": ["HT"], "P2": ["UT"], "P3": ["OT"], "P4": ["X1", "RT"], "P5": []}.get(stop_after, [])

    def scratch(name, shape, dt):
        kind = "ExternalOutput" if name in dbg_out else "Internal"
        return nc.dram_tensor(name, list(shape), dt, kind=kind).ap()

    HT = scratch("HT", [D, S], BF16)
    UT = scratch("UT", [CCH, S], BF16)
    OT = scratch("OT", [D, S], BF16)
    X1 = scratch("X1", [S, D], F32)
    XS = scratch("XS", [NSLOT, D], BF16)
    YS = scratch("YS", [NSLOT, D], BF16)
    RT = scratch("RT", [S, 4], F32)
    import os as _os
    _skip = tuple(int(v) for v in _os.environ.get("DBG_SKIP", "3,7,11,13,15").split(",") if v != "")
    PRE = [e for e in range(nexp) if e % 16 not in _skip] if nexp == NEXP else []
    pre_idx = {e: j for j, e in enumerate(PRE)}
    npre = max(1, len(PRE))
    EB1 = nc.dram_tensor("EB1", [npre, 128, 16 * FF], BF16).ap()
    EB3 = nc.dram_tensor("EB3", [npre, 128, 16 * FF], BF16).ap()
    EB2 = nc.dram_tensor("EB2", [npre, 128, 4 * D], BF16).ap()
    bg_list = [(e, w) for e in PRE for w in range(3)]
    bg_pos = [0]

    with ExitStack() as top:
        Sc = Sched(nc, top)

        def sb(st, name, shape, dt):
            return st.enter_context(nc.sbuf_tensor("s_" + name, list(shape), dt))

        def ps(st, name, shape, dt):
            return st.enter_context(nc.psum_tensor("p_" + name, list(shape), dt))

        identf = sb(top, "identf", [128, 128], F32)
        identb = sb(top, "identb", [128, 128], BF16)
        onesb = sb(top, "onesb", [128, 128], BF16)
        Sc.dma("sp", lambda h: h.dma_start(out=identf[:], in_=identf_d[:, :]), "c0", writes=["identf"])
        Sc.dma("pool", lambda h: h.dma_start(out=identb[:], in_=identf_d[:, :]), "c1", writes=["identb"])
        Sc.add("pool", lambda h: h.memset(onesb[:], 1.0), writes=["onesb"])

        def bg(n=1):
            for _ in range(n):
                if bg_pos[0] >= len(bg_list):
                    return
                e, w = bg_list[bg_pos[0]]
                bg_pos[0] += 1
                j = pre_idx[e]
                src = [exp_w1, exp_w3, exp_w2][w][e].rearrange("(k p) n -> p k n", p=128)
                dst = [EB1, EB3, EB2][w][j].rearrange("p (k n) -> p k n", n=(D if w == 2 else FF))
                Sc.dma("pool", lambda h, src=src, dst=dst: h.dma_start(out=dst, in_=src), "bg", writes=["EB%d_%d" % (e, w)])

        def load_T(st, name, dram_ap, R, dst_ap, MISC):
            stg = sb(st, "stg_" + name, [32, 128], F32)
            Sc.dma("sp", lambda h: h.dma_start(out=stg[0:R, :], in_=dram_ap), "ld_" + name, writes=["stg_" + name])
            Sc.add("pe", lambda h: h.transpose(out=MISC[:, 0:R], in_=stg[0:R, :], identity=identf[0:R, 0:R]),
                   reads=["stg_" + name, "identf"], writes=["MISC"])
            Sc.add("dve", lambda h: h.tensor_copy(out=dst_ap, in_=MISC[:, 0:R]), reads=["MISC"], writes=[name])

        with ExitStack() as stA:
            hT = sb(stA, "hT", [128, 16, S], BF16)
            with ExitStack() as st:
                TB = ps(st, "TB_1", [128, 8, 128], BF16); TB2 = ps(st, "TB2_1", [128, 8, 128], BF16); MISC = ps(st, "MISC_1", [128, 512], F32)
                TBS = [TB, TB2]
                gT = sb(st, "gT", [128, 16], F32)
                load_T(st, "gT", attn_norm[:, :], 16, gT[:, :], MISC)
                xt = [sb(st, "xt%d" % i, [128, D], F32) for i in range(2)]
                xb = [sb(st, "xb%d" % i, [128, D], BF16) for i in range(2)]
                junk = sb(st, "junk", [128, D], BF16)
                ss = [sb(st, "ss%d" % i, [128, 1], F32) for i in range(2)]
                import os
                for i in range(int(os.environ.get("DBG_NT", NT))):
                    s = i % 2
                    Sc.dma("sp", lambda h, i=i, s=s: h.dma_start(out=xt[s][:], in_=x[i * 128:(i + 1) * 128, :]), "xt%d" % s, writes=["xt%d" % s])
                    Sc.add("act", lambda h, s=s: h.activation(out=junk[:], in_=xt[s][:], func=AF.Square, accum_out=ss[s][:]),
                           reads=["xt%d" % s], writes=["junk", "ss%d" % s])
                    Sc.add("act", lambda h, s=s: h.activation(out=ss[s][:], in_=ss[s][:], func=AF.Sqrt, scale=1.0 / D, bias=EPS),
                           reads=["ss%d" % s], writes=["ss%d" % s])
                    Sc.add("dve", lambda h, s=s: h.reciprocal(out=ss[s][:], in_=ss[s][:]), reads=["ss%d" % s], writes=["ss%d" % s])
                    Sc.add("dve", lambda h, s=s: h.tensor_scalar(out=xb[s][:], in0=xt[s][:], scalar1=ss[s][:, 0:1], scalar2=None, op0=ALU.mult),
                           reads=["xt%d" % s, "ss%d" % s], writes=["xb%d" % s])
                    for c4 in range(4):
                        for j in range(4):
                            c = 4 * c4 + j
                            Sc.add("pe", lambda h, s=s, c=c, j=j, c4=c4: h.transpose(out=TBS[c4 % 2][:, j, :], in_=xb[s][:, c * 128:(c + 1) * 128], identity=identb[:]),
                                   reads=["xb%d" % s, "identb"], writes=[["TB", "TB2"][c4 % 2]])
                        Sc.add("dve", lambda h, i=i, c4=c4: h.tensor_tensor(out=hT[:, 4 * c4:4 * c4 + 4, i * 128:(i + 1) * 128], in0=TBS[c4 % 2][:, 0:4, :],
                                                                   in1=gT[:, 4 * c4:4 * c4 + 4].unsqueeze(2).to_broadcast([128, 4, 128]), op=ALU.mult),
                               reads=[["TB", "TB2"][c4 % 2], "gT"], writes=["hT"])
                for kc in range(16):
                    Sc.dma("sp", lambda h, kc=kc: h.dma_start(out=HT[kc * 128:(kc + 1) * 128, :], in_=hT[:, kc, :]), "HTw", reads=["hT"], writes=["HT"])
                Sc.flush()
            if stop_after == "P1":
                Sc.finish(); return nc

            with ExitStack() as st:
                B0 = ps(st, "B0_2", [128, 512], F32); B1 = ps(st, "B1_2", [128, 512], F32); MISC = ps(st, "MISC_2", [128, 512], F32)
                dwT = sb(st, "dwT", [128, 8, 32], F32)
                cb = sb(st, "cb", [128, 3, 8], F32)
                dws = sb(st, "dws", [32, CCH], F32)
                Sc.dma("sp", lambda h: h.dma_start(out=dws[0:TAPS, :], in_=conv_dw[:, :]), "dws", writes=["dws"])
                for cc in range(8):
                    Sc.add("pe", lambda h, cc=cc: h.transpose(out=MISC[:, 0:TAPS], in_=dws[0:TAPS, cc * 128:(cc + 1) * 128], identity=identf[0:TAPS, 0:TAPS]),
                           reads=["dws", "identf"], writes=["MISC"])
                    Sc.add("dve", lambda h, cc=cc: h.tensor_copy(out=dwT[:, cc, 0:TAPS], in_=MISC[:, 0:TAPS]), reads=["MISC"], writes=["dwT"])
                load_T(st, "cb0", conv_dw_b[:, :], 8, cb[:, 0, :], MISC)
                load_T(st, "cb1", conv_ln_g[:, :], 8, cb[:, 1, :], MISC)
                load_T(st, "cb2", conv_ln_b[:, :], 8, cb[:, 2, :], MISC)
                zt = sb(st, "zt", [128, 1, D], BF16)
                Sc.add("pool", lambda h: h.memset(zt[:], 0.0), writes=["zt"])
                for zi in range(NSLOT // 2048):
                    Sc.dma("sp", lambda h, zi=zi: h.dma_start(out=XS[zi * 2048:(zi + 1) * 2048, :].rearrange("(a p) d -> p a d", p=128), in_=zt[:, 0:1, :].to_broadcast([128, 16, D])), "xsz%d" % zi, reads=["zt"], writes=["XSz%d" % zi])
                y_all = sb(st, "y_all", [128, 8, S], BF16)
                w_in_v = w_in.rearrange("(k p) n -> p k n", p=128)
                with ExitStack() as sc:
                    CA = [ps(sc, "CA0_2", [128, 512], F32), ps(sc, "CA1_2", [128, 512], F32)]
                    wa = sb(sc, "wa", [128, 16, 512], BF16); wb = sb(sc, "wb", [128, 16, 512], BF16)
                    glu2 = [sb(sc, "glu%d" % i, [128, 30 + S], BF16) for i in range(2)]
                    diag2 = [sb(sc, "diag%d" % i, [128, TAPS, 128], BF16) for i in range(2)]
                    sig = sb(sc, "sig", [128, 512], F32)
                    for i in range(2):
                        Sc.add("pool", lambda h, i=i: h.memset(glu2[i][:, 0:30], 0.0), writes=["glu%d" % i])

                    def c_proj(cc, tt):
                        c4 = cc % 4
                        glu = glu2[cc % 2]; kg = "glu%d" % (cc % 2)
                        for k in range(16):
                            Sc.add("pe", lambda h, k=k: h.matmul(B0[:, :], lhsT=wa[:, k, c4 * 128:(c4 + 1) * 128], rhs=hT[:, k, tt * 512:(tt + 1) * 512], start=(k == 0), stop=(k == 15)),
                                   reads=["wa", "hT"], writes=["B0"])
                        for k in range(16):
                            Sc.add("pe", lambda h, k=k: h.matmul(B1[:, :], lhsT=wb[:, k, c4 * 128:(c4 + 1) * 128], rhs=hT[:, k, tt * 512:(tt + 1) * 512], start=(k == 0), stop=(k == 15)),
                                   reads=["wb", "hT"], writes=["B1"])
                        Sc.add("act", lambda h: h.activation(out=sig[:], in_=B1[:, :], func=AF.Sigmoid), reads=["B1"], writes=["sig"])
                        Sc.add("dve", lambda h: h.tensor_tensor(out=glu[:, 30 + tt * 512:30 + (tt + 1) * 512], in0=B0[:, :], in1=sig[:], op=ALU.mult),
                               reads=["B0", "sig"], writes=[kg])

                    def c_diag(cc):
                        dg = diag2[cc % 2]
                        for k in range(TAPS):
                            Sc.add("pool", lambda h, k=k: h.tensor_scalar(out=dg[:, k, :], in0=identb[:], scalar1=dwT[:, cc, k:k + 1], scalar2=None, op0=ALU.mult),
                                   reads=["identb", "dwT"], writes=["diag%d" % (cc % 2)])

                    def c_conv(cc, tt):
                        glu = glu2[cc % 2]; kg = "glu%d" % (cc % 2); dg = diag2[cc % 2]
                        bank = CA[tt % 2]; bkey = ["OA", "OA1"][tt % 2]
                        for k in range(TAPS):
                            Sc.add("pe", lambda h, k=k: h.matmul(bank[:, :], lhsT=dg[:, k, :], rhs=glu[:, k + tt * 512:k + (tt + 1) * 512], start=(k == 0), stop=(k == TAPS - 1)),
                                   reads=[kg, "diag%d" % (cc % 2)], writes=[bkey])
                        Sc.add("act", lambda h: h.activation(out=y_all[:, cc, tt * 512:(tt + 1) * 512], in_=bank[:, :], func=AF.Identity, bias=cb[:, 0, cc:cc + 1]),
                               reads=[bkey, "cb0"], writes=["y_all"])

                    def c_loadw(gi):
                        Sc.dma("pool", lambda h: h.dma_start(out=wa[:], in_=w_in_v[:, :, gi * 512:(gi + 1) * 512]), "wa", writes=["wa"])
                        Sc.dma("pool", lambda h: h.dma_start(out=wb[:], in_=w_in_v[:, :, CCH + gi * 512:CCH + (gi + 1) * 512]), "wb", writes=["wb"])

                    c_loadw(0)
                    c_diag(0); c_diag(1)
                    for tt in range(4):
                        c_proj(0, tt)
                    for cc in range(8):
                        if cc + 1 < 8 and (cc + 1) % 4 == 0:
                            c_loadw((cc + 1) // 4)
                            for tt in range(4):
                                c_conv(cc, tt)
                            for tt in range(4):
                                c_proj(cc + 1, tt)
                        else:
                            for tt in range(4):
                                if cc + 1 < 8:
                                    c_proj(cc + 1, tt)
                                c_conv(cc, tt)
                        if cc + 2 < 8:
                            c_diag(cc + 2)
                        bg(2)
                    Sc.flush()
                acc = sb(st, "acc", [128, S], F32)
                mean_b = sb(st, "mean_b", [128, S], F32); rstd_b = sb(st, "rstd_b", [128, S], F32)
                ysq = sb(st, "ysq", [128, 512], BF16); msq = sb(st, "msq", [128, 512], F32)
                for tt in range(4):
                    tsl = slice(tt * 512, (tt + 1) * 512)
                    for cc in range(8):
                        Sc.add("pe", lambda h, cc=cc, tsl=tsl: h.matmul(B0[:, :], lhsT=onesb[:, :], rhs=y_all[:, cc, tsl], start=(cc == 0), stop=(cc == 7)),
                               reads=["y_all", "onesb"], writes=["B0"])
                    for cc in range(8):
                        Sc.add("act", lambda h, cc=cc, tsl=tsl: h.activation(out=ysq[:], in_=y_all[:, cc, tsl], func=AF.Square), reads=["y_all"], writes=["ysq"])
                        Sc.add("pe", lambda h, cc=cc: h.matmul(B1[:, :], lhsT=onesb[:, :], rhs=ysq[:], start=(cc == 0), stop=(cc == 7)),
                               reads=["ysq", "onesb"], writes=["B1"])
                    Sc.add("dve", lambda h, tsl=tsl: h.tensor_scalar(out=mean_b[:, tsl], in0=B0[:, :], scalar1=1.0 / CCH, scalar2=None, op0=ALU.mult), reads=["B0"], writes=["mean_b"])
                    Sc.add("dve", lambda h, tsl=tsl: h.tensor_tensor(out=msq[:], in0=mean_b[:, tsl], in1=mean_b[:, tsl], op=ALU.mult), reads=["mean_b"], writes=["msq"])
                    Sc.add("dve", lambda h, tsl=tsl: h.scalar_tensor_tensor(out=rstd_b[:, tsl], in0=B1[:, :], scalar=1.0 / CCH, in1=msq[:], op0=ALU.mult, op1=ALU.subtract),
                           reads=["B1", "msq"], writes=["rstd_b"])
                    Sc.add("act", lambda h, tsl=tsl: h.activation(out=rstd_b[:, tsl], in_=rstd_b[:, tsl], func=AF.Sqrt, bias=EPS), reads=["rstd_b"], writes=["rstd_b"])
                    Sc.add("dve", lambda h, tsl=tsl: h.reciprocal(out=rstd_b[:, tsl], in_=rstd_b[:, tsl]), reads=["rstd_b"], writes=["rstd_b"])
                ub = [sb(st, "ub%d" % i, [128, S], BF16) for i in range(2)]
                for cc in range(8):
                    s = cc % 2
                    Sc.add("dve", lambda h, cc=cc: h.tensor_tensor(out=acc[:], in0=y_all[:, cc, :], in1=mean_b[:], op=ALU.subtract), reads=["y_all", "mean_b"], writes=["acc"])
                    Sc.add("dve", lambda h: h.tensor_tensor(out=acc[:], in0=acc[:], in1=rstd_b[:], op=ALU.mult), reads=["acc", "rstd_b"], writes=["acc"])
                    Sc.add("act", lambda h, cc=cc, s=s: h.activation(out=ub[s][:], in_=acc[:], func=AF.Silu, scale=cb[:, 1, cc:cc + 1], bias=cb[:, 2, cc:cc + 1]),
                           reads=["acc", "cb1", "cb2"], writes=["ub%d" % s])
                    Sc.dma("sp", lambda h, cc=cc, s=s: h.dma_start(out=UT[cc * 128:(cc + 1) * 128, :], in_=ub[s][:]), "UTw%d" % s, reads=["ub%d" % s], writes=["UT"])
                Sc.flush()
            if stop_after == "P2":
                Sc.finish(); return nc

            with ExitStack() as st:
                B0 = ps(st, "B0_3", [128, 512], F32); B1 = ps(st, "B1_3", [128, 512], F32); MISC = ps(st, "MISC_3", [128, 512], F32)
                OA0 = ps(st, "OA0_3", [128, 512], F32); OA1 = ps(st, "OA1_3", [128, 512], F32)
                OB0 = ps(st, "OB0_3", [128, 512], F32); OB1 = ps(st, "OB1_3", [128, 512], F32)
                TB = ps(st, "TB_3", [128, 8, 128], BF16)
                cs = sb(st, "cs", [128, NT, 32], F32)
                Sc.dma("sp", lambda h: h.dma_start(out=cs[:], in_=rope_cs.rearrange("(i p) c -> p i c", p=128)), "cs", writes=["cs"])
                cmpmask = sb(st, "cmpmask", [128, NT, 128], BF16)
                Sc.dma("pool", lambda h: h.dma_start(out=cmpmask[:], in_=cmpmask_d.rearrange("p (i q) -> p i q", q=128)), "cmpmask", writes=["cmpmask"])
                tri = sb(st, "tri", [128, 2, 128], BF16)
                Sc.dma("pool", lambda h: h.dma_start(out=tri[:], in_=tri_d.rearrange("p (i q) -> p i q", q=128)), "tri", writes=["tri"])
                esel = sb(st, "esel", [32, 16, 128], BF16)
                Sc.dma("pool", lambda h: h.dma_start(out=esel[:], in_=esel_d.rearrange("p (i q) -> p i q", q=128)), "esel", writes=["esel"])
                addm = sb(st, "addm", [128, 8, 32], F32)
                Sc.dma("sp", lambda h: h.dma_start(out=addm[:], in_=addm_d.rearrange("p (i q) -> p i q", q=32)), "addm", writes=["addm"])
                wg = sb(st, "wg", [128, 16, 48], BF16)
                w_in_v = w_in.rearrange("(k p) n -> p k n", p=128)
                Sc.dma("pool", lambda h: h.dma_start(out=wg[:], in_=w_in_v[:, :, 7168:7216]), "wg", writes=["wg"])
                w2 = [sb(st, "w2_%d" % i, [128, 2, 128], BF16) for i in range(2)]
                posT = [sb(st, "posT%d" % i, [128, 32], BF16) for i in range(2)]
                posf2 = [sb(st, "posf%d" % i, [128, 32], F32) for i in range(2)]
                for kv in range(2):
                    Sc.dma("pool", lambda h, kv=kv: h.dma_start(out=w2[kv][:], in_=cmp_w2[kv].rearrange("(b p) d -> p b d", p=128)), "w2_%d" % kv, writes=["w2_%d" % kv])
                    load_T(st, "posf%d" % kv, cmp_pos[kv][:, :], 32, posf2[kv][:, :], MISC)
                    Sc.add("dve", lambda h, kv=kv: h.tensor_copy(out=posT[kv][:], in_=posf2[kv][:]), reads=["posf%d" % kv], writes=["posT%d" % kv])
                gates = sb(st, "gates", [128, NT, 48], F32)
                arena = sb(st, "arena", [128, 20480], BF16)
                wq = arena[:, 0:8192].rearrange("p (k n) -> p k n", n=512)
                wkv = arena[:, 8192:20480].rearrange("p (k s n) -> p k s n", s=6, n=128)
                w1 = [arena[:, 0:8192].rearrange("p (j n) -> p j n", n=256), arena[:, 8192:16384].rearrange("p (j n) -> p j n", n=256)]
                qT = sb(st, "qT", [128, 4, S], BF16)
                kT = sb(st, "kT", [128, 3, S], BF16)
                vcT = sb(st, "vcT", [128, S], BF16)
                vslc = sb(st, "vslc", [128, NT, 132], BF16)
                vwin = sb(st, "vwin", [128, NT, 132], BF16)
                vcx = sb(st, "vcx", [128, 164], BF16)
                kcT = sb(st, "kcT", [128, 128], BF16)
                hid = [sb(st, "hid%d" % i, [128, 2, 128], BF16) for i in range(2)]
                qtok2 = [sb(st, "qtok%d" % i, [128, 4, 128], BF16) for i in range(2)]
                ktok2 = [sb(st, "ktok%d" % i, [128, 4, 128], BF16) for i in range(2)]
                rt = [sb(st, "rt%d" % i, [128, 4, 16], F32) for i in range(2)]
                cmpAf = sb(st, "cmpAf", [128, 32], F32)
                Sc.dma("sp", lambda h: h.dma_start(out=cmpAf[:], in_=cmpA[:, :]), "cmpAf", writes=["cmpAf"])
                Sc.add("pool", lambda h: h.memset(vcx[:], 0.0), writes=["vcx"])
                Sc.add("pool", lambda h: h.memset(vcx[:, 128:129], 1.0), reads=["vcx"], writes=["vcx"])
                Sc.add("dve", lambda h: h.tensor_copy(out=vcx[:, 129:161], in_=cmpAf[:]), reads=["cmpAf", "vcx"], writes=["vcx"])
                Sc.add("pool", lambda h: h.memset(vslc[:, :, 128:129], 1.0), writes=["vslc"])
                Sc.add("pool", lambda h: h.memset(vwin[:, :, 128:129], 1.0), writes=["vwin"])
                pT = [sb(st, "pT%d" % i, [128, 512], BF16) for i in range(3)]
                gsq = sb(st, "gsq", [128, 128], F32); gu = sb(st, "gu", [128, 128], F32)
                den = sb(st, "den", [128, 3, 4], F32); fac = sb(st, "fac", [128, 3, 4], F32)
                imp = sb(st, "imp", [128, 32], F32); impw = sb(st, "impw", [128, 32], F32); m8 = sb(st, "m8", [128, 16], F32)
                negsel = sb(st, "negsel", [128, 32], BF16); negselT = sb(st, "negselT", [32, 128], BF16)
                oacc = sb(st, "oacc", [128, 4, 128], F32); otok = sb(st, "otok", [128, 4, 128], BF16)
                oTb = [sb(st, "oTb%d" % i, [128, 4, 512], BF16) for i in range(2)]
                SB = [B0, B1, MISC]
                OAv = [OA0[:, :].rearrange("p (a b) -> p a b", b=256), OA1[:, :].rearrange("p (a b) -> p a b", b=256)]
                OBv = [OB0[:, :].rearrange("p (a b) -> p a b", b=256), OB1[:, :].rearrange("p (a b) -> p a b", b=256)]
                pcount = [0]

                def rope(src, nh, dst, i, tag, wkey):
                    cosb = cs[:, i, 0:16].unsqueeze(1).to_broadcast([128, nh, 16])
                    sinb = cs[:, i, 16:32].unsqueeze(1).to_broadcast([128, nh, 16])
                    x1 = src[:, :, 0:16]; x2 = src[:, :, 16:32]
                    t0 = rt[0][:, 0:nh, :]; t1 = rt[1][:, 0:nh, :]
                    rd = [tag]
                    Sc.add("dve", lambda h: h.tensor_tensor(out=t0, in0=x1, in1=cosb, op=ALU.mult), reads=rd + ["cs"], writes=["rt0"])
                    Sc.add("dve", lambda h: h.tensor_tensor(out=t1, in0=x2, in1=sinb, op=ALU.mult), reads=rd + ["cs"], writes=["rt1"])
                    Sc.add("dve", lambda h: h.tensor_tensor(out=dst[:, :, 0:16], in0=t0, in1=t1, op=ALU.subtract), reads=["rt0", "rt1"], writes=[wkey])
                    Sc.add("dve", lambda h: h.tensor_tensor(out=t0, in0=x1, in1=sinb, op=ALU.mult), reads=rd + ["cs"], writes=["rt0"])
                    Sc.add("dve", lambda h: h.tensor_tensor(out=t1, in0=x2, in1=cosb, op=ALU.mult), reads=rd + ["cs"], writes=["rt1"])
                    Sc.add("dve", lambda h: h.tensor_tensor(out=dst[:, :, 16:32], in0=t0, in1=t1, op=ALU.add), reads=["rt0", "rt1"], writes=[wkey])

                for g in range(NG):
                    Sc.dma("pool", lambda h, g=g: h.dma_start(out=wq, in_=w_in_v[:, :, 2048 + g * 512:2048 + (g + 1) * 512]), "wq", writes=["arena0"])
                    for s6 in range(6):
                        c0 = 4096 + s6 * 512 + g * 128
                        Sc.dma("pool", lambda h, s6=s6, c0=c0: h.dma_start(out=wkv[:, :, s6, :], in_=w_in_v[:, :, c0:c0 + 128]), "wkv", writes=["arena1"])
                    def proj_mm(i):
                        p = i % 2
                        tsl = slice(i * 128, (i + 1) * 128)
                        bq = [B0, MISC][p]; kq = ["B0", "MISC"][p]
                        b1 = [B1, OB1][p]; k1 = ["B1", "OB1"][p]
                        b2 = [OA0, OA1][p]; k2 = ["OA", "OA1"][p]
                        for k in range(16):
                            Sc.add("pe", lambda h, k=k: h.matmul(bq[:, :], lhsT=hT[:, k, tsl], rhs=wq[:, k, :], start=(k == 0), stop=(k == 15)), reads=["hT", "arena0"], writes=[kq])
                        for k in range(16):
                            Sc.add("pe", lambda h, k=k: h.matmul(b1[:, :], lhsT=hT[:, k, tsl], rhs=wkv[:, k, 0:4, :], start=(k == 0), stop=(k == 15)), reads=["hT", "arena1"], writes=[k1])
                        for k in range(16):
                            Sc.add("pe", lambda h, k=k: h.matmul(b2[:, 0:256], lhsT=hT[:, k, tsl], rhs=wkv[:, k, 4:6, :], start=(k == 0), stop=(k == 15)), reads=["hT", "arena1"], writes=[k2])
                        if g == 0:
                            for k in range(16):
                                Sc.add("pe", lambda h, k=k: h.matmul(OB0[:, 0:48], lhsT=hT[:, k, tsl], rhs=wg[:, k, :], start=(k == 0), stop=(k == 15)), reads=["hT", "wg"], writes=["OB"])

                    def proj_evac(i):
                        p = i % 2
                        bq = [B0, MISC][p]; kq = ["B0", "MISC"][p]
                        b1 = [B1, OB1][p]; k1 = ["B1", "OB1"][p]
                        b2 = [OA0, OA1][p]; k2 = ["OA", "OA1"][p]
                        qtk = qtok2[p]; ktk = ktok2[p]; sq = "qtok%d" % p; sk = "ktok%d" % p
                        if g == 0:
                            Sc.add("act", lambda h: h.activation(out=gates[:, i, :], in_=OB0[:, 0:48], func=AF.Sigmoid), reads=["OB"], writes=["gates"])
                        B0v = bq[:, :].rearrange("p (a b) -> p a b", b=128)
                        B1v = b1[:, :].rearrange("p (a b) -> p a b", b=128)
                        OAp = b2[:, 0:256].rearrange("p (a b) -> p a b", b=128)
                        Sc.add("act", lambda h: h.activation(out=qtk[:, :, 32:128], in_=B0v[:, :, 32:128], func=AF.Copy), reads=[kq], writes=[sq + "a"])
                        rope(B0v, 4, qtk[:, :, :], i, kq, sq + "r")
                        Sc.add("act", lambda h: h.activation(out=ktk[:, 0:2, 32:128], in_=B1v[:, 0:4:2, 32:128], func=AF.Copy), reads=[k1], writes=[sk + "a"])
                        rope(B1v[:, 0:4:2, :], 2, ktk[:, 0:2, :], i, k1, sk + "r1")
                        Sc.add("act", lambda h: h.activation(out=ktk[:, 2:3, 32:128], in_=OAp[:, 0:1, 32:128], func=AF.Copy), reads=[k2], writes=[sk + "b"])
                        rope(OAp[:, 0:1, :], 1, ktk[:, 2:3, :], i, k2, sk + "r2")
                        Sc.add("act", lambda h: h.activation(out=ktk[:, 3, :], in_=B1v[:, 1, :], func=AF.Copy), reads=[k1], writes=[sk + "c"])
                        Sc.add("act", lambda h: h.activation(out=vslc[:, i, 0:128], in_=B1v[:, 3, :], func=AF.Copy), reads=[k1], writes=["vslc"])
                        Sc.add("act", lambda h: h.activation(out=vwin[:, i, 0:128], in_=OAp[:, 1, :], func=AF.Copy), reads=[k2], writes=["vwin"])

                    def proj_tr(i):
                        p = i % 2
                        tsl = slice(i * 128, (i + 1) * 128)
                        qtk = qtok2[p]; ktk = ktok2[p]; sq = "qtok%d" % p; sk = "ktok%d" % p
                        for r in range(4):
                            Sc.add("pe", lambda h, r=r: h.transpose(out=TB[:, r, :], in_=qtk[:, r, :], identity=identb[:]), reads=[sq + "a", sq + "r", "identb"], writes=["TB"])
                        for r in range(4):
                            Sc.add("pe", lambda h, r=r: h.transpose(out=TB[:, 4 + r, :], in_=ktk[:, r, :], identity=identb[:]),
                                   reads=[sk + "a", sk + "b", sk + "c", sk + "r1", sk + "r2", "identb"], writes=["TB"])
                        Sc.add("act", lambda h: h.activation(out=qT[:, :, tsl], in_=TB[:, 0:4, :], func=AF.Copy), reads=["TB"], writes=["qT"])
                        Sc.add("act", lambda h: h.activation(out=kT[:, :, tsl], in_=TB[:, 4:7, :], func=AF.Copy), reads=["TB"], writes=["kT"])
                        Sc.add("act", lambda h: h.activation(out=vcT[:, tsl], in_=TB[:, 7, :], func=AF.Copy), reads=["TB"], writes=["vcT"])

                    proj_mm(0); proj_evac(0)
                    for i in range(NT):
                        if i + 1 < NT:
                            proj_mm(i + 1); proj_evac(i + 1)
                        proj_tr(i)
                        if i % 2 == 1:
                            bg(1)
                    for kv in range(2):
                        Sc.dma("pool", lambda h, kv=kv: h.dma_start(out=w1[kv], in_=cmp_w1[kv].rearrange("(j d) n -> d j n", d=128)), "w1_%d" % kv, writes=["arena%d" % kv])
                    for kv in range(2):
                        srcT = kT[:, 0, :] if kv == 0 else vcT[:, :]
                        skey = "kT" if kv == 0 else "vcT"
                        for blk in range(2):
                            for j in range(32):
                                rhs = bass.AP(srcT.tensor, srcT.offset + j, [[srcT.ap[0][0], 128], [16, 127]])
                                Sc.add("pe", lambda h, kv=kv, blk=blk, j=j, rhs=rhs: h.matmul(MISC[:, 0:127], lhsT=w1[kv][:, j, blk * 128:(blk + 1) * 128], rhs=rhs, start=(j == 0), stop=False),
                                       reads=["arena%d" % kv, skey], writes=["MISC"])
                                Sc.add("pe", lambda h, kv=kv, blk=blk, j=j: h.matmul(MISC[:, 0:127], lhsT=w1[kv][:, j, blk * 128:(blk + 1) * 128], rhs=posT[kv][:, j:j + 1].to_broadcast([128, 127]), start=False, stop=(j == 31)),
                                       reads=["arena%d" % kv, "posT%d" % kv], writes=["MISC"])
                            Sc.add("act", lambda h: h.activation(out=gsq[:, 0:127], in_=MISC[:, 0:127], func=AF.Square), reads=["MISC"], writes=["gsq"])
                            Sc.add("dve", lambda h: h.tensor_scalar(out=gsq[:, 0:127], in0=gsq[:, 0:127], scalar1=0.044715, scalar2=1.0, op0=ALU.mult, op1=ALU.add), reads=["gsq"], writes=["gsq"])
                            Sc.add("dve", lambda h: h.tensor_tensor(out=gu[:, 0:127], in0=gsq[:, 0:127], in1=MISC[:, 0:127], op=ALU.mult), reads=["gsq", "MISC"], writes=["gu"])
                            Sc.add("act", lambda h: h.activation(out=gu[:, 0:127], in_=gu[:, 0:127], func=AF.Tanh, scale=0.7978845608), reads=["gu"], writes=["gu"])
                            Sc.add("dve", lambda h: h.tensor_scalar(out=gu[:, 0:127], in0=gu[:, 0:127], scalar1=1.0, scalar2=0.5, op0=ALU.add, op1=ALU.mult), reads=["gu"], writes=["gu"])
                            Sc.add("dve", lambda h, kv=kv, blk=blk: h.tensor_tensor(out=hid[kv][:, blk, 0:127], in0=gu[:, 0:127], in1=MISC[:, 0:127], op=ALU.mult), reads=["gu", "MISC"], writes=["hid%d" % kv])
                    for blk in range(2):
                        Sc.add("pe", lambda h, blk=blk: h.matmul(MISC[:, 0:127], lhsT=w2[0][:, blk, :], rhs=hid[0][:, blk, 0:127], start=(blk == 0), stop=(blk == 1)), reads=["w2_0", "hid0"], writes=["MISC"])
                    Sc.add("dve", lambda h: h.tensor_copy(out=kcT[:, 0:127], in_=MISC[:, 0:127]), reads=["MISC"], writes=["kcT"])
                    for blk in range(2):
                        Sc.add("pe", lambda h, blk=blk: h.matmul(MISC[0:127, 0:128], lhsT=hid[1][:, blk, 0:127], rhs=w2[1][:, blk, :], start=(blk == 0), stop=(blk == 1)), reads=["w2_1", "hid1"], writes=["MISC"])
                    Sc.add("dve", lambda h: h.tensor_copy(out=vcx[0:127, 0:128], in_=MISC[0:127, 0:128]), reads=["MISC"], writes=["vcx"])

                    OAK = ["OA", "OA1"]; OBK = ["OB", "OB1"]
                    def emit_qk(job):
                        sidx = pcount[0] % 3
                        pcount[0] += 1
                        job["sidx"] = sidx
                        bank = SB[sidx]; bkey = ["B0", "B1", "MISC"][sidx]
                        i = job["i"]; npart = job["npart"]; lhsT = job["lhsT"]; extra = job["extra"]
                        qs = qT[:, :, i * 128:(i + 1) * 128]
                        n = len(extra)
                        Sc.add("pe", lambda h: h.matmul(bank[0:npart, :], lhsT=lhsT, rhs=qs, start=True, stop=(n == 0)), reads=["qT"] + job["lkeys"], writes=[bkey])
                        for xi, (l2, r2, k2) in enumerate(extra):
                            Sc.add("pe", lambda h, l2=l2, r2=r2, xi=xi: h.matmul(bank[0:npart, :], lhsT=l2, rhs=r2, start=False, stop=(xi == n - 1)), reads=k2, writes=[bkey])
                        Sc.add("act", lambda h: h.activation(out=pT[sidx][0:npart, :], in_=bank[0:npart, :], func=AF.Exp, scale=SCALE), reads=[bkey], writes=["pT%d" % sidx])

                    def bc4(ap2, npart):
                        return ap2.unsqueeze(1).to_broadcast([npart, 4, 128])

                    def denfac(branch, Ov, okey, i, g):
                        for b in range(2):
                            Sc.add("dve", lambda h, b=b: h.tensor_scalar(out=den[:, branch, 2 * b:2 * b + 2], in0=Ov[b][:, :, 128], scalar1=1e-30, scalar2=None, op0=ALU.max), reads=okey, writes=["den%d" % branch])
                        Sc.add("dve", lambda h: h.reciprocal(out=den[:, branch, :], in_=den[:, branch, :]), reads=["den%d" % branch], writes=["den%d" % branch])
                        gc0 = branch * 16 + g * 4
                        Sc.add("dve", lambda h: h.tensor_tensor(out=fac[:, branch, :], in0=den[:, branch, :], in1=gates[:, i, gc0:gc0 + 4], op=ALU.mult), reads=["den%d" % branch, "gates"], writes=["fac%d" % branch])

                    pending_pe = []

                    def make_jobs(i, g):
                        jobs = []

                        def pv_cmp(sidx):
                            for r in range(4):
                                Sc.add("pe", lambda h, r=r: h.matmul(OBv[r // 2][:, r % 2, 0:161], lhsT=pT[sidx][0:127, r * 128:(r + 1) * 128], rhs=vcx[0:127, 0:161], start=True, stop=True),
                                       reads=["pT%d" % sidx, "vcx"], writes=OBK)

                        def post_cmp():
                            denfac(0, OBv, OBK, i, g)
                            if i >= 8:
                                for r in range(4):
                                    if r == 0:
                                        Sc.add("dve", lambda h: h.tensor_scalar(out=imp[:], in0=OBv[0][:, 0, 129:161], scalar1=den[:, 0, 0:1], scalar2=None, op0=ALU.mult), reads=OBK + ["den0"], writes=["imp"])
                                    else:
                                        Sc.add("dve", lambda h, r=r: h.scalar_tensor_tensor(out=imp[:], in0=OBv[r // 2][:, r % 2, 129:161], scalar=den[:, 0, r:r + 1], in1=imp[:], op0=ALU.mult, op1=ALU.add), reads=OBK + ["den0", "imp"], writes=["imp"])
                            for r in range(4):
                                Sc.add("dve", lambda h, r=r: h.tensor_scalar(out=oacc[:, r, :], in0=OBv[r // 2][:, r % 2, 0:128], scalar1=fac[:, 0, r:r + 1], scalar2=None, op0=ALU.mult), reads=OBK + ["fac0"], writes=["oacc"])
                            if i >= 8:
                                Sc.add("dve", lambda h: h.tensor_tensor(out=imp[:], in0=imp[:], in1=addm[:, i - 8, :], op=ALU.add), reads=["imp", "addm"], writes=["imp"])
                                Sc.add("dve", lambda h: h.max(out=m8[:, 0:8], in_=imp[:]), reads=["imp"], writes=["m8"])
                                Sc.add("dve", lambda h: h.match_replace(out=impw[:], in_to_replace=m8[:, 0:8], in_values=imp[:], imm_value=-3e38), reads=["imp", "m8"], writes=["impw"])
                                Sc.add("dve", lambda h: h.max(out=m8[:, 8:16], in_=impw[:]), reads=["impw"], writes=["m8"])
                                Sc.add("dve", lambda h: h.tensor_scalar(out=impw[:], in0=imp[:], scalar1=m8[:, 15:16], scalar2=None, op0=ALU.is_ge), reads=["imp", "m8"], writes=["impw"])
                                Sc.add("dve", lambda h: h.tensor_scalar(out=negsel[:], in0=impw[:], scalar1=-1.0, scalar2=-NEG, op0=ALU.add, op1=ALU.mult), reads=["impw"], writes=["negsel"])
                                def pe_part():
                                    Sc.add("pe", lambda h: h.transpose(out=TB[0:32, 4, :], in_=negsel[:, :], identity=identb[:]), reads=["negsel", "identb"], writes=["TB"])
                                    Sc.add("dve", lambda h: h.tensor_copy(out=negselT[:, :], in_=TB[0:32, 4, :]), reads=["TB"], writes=["negselT"])
                                pending_pe.append(pe_part)

                        jobs.append(dict(i=i, lhsT=kcT[:, 0:127], lkeys=["kcT"], npart=127, pv=pv_cmp, post=post_cmp,
                                         extra=[(identb[0:127, 0:127], bc4(cmpmask[0:127, i, :], 127), ["identb", "cmpmask"])]))

                        j0 = max(0, i - 4)
                        for j in range(j0, i + 1):
                            extra = []
                            if j == i:
                                extra.append((identb[:, :], bc4(tri[:, 0, :], 128), ["identb", "tri"]))
                            if j == i - 4:
                                extra.append((identb[:, :], bc4(tri[:, 1, :], 128), ["identb", "tri"]))

                            def pv_win(sidx, j=j):
                                for r in range(4):
                                    Sc.add("pe", lambda h, r=r: h.matmul(OAv[r // 2][:, r % 2, 0:129], lhsT=pT[sidx][:, r * 128:(r + 1) * 128], rhs=vwin[:, j, 0:129], start=(j == j0 and r % 2 == 0), stop=(j == i), skip_group_check=True),
                                           reads=["pT%d" % sidx, "vwin"], writes=OAK)

                            jobs.append(dict(i=i, lhsT=kT[:, 2, j * 128:(j + 1) * 128], lkeys=["kT"], npart=128, pv=pv_win, post=None, extra=extra))

                        def post_combine():
                            denfac(1, OBv, OBK, i, g)
                            for r in range(4):
                                Sc.add("dve", lambda h, r=r: h.scalar_tensor_tensor(out=oacc[:, r, :], in0=OBv[r // 2][:, r % 2, 0:128], scalar=fac[:, 1, r:r + 1], in1=oacc[:, r, :], op0=ALU.mult, op1=ALU.add), reads=OBK + ["fac1", "oacc"], writes=["oacc"])
                            denfac(2, OAv, OAK, i, g)
                            for r in range(4):
                                Sc.add("dve", lambda h, r=r: h.scalar_tensor_tensor(out=otok[:, r, :], in0=OAv[r // 2][:, r % 2, 0:128], scalar=fac[:, 2, r:r + 1], in1=oacc[:, r, :], op0=ALU.mult, op1=ALU.add), reads=OAK + ["fac2", "oacc"], writes=["otok"])
                            bg(1)

                            def pe_part():
                                for r in range(4):
                                    Sc.add("pe", lambda h, r=r: h.transpose(out=TB[:, r, :], in_=otok[:, r, :], identity=identb[:]), reads=["otok", "identb"], writes=["TB"])
                                ob = (i // 4) % 2
                                Sc.add("act", lambda h: h.activation(out=oTb[ob][:, :, (i % 4) * 128:(i % 4 + 1) * 128], in_=TB[:, 0:4, :], func=AF.Copy), reads=["TB"], writes=["oTb%d" % ob])
                                if i % 4 == 3:
                                    t0 = (i // 4) * 512
                                    Sc.dma("sp", lambda h: h.dma_start(out=OT.rearrange("(hh d) t -> d hh t", d=128)[:, g * 4:(g + 1) * 4, t0:t0 + 512], in_=oTb[ob][:]), "OTw%d" % ob, reads=["oTb%d" % ob], writes=["OT"])
                            pending_pe.append(pe_part)

                        for j in range(0, i + 1):
                            extra = []
                            if j == i:
                                extra.append((identb[:, :], bc4(tri[:, 0, :], 128), ["identb", "tri"]))
                            if i >= 8:
                                extra.append((esel[0:32, j, :], bc4(negselT[0:32, :], 32), ["esel", "negselT"]))

                            def pv_slc(sidx, j=j):
                                for r in range(4):
                                    Sc.add("pe", lambda h, r=r: h.matmul(OBv[r // 2][:, r % 2, 0:129], lhsT=pT[sidx][:, r * 128:(r + 1) * 128], rhs=vslc[:, j, 0:129], start=(j == 0 and r % 2 == 0), stop=(j == i), skip_group_check=True),
                                           reads=["pT%d" % sidx, "vslc"], writes=OBK)

                            jobs.append(dict(i=i, lhsT=kT[:, 1, j * 128:(j + 1) * 128], lkeys=["kT"], npart=128, pv=pv_slc, post=(post_combine if j == i else None), extra=extra))
                        return jobs

                    jobs = []
                    for i in range(NT):
                        jobs += make_jobs(i, g)
                    LOOK = 2
                    for idx in range(min(LOOK, len(jobs))):
                        emit_qk(jobs[idx])
                    deferred = []
                    for idx, job in enumerate(jobs):
                        job["pv"](job["sidx"])
                        if job["post"] is not None:
                            job["post"]()
                        while pending_pe:
                            deferred.append((idx + 2, pending_pe.pop(0)))
                        while deferred and deferred[0][0] <= idx:
                            deferred.pop(0)[1]()
                        if idx + LOOK < len(jobs):
                            emit_qk(jobs[idx + LOOK])
                    while deferred:
                        deferred.pop(0)[1]()
                Sc.flush()
        if stop_after == "P3":
            Sc.finish(); return nc

        dest_i = sb(top, "dest_i", [128, NT, 2], I32)
        gate2 = sb(top, "gate2", [128, NT, 2], F32)
        with ExitStack() as st:
            B0 = ps(st, "B0_4", [128, 512], F32); B1 = ps(st, "B1_4", [128, 512], F32); MISC = ps(st, "MISC_4", [128, 512], F32)
            OA0 = ps(st, "OA0_4", [128, 512], F32); OB0 = ps(st, "OB0_4", [128, 512], F32)
            TB = ps(st, "TB_4", [128, 8, 128], BF16); TB2 = ps(st, "TB2_4", [128, 8, 128], BF16)
            TBS = [TB, TB2]
            bmT = sb(st, "bmT", [128, 32], F32)
            load_T(st, "bmT", b_merge[:, :], 32, bmT[:, :], MISC)
            fng = sb(st, "fng", [128, D], F32)
            Sc.dma("sp", lambda h: h.dma_start(out=fng[:], in_=ffn_norm[0:1, :].broadcast_to([128, D])), "fng", writes=["fng"])
            bgr = sb(st, "bgr", [128, 72], F32)
            Sc.dma("sp", lambda h: h.dma_start(out=bgr[:], in_=b_gr[0:1, :].broadcast_to([128, 72])), "bgr", writes=["bgr"])
            wgr = sb(st, "wgr", [128, 16, 72], BF16)
            Sc.dma("pool", lambda h: h.dma_start(out=wgr[:], in_=w_gr.rearrange("(k p) n -> p k n", p=128)), "wgr", writes=["wgr"])
            upper = sb(st, "upper", [128, 128], BF16)
            Sc.dma("pool", lambda h: h.dma_start(out=upper[:], in_=upper_d[:, :]), "upper", writes=["upper"])
            eoff = sb(st, "eoff", [128, 64], F32)
            Sc.dma("sp", lambda h: h.dma_start(out=eoff[:], in_=eoff_d[:, :]), "eoff", writes=["eoff"])
            base = sb(st, "base", [128, 64], F32)
            Sc.add("pool", lambda h: h.memset(base[:], 0.0), writes=["base"])

            HALF = 1024
            mT = sb(st, "mT", [128, 16, HALF], BF16)
            lg = sb(st, "lg", [128, 72], F32)
            r8 = sb(st, "r8", [128, 8], F32); r1 = sb(st, "r1", [128, 8], F32)
            oh = [sb(st, "oh%d" % i, [128, 64], F32) for i in range(2)]
            msk = sb(st, "msk", [128, 64], F32); Ab = sb(st, "Ab", [128, 64], BF16)
            cnt = sb(st, "cnt", [128, 64], F32); tmp64 = sb(st, "tmp64", [128, 64], F32)
            dst_f = sb(st, "dst_f", [128, 4], F32)
            ss = sb(st, "ss4", [128, 1], F32)
            w_merge_v = w_merge.rearrange("(k p) n -> p k n", p=128)
            w_pc_v = w_proj_conv.rearrange("(k p) n -> p k n", p=128)
            w_pn_v = w_proj_nsa.rearrange("(k p) n -> p k n", p=128)
            w_out_v = w_out.rearrange("(k p) n -> p k n", p=128)
            for hf in range(2):
              h0 = hf * HALF
              with ExitStack() as sa:
                hTh = sb(sa, "hTh%d" % hf, [128, 16, HALF], BF16)
                uTh = sb(sa, "uTh%d" % hf, [128, 8, HALF], BF16)
                oTh = sb(sa, "oTh%d" % hf, [128, 16, HALF], BF16)
                wm = [sb(sa, "wm%d_%d" % (hf, i), [128, 16, 2, 256], BF16) for i in range(2)]
                wpc = [sb(sa, "wpc%d_%d" % (hf, i), [128, 8, 256], BF16) for i in range(2)]
                wpn = [sb(sa, "wpn%d_%d" % (hf, i), [128, 16, 256], BF16) for i in range(2)]
                gcs = sb(sa, "gcs%d" % hf, [128, 512], F32); gns = sb(sa, "gns%d" % hf, [128, 512], F32); tmpm = sb(sa, "tmpm%d" % hf, [128, 512], F32)
                for kc in range(16):
                    Sc.dma("sp", lambda h, kc=kc, h0=h0: h.dma_start(out=hTh[:, kc, :], in_=HT[kc * 128:(kc + 1) * 128, h0:h0 + HALF]), "hTh%d" % kc, reads=["HT"], writes=["hTh%d" % kc])
                    Sc.dma("sp", lambda h, kc=kc, h0=h0: h.dma_start(out=oTh[:, kc, :], in_=OT[kc * 128:(kc + 1) * 128, h0:h0 + HALF]), "oTh%d" % kc, reads=["OT"], writes=["oTh%d" % kc])
                for kc in range(8):
                    Sc.dma("sp", lambda h, kc=kc, h0=h0: h.dma_start(out=uTh[:, kc, :], in_=UT[kc * 128:(kc + 1) * 128, h0:h0 + HALF]), "uTh%d" % kc, reads=["UT"], writes=["uTh%d" % kc])

                def load_w(d8):
                    wbi = d8 % 2
                    c0 = d8 * 256
                    Sc.dma("pool", lambda h: h.dma_start(out=wm[wbi][:, :, 0, :], in_=w_merge_v[:, :, c0:c0 + 256]), "wm%d" % wbi, writes=["wm%d" % wbi])
                    Sc.dma("pool", lambda h: h.dma_start(out=wm[wbi][:, :, 1, :], in_=w_merge_v[:, :, D + c0:D + c0 + 256]), "wm%d" % wbi, writes=["wm%d" % wbi])
                    Sc.dma("pool", lambda h: h.dma_start(out=wpc[wbi][:], in_=w_pc_v[:, :, c0:c0 + 256]), "wpc%d" % wbi, writes=["wpc%d" % wbi])
                    Sc.dma("pool", lambda h: h.dma_start(out=wpn[wbi][:], in_=w_pn_v[:, :, c0:c0 + 256]), "wpn%d" % wbi, writes=["wpn%d" % wbi])

                def stage_a(d8, db, t2):
                    wbi = d8 % 2
                    dblk = d8 * 2 + db
                    csl = slice(db * 128, (db + 1) * 128)
                    tsl = slice(t2 * 512, (t2 + 1) * 512)
                    kwm = "wm%d" % wbi; kpc = "wpc%d" % wbi; kpn = "wpn%d" % wbi
                    for k in range(16):
                        Sc.add("pe", lambda h, k=k: h.matmul(OA0[:, :], lhsT=wm[wbi][:, k, 0, csl], rhs=hTh[:, k, tsl], start=(k == 0), stop=(k == 15)), reads=[kwm, "hTh%d" % k], writes=["OA"])
                    for k in range(16):
                        Sc.add("pe", lambda h, k=k: h.matmul(OB0[:, :], lhsT=wm[wbi][:, k, 1, csl], rhs=hTh[:, k, tsl], start=(k == 0), stop=(k == 15)), reads=[kwm, "hTh%d" % k], writes=["OB"])
                    for k in range(8):
                        Sc.add("pe", lambda h, k=k: h.matmul(B0[:, :], lhsT=wpc[wbi][:, k, csl], rhs=uTh[:, k, tsl], start=(k == 0), stop=(k == 7)), reads=[kpc, "uTh%d" % k], writes=["B0"])
                    for k in range(16):
                        Sc.add("pe", lambda h, k=k: h.matmul(B1[:, :], lhsT=wpn[wbi][:, k, csl], rhs=oTh[:, k, tsl], start=(k == 0), stop=(k == 15)), reads=[kpn, "oTh%d" % k], writes=["B1"])
                    Sc.add("act", lambda h: h.activation(out=gcs[:], in_=OA0[:, :], func=AF.Sigmoid, bias=bmT[:, dblk:dblk + 1]), reads=["OA", "bmT"], writes=["gcs"])
                    Sc.add("act", lambda h: h.activation(out=gns[:], in_=OB0[:, :], func=AF.Sigmoid, bias=bmT[:, 16 + dblk:17 + dblk]), reads=["OB", "bmT"], writes=["gns"])
                    Sc.add("dve", lambda h: h.tensor_tensor(out=tmpm[:], in0=gcs[:], in1=B0[:, :], op=ALU.mult), reads=["gcs", "B0"], writes=["tmpm"])
                    Sc.add("dve", lambda h: h.tensor_tensor(out=gns[:], in0=gns[:], in1=B1[:, :], op=ALU.mult), reads=["gns", "B1"], writes=["gns"])
                    Sc.add("dve", lambda h: h.tensor_tensor(out=mT[:, dblk, tsl], in0=tmpm[:], in1=gns[:], op=ALU.add), reads=["tmpm", "gns"], writes=["mT"])

                load_w(0)
                for d8 in range(8):
                    if d8 + 1 < 8:
                        load_w(d8 + 1)
                    bg(1)
                    for db in range(2):
                        for t2 in range(2):
                            stage_a(d8, db, t2)
                Sc.flush()
              with ExitStack() as sk:
                wo4 = sb(sk, "wo4_%d" % hf, [128, 4, 16, 512], BF16)
                xin2 = [sb(sk, "xin%d_%d" % (hf, i), [128, D], F32) for i in range(2)]
                x1t2 = [sb(sk, "x1t%d_%d" % (hf, i), [128, D], F32) for i in range(2)]
                xnb = [sb(sk, "xnb%d_%d" % (i, hf), [128, D], BF16) for i in range(2)]
                junk = sb(sk, "junk4_%d" % hf, [128, D], BF16)
                xnT = sb(sk, "xnT%d" % hf, [128, 16, 128], BF16)
                for d4 in range(4):
                    Sc.dma("pool", lambda h, d4=d4: h.dma_start(out=wo4[:, d4, :, :], in_=w_out_v[:, :, d4 * 512:(d4 + 1) * 512]), "wo4_%d" % d4, writes=["wo4_%d" % d4])
                def sb_mm(t8):
                    tt = hf * 8 + t8
                    tsl = slice(t8 * 128, (t8 + 1) * 128)
                    xin = xin2[t8 % 2]; x1t = x1t2[t8 % 2]; kxin = "xin%d" % (t8 % 2); kx1t = "x1t%d" % (t8 % 2)
                    Sc.dma("sp", lambda h, tt=tt, xin=xin: h.dma_start(out=xin[:], in_=x[tt * 128:(tt + 1) * 128, :]), kxin, writes=[kxin])
                    for d4 in range(4):
                        dsl = slice(d4 * 512, (d4 + 1) * 512)
                        bank = [B0, B1, OA0, OB0][d4]; bkey = ["B0", "B1", "OA", "OB"][d4]
                        for k in range(16):
                            Sc.add("pe", lambda h, k=k, tsl=tsl, bank=bank, d4=d4: h.matmul(bank[:, :], lhsT=mT[:, k, tsl], rhs=wo4[:, d4, k, :], start=(k == 0), stop=(k == 15)), reads=["mT", "wo4_%d" % d4], writes=[bkey])
                        Sc.add("dve", lambda h, dsl=dsl, bank=bank, x1t=x1t, xin=xin: h.tensor_tensor(out=x1t[:, dsl], in0=bank[:, :], in1=xin[:, dsl], op=ALU.add), reads=[bkey, kxin], writes=[kx1t])

                def sb_post(t8):
                    tt = hf * 8 + t8
                    tsl = slice(t8 * 128, (t8 + 1) * 128)
                    xin = xin2[t8 % 2]; x1t = x1t2[t8 % 2]; kxin = "xin%d" % (t8 % 2); kx1t = "x1t%d" % (t8 % 2)
                    Sc.dma("sp", lambda h, tt=tt, x1t=x1t: h.dma_start(out=X1[tt * 128:(tt + 1) * 128, :], in_=x1t[:]), "X1w%d" % (t8 % 2), reads=[kx1t], writes=["X1"])
                    s = tt % 2
                    Sc.add("act", lambda h, x1t=x1t: h.activation(out=junk[:], in_=x1t[:], func=AF.Square, accum_out=ss[:]), reads=[kx1t], writes=["junk4", "ss4"])
                    Sc.add("act", lambda h: h.activation(out=ss[:], in_=ss[:], func=AF.Sqrt, scale=1.0 / D, bias=EPS), reads=["ss4"], writes=["ss4"])
                    Sc.add("dve", lambda h: h.reciprocal(out=ss[:], in_=ss[:]), reads=["ss4"], writes=["ss4"])
                    Sc.add("dve", lambda h, s=s, x1t=x1t: h.scalar_tensor_tensor(out=xnb[s][:], in0=x1t[:], scalar=ss[:, 0:1], in1=fng[:], op0=ALU.mult, op1=ALU.mult), reads=[kx1t, "ss4", "fng"], writes=["xnb%d" % s])
                    for c4 in range(4):
                        for j in range(4):
                            c = 4 * c4 + j
                            Sc.add("pe", lambda h, s=s, c=c, j=j, c4=c4: h.transpose(out=TBS[c4 % 2][:, j, :], in_=xnb[s][:, c * 128:(c + 1) * 128], identity=identb[:]), reads=["xnb%d" % s, "identb"], writes=[["TB", "TB2"][c4 % 2]])
                        Sc.add("act", lambda h, c4=c4: h.activation(out=xnT[:, 4 * c4:4 * c4 + 4, :], in_=TBS[c4 % 2][:, 0:4, :], func=AF.Copy), reads=[["TB", "TB2"][c4 % 2]], writes=["xnT"])
                    for k in range(16):
                        Sc.add("pe", lambda h, k=k: h.matmul(MISC[:, 0:72], lhsT=xnT[:, k, :], rhs=wgr[:, k, :], start=(k == 0), stop=(k == 15)), reads=["xnT", "wgr"], writes=["MISC"])
                    Sc.add("dve", lambda h: h.tensor_tensor(out=lg[:], in0=MISC[:, 0:72], in1=bgr[:], op=ALU.add), reads=["MISC", "bgr"], writes=["lg"])
                    Sc.add("dve", lambda h: h.max(out=r8[:], in_=lg[:, 0:8]), reads=["lg"], writes=["r8"])
                    Sc.add("dve", lambda h: h.tensor_scalar(out=r1[:, 0:1], in0=r8[:, 0:1], scalar1=-1.0, scalar2=None, op0=ALU.mult), reads=["r8"], writes=["r1a"])
                    Sc.add("act", lambda h: h.activation(out=r1[:, 0:8], in_=lg[:, 0:8], func=AF.Exp, bias=r1[:, 0:1], accum_out=r1[:, 1:2]) if False else h.activation(out=tmp64[:, 0:8], in_=lg[:, 0:8], func=AF.Exp, bias=r1[:, 0:1], accum_out=r1[:, 1:2]),
                           reads=["lg", "r1a"], writes=["tmp64", "r1b"])
                    Sc.add("dve", lambda h: h.reciprocal(out=r1[:, 2:3], in_=r1[:, 1:2]), reads=["r1b"], writes=["r1c"])
                    Sc.add("dve", lambda h: h.tensor_scalar(out=tmp64[:, 8:16], in0=lg[:, 0:8], scalar1=r8[:, 0:1], scalar2=None, op0=ALU.is_ge), reads=["lg", "r8", "tmp64"], writes=["tmp64"])
                    Sc.add("dve", lambda h: h.tensor_scalar(out=tmp64[:, 8:16], in0=tmp64[:, 8:16], scalar1=-1.0, scalar2=1e9, op0=ALU.add, op1=ALU.mult), reads=["tmp64"], writes=["tmp64"])
                    Sc.add("dve", lambda h: h.tensor_tensor(out=msk[:, :].rearrange("p (g e) -> p g e", e=8), in0=lg[:, 8:72].rearrange("p (g e) -> p g e", e=8),
                                                            in1=tmp64[:, 8:16].unsqueeze(2).to_broadcast([128, 8, 8]), op=ALU.add), reads=["lg", "tmp64"], writes=["msk"])
                    Sc.add("dve", lambda h: h.max(out=r8[:], in_=msk[:]), reads=["msk", "r8"], writes=["r8"])
                    Sc.add("dve", lambda h: h.tensor_scalar(out=oh[0][:], in0=msk[:], scalar1=r8[:, 0:1], scalar2=None, op0=ALU.is_equal), reads=["msk", "r8"], writes=["oh0"])
                    Sc.add("dve", lambda h: h.tensor_scalar(out=oh[1][:], in0=msk[:], scalar1=r8[:, 1:2], scalar2=None, op0=ALU.is_equal), reads=["msk", "r8"], writes=["oh1"])
                    Sc.add("dve", lambda h: h.tensor_tensor(out=r1[:, 3:4], in0=r8[:, 0:1], in1=r8[:, 1:2], op=ALU.subtract), reads=["r8"], writes=["r1d"])
                    Sc.add("act", lambda h: h.activation(out=r1[:, 4:5], in_=r1[:, 3:4], func=AF.Sigmoid), reads=["r1d"], writes=["r1e"])
                    Sc.add("dve", lambda h, tt=tt: h.tensor_tensor(out=gate2[:, tt, 0:1], in0=r1[:, 4:5], in1=r1[:, 2:3], op=ALU.mult), reads=["r1e", "r1c"], writes=["gate2"])
                    Sc.add("dve", lambda h, tt=tt: h.tensor_tensor(out=gate2[:, tt, 1:2], in0=r1[:, 2:3], in1=gate2[:, tt, 0:1], op=ALU.subtract), reads=["r1c", "gate2"], writes=["gate2"])
                    Sc.add("dve", lambda h: h.tensor_tensor(out=Ab[:], in0=oh[0][:], in1=oh[1][:], op=ALU.add), reads=["oh0", "oh1"], writes=["Ab"])
                    Sc.add("pe", lambda h: h.matmul(MISC[:, 128:192], lhsT=upper[:, :], rhs=Ab[:], start=True, stop=True), reads=["upper", "Ab"], writes=["MISC"])
                    Sc.add("pe", lambda h: h.matmul(MISC[:, 192:256], lhsT=onesb[:, :], rhs=Ab[:], start=True, stop=True), reads=["onesb", "Ab"], writes=["MISC"])
                    Sc.add("dve", lambda h: h.tensor_tensor(out=cnt[:], in0=MISC[:, 128:192], in1=base[:], op=ALU.add), reads=["MISC", "base"], writes=["cnt"])
                    Sc.add("dve", lambda h: h.tensor_tensor(out=base[:], in0=MISC[:, 192:256], in1=base[:], op=ALU.add), reads=["MISC", "base"], writes=["base"])
                    for kk in range(2):
                        Sc.add("dve", lambda h, kk=kk: h.tensor_tensor(out=tmp64[:], in0=oh[kk][:], in1=cnt[:], op=ALU.mult), reads=["oh%d" % kk, "cnt"], writes=["tmp64"])
                        Sc.add("dve", lambda h, kk=kk: h.reduce_sum(out=dst_f[:, kk:kk + 1], in_=tmp64[:], axis=mybir.AxisListType.X), reads=["tmp64"], writes=["dst_f"])
                        Sc.add("dve", lambda h, kk=kk: h.tensor_tensor(out=tmp64[:], in0=oh[kk][:], in1=eoff[:], op=ALU.mult), reads=["oh%d" % kk, "eoff"], writes=["tmp64"])
                        Sc.add("dve", lambda h, kk=kk: h.reduce_sum(out=dst_f[:, 2 + kk:3 + kk], in_=tmp64[:], axis=mybir.AxisListType.X), reads=["tmp64"], writes=["dst_f"])
                        Sc.add("dve", lambda h, kk=kk: h.tensor_scalar(out=tmp64[:, 0:1], in0=dst_f[:, kk:kk + 1], scalar1=float(CAP), scalar2=1e6, op0=ALU.is_ge, op1=ALU.mult), reads=["dst_f"], writes=["tmp64"])
                        Sc.add("dve", lambda h, kk=kk: h.tensor_tensor(out=dst_f[:, kk:kk + 1], in0=dst_f[:, kk:kk + 1], in1=dst_f[:, 2 + kk:3 + kk], op=ALU.add), reads=["dst_f"], writes=["dst_f"])
                        Sc.add("dve", lambda h, kk=kk: h.tensor_tensor(out=dst_f[:, kk:kk + 1], in0=dst_f[:, kk:kk + 1], in1=tmp64[:, 0:1], op=ALU.add), reads=["dst_f", "tmp64"], writes=["dst_f"])
                    Sc.add("dve", lambda h, tt=tt: h.tensor_copy(out=dest_i[:, tt, :], in_=dst_f[:, 0:2]), reads=["dst_f"], writes=["dest_i"])
                    for kk in range(2):
                        Sc.dma("pool", lambda h, tt=tt, kk=kk, s=s: h.indirect_dma_start(out=XS[:, :], out_offset=bass.IndirectOffsetOnAxis(ap=dest_i[:, tt, kk:kk + 1], axis=0), in_=xnb[s][:, :], in_offset=None,
                                                                                  bounds_check=Sc.bound_reg(h, NSLOT - 1), oob_is_err=False), "xs_sc%d" % s, reads=["xnb%d" % s, "dest_i"], writes=["XS"])
                    if dbg:
                        Sc.add("dve", lambda h, tt=tt: h.tensor_copy(out=dst_f[:, 2:4], in_=gate2[:, tt, :]), reads=["gate2", "dst_f"], writes=["dst_f"])
                        Sc.dma("sp", lambda h, tt=tt: h.dma_start(out=RT[tt * 128:(tt + 1) * 128, :], in_=dst_f[:]), "RTw", reads=["dst_f"], writes=["RT"])

                sb_mm(0)
                for t8 in range(8):
                    if t8 + 1 < 8:
                        sb_mm(t8 + 1)
                    sb_post(t8)
                    if t8 % 2 == 1:
                        bg(1)
                if hf == 1:
                    bg(len(bg_list))
                Sc.flush()
        if stop_after == "P4":
            Sc.finish(); return nc

        with ExitStack() as st:
            B0 = ps(st, "B0_5", [128, 512], F32); B1 = ps(st, "B1_5", [128, 512], F32)
            OA0 = ps(st, "OA0_5", [128, 512], F32); OA1 = ps(st, "OA1_5", [128, 512], F32)
            OB0 = ps(st, "OB0_5", [128, 512], F32); OB1 = ps(st, "OB1_5", [128, 512], F32)
            TB = ps(st, "TB_5", [128, 8, 128], BF16); TB2 = ps(st, "TB2_5", [128, 8, 128], BF16)
            TBS = [TB, TB2]
            ew1 = [sb(st, "ew1_%d" % i, [128, 16, FF], BF16) for i in range(2)]
            ew3 = [sb(st, "ew3_%d" % i, [128, 16, FF], BF16) for i in range(2)]
            ew2 = [sb(st, "ew2_%d" % i, [128, 4, D], BF16) for i in range(2)]
            xe = [sb(st, "xe%d" % i, [128, 2, D], BF16) for i in range(2)]
            xeT = sb(st, "xeT", [128, 16, 256], BF16)
            sg = sb(st, "sg", [128, 4, 256], F32)
            hTe = sb(st, "hTe", [128, 4, 256], BF16)
            ye = [sb(st, "ye%d" % i, [128, D], BF16) for i in range(4)]
            Av = [B0[:, :].rearrange("p (a b) -> p a b", b=256), B1[:, :].rearrange("p (a b) -> p a b", b=256)]
            Bv = [OA0[:, :].rearrange("p (a b) -> p a b", b=256), OA1[:, :].rearrange("p (a b) -> p a b", b=256)]
            akey = ["B0", "B1"]; bkeys = ["OA", "OA1"]
            ycount = [0]
            xeT2 = [xeT, sb(st, "xeTb", [128, 16, 256], BF16)]

            def e_ld13(e):
                s = e % 2
                if e in pre_idx:
                    j = pre_idx[e]
                    Sc.dma("pool", lambda h: h.dma_start(out=ew1[s][:], in_=EB1[j].rearrange("p (k n) -> p k n", n=FF)), "ew1_%d" % s, reads=["EB%d_0" % e], writes=["ew1_%d" % s])
                    Sc.dma("pool", lambda h: h.dma_start(out=ew3[s][:], in_=EB3[j].rearrange("p (k n) -> p k n", n=FF)), "ew3_%d" % s, reads=["EB%d_1" % e], writes=["ew3_%d" % s])
                else:
                    Sc.dma("pool", lambda h: h.dma_start(out=ew1[s][:], in_=exp_w1[e].rearrange("(k p) n -> p k n", p=128)), "ew1_%d" % s, writes=["ew1_%d" % s])
                    Sc.dma("pool", lambda h: h.dma_start(out=ew3[s][:], in_=exp_w3[e].rearrange("(k p) n -> p k n", p=128)), "ew3_%d" % s, writes=["ew3_%d" % s])

            def e_ld2(e):
                s = e % 2
                if e in pre_idx:
                    j = pre_idx[e]
                    Sc.dma("pool", lambda h: h.dma_start(out=ew2[s][:], in_=EB2[j].rearrange("p (k n) -> p k n", n=D)), "ew2_%d" % s, reads=["EB%d_2" % e], writes=["ew2_%d" % s])
                else:
                    Sc.dma("pool", lambda h: h.dma_start(out=ew2[s][:], in_=exp_w2[e].rearrange("(k p) n -> p k n", p=128)), "ew2_%d" % s, writes=["ew2_%d" % s])

            def e_ldx(e):
                s = e % 2
                Sc.dma("sp", lambda h: h.dma_start(out=xe[s][:, 0, :], in_=XS[e * CAP:e * CAP + 128, :]), "xe%da" % s, reads=["XS"], writes=["xe%da" % s])
                Sc.dma("sp", lambda h: h.dma_start(out=xe[s][0:R2, 1, :], in_=XS[e * CAP + 128:(e + 1) * CAP, :]), "xe%db" % s, reads=["XS"], writes=["xe%db" % s])

            def e_tr(e):
                s = e % 2
                xt = xeT2[s]; kx = "xeT%d" % s
                tcount = 0
                for a2 in range(2):
                    for c4 in range(4):
                        tb = tcount % 2; tcount += 1
                        for j in range(4):
                            c = 4 * c4 + j
                            nr = 128 if a2 == 0 else R2
                            Sc.add("pe", lambda h, c=c, j=j, tb=tb, a2=a2, nr=nr: h.transpose(out=TBS[tb][:, j, 0:nr], in_=xe[s][0:nr, a2, c * 128:(c + 1) * 128], identity=identb[0:nr, 0:nr]), reads=["xe%d%s" % (s, "ab"[a2]), "identb"], writes=[["TB", "TB2"][tb]])
                        nr = 128 if a2 == 0 else R2
                        if tb == 0:
                            Sc.add("act", lambda h, c4=c4, a2=a2, nr=nr: h.activation(out=xt[:, 4 * c4:4 * c4 + 4, a2 * 128:a2 * 128 + nr], in_=TB[:, 0:4, 0:nr], func=AF.Copy), reads=["TB"], writes=[kx])
                        else:
                            Sc.add("dve", lambda h, c4=c4, a2=a2, nr=nr: h.tensor_copy(out=xt[:, 4 * c4:4 * c4 + 4, a2 * 128:a2 * 128 + nr], in_=TB2[:, 0:4, 0:nr]), reads=["TB2"], writes=[kx])

            def e_s1(e):
                s = e % 2
                xt = xeT2[s]; kx = "xeT%d" % s
                for fb in range(4):
                    for k in range(16):
                        Sc.add("pe", lambda h, fb=fb, k=k: h.matmul(Av[fb // 2][:, fb % 2, 0:CAP], lhsT=ew1[s][:, k, fb * 128:(fb + 1) * 128], rhs=xt[:, k, 0:CAP], start=(k == 0), stop=(k == 15)), reads=["ew1_%d" % s, kx], writes=[akey[fb // 2]])
                for fb in range(4):
                    for k in range(16):
                        Sc.add("pe", lambda h, fb=fb, k=k: h.matmul(Bv[fb // 2][:, fb % 2, 0:CAP], lhsT=ew3[s][:, k, fb * 128:(fb + 1) * 128], rhs=xt[:, k, 0:CAP], start=(k == 0), stop=(k == 15)), reads=["ew3_%d" % s, kx], writes=[bkeys[fb // 2]])
                for hb in range(2):
                    Sc.add("act", lambda h, hb=hb: h.activation(out=sg[:, 2 * hb:2 * hb + 2, 0:CAP], in_=Av[hb][:, :, 0:CAP], func=AF.Silu), reads=[akey[hb]], writes=["sg%d" % hb])
                    Sc.add("dve", lambda h, hb=hb: h.tensor_tensor(out=hTe[:, 2 * hb:2 * hb + 2, 0:CAP], in0=sg[:, 2 * hb:2 * hb + 2, 0:CAP], in1=Bv[hb][:, :, 0:CAP], op=ALU.mult), reads=["sg%d" % hb, bkeys[hb]], writes=["hTe"])

            def e_s2(e):
                s = e % 2
                for a2 in range(2):
                    yb = ye[(e % 2) * 2 + a2]; ykey = "ye%d" % ((e % 2) * 2 + a2)
                    nr = 128 if a2 == 0 else R2
                    for d4 in range(4):
                        bank = [OB0, OB1][ycount[0] % 2]; bkey = ["OB", "OB1"][ycount[0] % 2]; ycount[0] += 1
                        for fb in range(4):
                            Sc.add("pe", lambda h, fb=fb, d4=d4, bank=bank, a2=a2, nr=nr: h.matmul(bank[0:nr, :], lhsT=hTe[:, fb, a2 * 128:a2 * 128 + nr], rhs=ew2[s][:, fb, d4 * 512:(d4 + 1) * 512], start=(fb == 0), stop=(fb == 3)), reads=["hTe", "ew2_%d" % s], writes=[bkey])
                        if d4 % 2 == 0:
                            Sc.add("act", lambda h, d4=d4, bank=bank, yb=yb, nr=nr: h.activation(out=yb[0:nr, d4 * 512:(d4 + 1) * 512], in_=bank[0:nr, :], func=AF.Copy), reads=[bkey], writes=[ykey])
                        else:
                            Sc.add("dve", lambda h, d4=d4, bank=bank, yb=yb, nr=nr: h.tensor_copy(out=yb[0:nr, d4 * 512:(d4 + 1) * 512], in_=bank[0:nr, :]), reads=[bkey], writes=[ykey])
                    Sc.dma("sp", lambda h, a2=a2, yb=yb, nr=nr: h.dma_start(out=YS[e * CAP + a2 * 128:e * CAP + a2 * 128 + nr, :], in_=yb[0:nr, :]), "w" + ykey, reads=[ykey], writes=["YS"])

            for e0 in range(min(2, nexp)):
                e_ld13(e0); e_ld2(e0); e_ldx(e0)
            e_tr(0)
            for e in range(nexp):
                e_s1(e)
                if e + 2 < nexp:
                    e_ld13(e + 2)
                if e + 1 < nexp:
                    e_tr(e + 1)
                if e + 2 < nexp:
                    e_ldx(e + 2)
                e_s2(e)
                if e + 2 < nexp:
                    e_ld2(e + 2)
            Sc.flush()
        if stop_after == "P5":
            Sc.finish(); return nc

        with ExitStack() as st:
            fin = sb(st, "fin", [128, D], F32)
            Sc.dma("sp", lambda h: h.dma_start(out=fin[:], in_=final_norm[0:1, :].broadcast_to([128, D])), "fin", writes=["fin"])
            x1b = [sb(st, "x1b%d" % i, [128, D], F32) for i in range(4)]
            y0 = [sb(st, "y0_%d" % i, [128, D], BF16) for i in range(4)]
            y1 = [sb(st, "y1_%d" % i, [128, D], BF16) for i in range(4)]
            junk = sb(st, "junk6", [128, D], BF16)
            ss = sb(st, "ss6", [128, 1], F32)
            for tt in range(NT):
                s = tt % 4
                Sc.dma("sp", lambda h, tt=tt, s=s: h.dma_start(out=x1b[s][:], in_=X1[tt * 128:(tt + 1) * 128, :]), "x1b%d" % s, reads=["X1"], writes=["x1b%d" % s])
                if tt < 4:
                    Sc.add("pool", lambda h, s=s: h.memset(y0[s][:], 0.0), writes=["y0_%d" % s])
                    Sc.add("pool", lambda h, s=s: h.memset(y1[s][:], 0.0), writes=["y1_%d" % s])
                for kk, yy in enumerate((y0, y1)):
                    Sc.dma("pool", lambda h, tt=tt, kk=kk, s=s, yy=yy: h.indirect_dma_start(out=yy[s][:, :], out_offset=None, in_=YS[:, :], in_offset=bass.IndirectOffsetOnAxis(ap=dest_i[:, tt, kk:kk + 1], axis=0),
                                                                                       bounds_check=Sc.bound_reg(h, NSLOT - 1), oob_is_err=False), "yg%d_%d" % (kk, s), reads=["YS", "dest_i"], writes=["y%d_%d" % (kk, s)])
                Sc.add("dve", lambda h, tt=tt, s=s: h.scalar_tensor_tensor(out=x1b[s][:], in0=y0[s][:], scalar=gate2[:, tt, 0:1], in1=x1b[s][:], op0=ALU.mult, op1=ALU.add), reads=["y0_%d" % s, "gate2", "x1b%d" % s], writes=["x1b%d" % s])
                Sc.add("dve", lambda h, tt=tt, s=s: h.scalar_tensor_tensor(out=x1b[s][:], in0=y1[s][:], scalar=gate2[:, tt, 1:2], in1=x1b[s][:], op0=ALU.mult, op1=ALU.add), reads=["y1_%d" % s, "gate2", "x1b%d" % s], writes=["x1b%d" % s])
                Sc.add("act", lambda h, s=s: h.activation(out=junk[:], in_=x1b[s][:], func=AF.Square, accum_out=ss[:]), reads=["x1b%d" % s], writes=["junk6", "ss6"])
                Sc.add("act", lambda h: h.activation(out=ss[:], in_=ss[:], func=AF.Sqrt, scale=1.0 / D, bias=EPS), reads=["ss6"], writes=["ss6"])
                Sc.add("dve", lambda h: h.reciprocal(out=ss[:], in_=ss[:]), reads=["ss6"], writes=["ss6"])
                Sc.add("dve", lambda h, s=s: h.scalar_tensor_tensor(out=x1b[s][:], in0=x1b[s][:], scalar=ss[:, 0:1], in1=fin[:], op0=ALU.mult, op1=ALU.mult), reads=["x1b%d" % s, "ss6", "fin"], writes=["x1b%d" % s])
                Sc.dma("sp", lambda h, tt=tt, s=s: h.dma_start(out=out[tt * 128:(tt + 1) * 128, :], in_=x1b[s][:]), "outw%d" % s, reads=["x1b%d" % s], writes=["out"])
            Sc.flush()
        Sc.finish()
    return nc


def make_in_maps(inputs, cores):
    c = _consts()
    g = lambda k: np.ascontiguousarray(np.asarray(inputs[k], dtype=np.float32))
    shared = {
        "attn_norm": g("attn_norm")[0].reshape(16, 128),
        "w_in": g("w_in")[0],
        "conv_dw": g("conv_dw")[0],
        "conv_dw_b": g("conv_dw_b")[0].reshape(8, 128),
        "conv_ln_g": g("conv_ln_g")[0].reshape(8, 128),
        "conv_ln_b": g("conv_ln_b")[0].reshape(8, 128),
        "cmp_pos_k": g("cmp_pos_k")[0], "cmp_pos_v": g("cmp_pos_v")[0],
        "cmp_k_w1": g("cmp_k_w1")[0], "cmp_v_w1": g("cmp_v_w1")[0],
        "cmp_k_w2": g("cmp_k_w2")[0], "cmp_v_w2": g("cmp_v_w2")[0],
        "w_proj_conv": g("w_proj_conv")[0], "w_proj_nsa": g("w_proj_nsa")[0],
        "w_merge": g("w_merge")[0], "b_merge": g("b_merge")[0].reshape(32, 128), "w_out": g("w_out")[0],
        "ffn_norm": g("ffn_norm")[0].reshape(1, D),
        "w_gr": np.ascontiguousarray(np.concatenate([g("w_grp")[0], g("w_exp")[0]], axis=1)),
        "b_gr": np.concatenate([g("b_grp")[0], g("b_exp")[0]]).reshape(1, 72),
        "exp_w1": g("exp_w1")[0], "exp_w3": g("exp_w3")[0], "exp_w2": g("exp_w2")[0],
        "final_norm": g("final_norm").reshape(1, D),
    }
    shared.update(c)
    xs = g("x")
    return [dict(shared, x=xs[b]) for b in cores]


def kernel(**inputs):
    nc = build()
    cores = list(range(8))
    in_maps = make_in_maps(inputs, cores)
    res = run_bass_kernel_spmd(nc, in_maps, core_ids=cores)
    return np.stack([res.results[i]["out"] for i in range(8)], axis=0).astype(np.float32)
```

```python
import numpy as np
import ml_dtypes
from contextlib import ExitStack
import concourse.bass as bass
import concourse.mybir as mybir
from concourse.bass_utils import run_bass_kernel_spmd

F32 = mybir.dt.float32
BF16 = mybir.dt.bfloat16
I32 = mybir.dt.int32
AF = mybir.ActivationFunctionType
ALU = mybir.AluOpType

D = 2048
S = 2048
NT = 16
CCH = 1024
TAPS = 31
NH = 16
NG = 4
INC = 7216
NEXP = 64
CAP = 192
R2 = CAP - 128
FF = 512
EPS = 1e-6
SCALE = 128 ** -0.5
NEG = -1.0e5
NSLOT = NEXP * CAP


class Op:
    __slots__ = ("eng", "fn", "deps", "signal", "val", "sem", "is_dma", "phase")


class Sched:
    ENG = ("pe", "act", "dve", "pool", "sp")
    BLK = {"pe": "tensor", "act": "scalar", "dve": "vector", "pool": "gpsimd", "sp": "sync"}
    PSUM_KEYS = {"B0", "B1", "OA", "OB", "OA1", "OB1", "TB", "TB2", "MISC"}

    def __init__(self, nc, stack):
        self.nc = nc
        self.stack = stack
        self.sems = {e: stack.enter_context(nc.semaphore("c_" + e)) for e in self.ENG}
        self.count = {e: 0 for e in self.ENG}
        self.ops = {e: [] for e in self.ENG}
        self.lastw = {}
        self.readers = {}
        self.dsems = {}
        self.dcount = {}
        self.waited = {e: {} for e in self.ENG}
        self.phase = 0
        self.phase_last = {}
        self.barrier = {e: [] for e in self.ENG}
        self.phase_map = {}

    def _mk(self, eng, fn, reads, writes):
        op = Op()
        op.eng = eng; op.fn = fn; op.signal = False; op.val = None; op.sem = None
        op.is_dma = False; op.phase = self.phase
        deps = []
        for k in reads:
            w = self.lastw.get(k)
            if w is not None:
                deps.append(w)
            if k in self.PSUM_KEYS:
                deps.extend(r for r in self.readers.get(k, []) if r.eng != eng)
            self.readers.setdefault(k, []).append(op)
        for k in writes:
            w = self.lastw.get(k)
            if w is not None:
                deps.append(w)
            deps.extend(self.readers.get(k, []))
            self.lastw[k] = op
            self.readers[k] = []
        deps.extend(self.barrier[eng])
        self.barrier[eng] = []
        out = []
        seen = set()
        for d in deps:
            if d is op:
                continue
            if (not d.is_dma) and d.phase < self.phase:
                d = self.phase_last[d.phase][d.eng]
            if id(d) in seen:
                continue
            seen.add(id(d))
            if (not d.is_dma) and d.eng == eng and eng == "pe":
                continue
            d.signal = True
            out.append(d)
        op.deps = out
        self.ops[eng].append(op)
        return op

    def add(self, eng, fn, reads=(), writes=()):
        return self._mk(eng, fn, reads, writes)

    def dma(self, eng, fn, sem, reads=(), writes=()):
        op = self._mk(eng, fn, reads, writes)
        op.is_dma = True
        pm = self.phase_map.setdefault(eng, {})
        sem = "g%s%d" % (eng, pm.setdefault(sem, len(pm)))
        if sem not in self.dsems:
            self.dsems[sem] = self.stack.enter_context(self.nc.semaphore("d_" + sem))
            self.dcount[sem] = 0
        self.dcount[sem] += 16
        op.sem = sem
        op.val = self.dcount[sem]
        return op

    def flush(self):
        nc = self.nc
        last = {}
        for e in self.ENG:
            comp = [o for o in self.ops[e] if not o.is_dma]
            if comp:
                comp[-1].signal = True
                last[e] = comp[-1]
        self.phase_last[self.phase] = last
        for e in self.ENG:
            c = self.count[e]
            for o in self.ops[e]:
                if not o.is_dma and o.signal:
                    c += 1
                    o.val = c
            self.count[e] = c
        ops_by_eng = {e: list(self.ops[e]) for e in self.ENG}
        outstanding_dma = [o for e in self.ENG for o in self.ops[e] if o.is_dma]
        with nc.Block() as block:
            for e in self.ENG:
                ops = ops_by_eng[e]
                if not ops:
                    continue

                def runner(h, ops=ops, e=e):
                    waited = self.waited[e]
                    for o in ops:
                        need = {}
                        for d in o.deps:
                            if d.is_dma:
                                s = self.dsems[d.sem]; key = "d_" + d.sem
                            else:
                                s = self.sems[d.eng]; key = "c_" + d.eng
                            if key not in need or need[key][1] < d.val:
                                need[key] = (s, d.val)
                        for key, (s, val) in need.items():
                            if waited.get(key, 0) >= val:
                                continue
                            h.wait_ge(s, val)
                            waited[key] = val
                        ins = o.fn(h)
                        if o.is_dma:
                            ins.then_inc(self.dsems[o.sem], 16)
                        elif o.signal:
                            ins.then_inc(self.sems[o.eng], 1)

                getattr(block, self.BLK[e])(runner)
        bl = list(last.values()) + outstanding_dma
        for e in self.ENG:
            self.barrier[e] = self.barrier[e] + bl
        self.ops = {e: [] for e in self.ENG}
        self.phase_map = {}
        self.phase += 1

    def bound_reg(self, h, val):
        if getattr(self, "_breg", None) is None:
            self._breg = h.alloc_register("bc")
            h.reg_mov(self._breg, val)
        return self._breg

    def finish(self):
        nc = self.nc
        with nc.Block() as block:
            def runner(h):
                for name, sem in self.dsems.items():
                    if self.dcount[name] > 0:
                        h.wait_ge(sem, self.dcount[name])
                for e in self.ENG:
                    if self.count[e] > 0:
                        h.wait_ge(self.sems[e], self.count[e])
            block.sync(runner)


def _consts():
    c = {}
    pos = np.arange(S, dtype=np.float32)
    inv = (500000.0 ** (-np.arange(0, 32, 2, dtype=np.float32) / 32)).astype(np.float32)
    ang = pos[:, None] * inv[None, :]
    c["rope_cs"] = np.concatenate([np.cos(ang), np.sin(ang)], axis=1).astype(np.float32)
    a = np.zeros((128, 32), np.float32)
    j = np.arange(32)
    for m in range(4):
        for n in range(2):
            i = 4 * j + m - n
            ok = (i >= 0) & (i < 127)
            np.add.at(a, (i[ok], j[ok]), 1.0)
    c["cmpA"] = a
    cc = np.arange(128)[:, None, None]; ii = np.arange(16)[None, :, None]; qq = np.arange(128)[None, None, :]
    c["cmpmask"] = np.where(16 * cc + 31 <= 128 * ii + qq, 0.0, NEG).astype(np.float32).reshape(128, 16 * 128)
    k = np.arange(128)[:, None]; q = np.arange(128)[None, :]
    tri = np.stack([np.where(k <= q, 0.0, NEG), np.where(k > q, 0.0, NEG)], axis=1).astype(np.float32)
    c["tri"] = tri.reshape(128, 256)
    e = np.zeros((32, 16, 128), np.float32)
    for jj in range(16):
        e[2 * jj, jj, :64] = 1.0
        e[2 * jj + 1, jj, 64:] = 1.0
    c["esel"] = e.reshape(32, 16 * 128)
    addm = np.zeros((128, 8, 32), np.float32)
    for i in range(8, 16):
        t = 128 * i + np.arange(128)[:, None]
        blk = np.arange(32)[None, :]
        cur = t // 64
        forced = (blk == 0) | ((blk <= cur) & (blk > cur - 2))
        avail = blk * 64 <= t
        addm[:, i - 8, :] = np.where(avail, np.where(forced, 1e4, 0.0), -1e30)
    c["addm"] = addm.reshape(128, 256)
    c["identf"] = np.eye(128, dtype=np.float32)
    c["upper"] = (np.arange(128)[:, None] < np.arange(128)[None, :]).astype(np.float32)
    c["eoff"] = np.broadcast_to((np.arange(64, dtype=np.float32) * CAP)[None, :], (128, 64)).copy()
    return c


def build(stop_after=None, nexp=NEXP):
    nc = bass.Bass("TRN2", target_bir_lowering=False)

    def din(name, shape, dt=F32):
        return nc.dram_tensor(name, list(shape), dt, kind="ExternalInput").ap()

    x = din("x", [S, D]); attn_norm = din("attn_norm", [16, 128]); w_in = din("w_in", [D, INC])
    conv_dw = din("conv_dw", [TAPS, CCH]); conv_dw_b = din("conv_dw_b", [8, 128])
    conv_ln_g = din("conv_ln_g", [8, 128]); conv_ln_b = din("conv_ln_b", [8, 128])
    cmp_pos = [din("cmp_pos_k", [32, 128]), din("cmp_pos_v", [32, 128])]
    cmp_w1 = [din("cmp_k_w1", [4096, 256]), din("cmp_v_w1", [4096, 256])]
    cmp_w2 = [din("cmp_k_w2", [256, 128]), din("cmp_v_w2", [256, 128])]
    w_proj_conv = din("w_proj_conv", [CCH, D]); w_proj_nsa = din("w_proj_nsa", [D, D])
    w_merge = din("w_merge", [D, 2 * D]); b_merge = din("b_merge", [32, 128]); w_out = din("w_out", [D, D])
    ffn_norm = din("ffn_norm", [1, D]); w_gr = din("w_gr", [D, 72]); b_gr = din("b_gr", [1, 72])
    exp_w1 = din("exp_w1", [nexp, D, FF]); exp_w3 = din("exp_w3", [nexp, D, FF]); exp_w2 = din("exp_w2", [nexp, FF, D])
    final_norm = din("final_norm", [1, D])
    rope_cs = din("rope_cs", [S, 32]); cmpA = din("cmpA", [128, 32]); cmpmask_d = din("cmpmask", [128, 2048])
    tri_d = din("tri", [128, 256]); esel_d = din("esel", [32, 2048]); addm_d = din("addm", [128, 256])
    identf_d = din("identf", [128, 128]); upper_d = din("upper", [128, 128]); eoff_d = din("eoff", [128, 64])

    dbg = stop_after is not None
    out = nc.dram_tensor("out", [S, D], F32, kind="ExternalOutput").ap()

    dbg_out = {"P1": ["HT"], "P2": ["UT"], "P3": ["OT"], "P4": ["X1", "RT"], "P5": []}.get(stop_after, [])

    def scratch(name, shape, dt):
        kind = "ExternalOutput" if name in dbg_out else "Internal"
        return nc.dram_tensor(name, list(shape), dt, kind=kind).ap()

    HT = scratch("HT", [D, S], BF16)
    UT = scratch("UT", [CCH, S], BF16)
    OT = scratch("OT", [D, S], BF16)
    X1 = scratch("X1", [S, D], F32)
    XS = scratch("XS", [NSLOT, D], BF16)
    YS = scratch("YS", [NSLOT, D], BF16)
    RT = scratch("RT", [S, 4], F32)
    import os as _os
    _skip = tuple(int(v) for v in _os.environ.get("DBG_SKIP", "3,7,11,13,15").split(",") if v != "")
    PRE = [e for e in range(nexp) if e % 16 not in _skip] if nexp == NEXP else []
    pre_idx = {e: j for j, e in enumerate(PRE)}
    npre = max(1, len(PRE))
    EB1 = nc.dram_tensor("EB1", [npre, 128, 16 * FF], BF16).ap()
    EB3 = nc.dram_tensor("EB3", [npre, 128, 16 * FF], BF16).ap()
    EB2 = nc.dram_tensor("EB2", [npre, 128, 4 * D], BF16).ap()
    bg_list = [(e, w) for e in PRE for w in range(3)]
    bg_pos = [0]

    with ExitStack() as top:
        Sc = Sched(nc, top)

        def sb(st, name, shape, dt):
            return st.enter_context(nc.sbuf_tensor("s_" + name, list(shape), dt))

        def ps(st, name, shape, dt):
            return st.enter_context(nc.psum_tensor("p_" + name, list(shape), dt))

        identf = sb(top, "identf", [128, 128], F32)
        identb = sb(top, "identb", [128, 128], BF16)
        onesb = sb(top, "onesb", [128, 128], BF16)
        Sc.dma("sp", lambda h: h.dma_start(out=identf[:], in_=identf_d[:, :]), "c0", writes=["identf"])
        Sc.dma("pool", lambda h: h.dma_start(out=identb[:], in_=identf_d[:, :]), "c1", writes=["identb"])
        Sc.add("pool", lambda h: h.memset(onesb[:], 1.0), writes=["onesb"])

        def bg(n=1):
            for _ in range(n):
                if bg_pos[0] >= len(bg_list):
                    return
                e, w = bg_list[bg_pos[0]]
                bg_pos[0] += 1
                j = pre_idx[e]
                src = [exp_w1, exp_w3, exp_w2][w][e].rearrange("(k p) n -> p k n", p=128)
                dst = [EB1, EB3, EB2][w][j].rearrange("p (k n) -> p k n", n=(D if w == 2 else FF))
                Sc.dma("pool", lambda h, src=src, dst=dst: h.dma_start(out=dst, in_=src), "bg", writes=["EB%d_%d" % (e, w)])

        def load_T(st, name, dram_ap, R, dst_ap, MISC):
            stg = sb(st, "stg_" + name, [32, 128], F32)
            Sc.dma("sp", lambda h: h.dma_start(out=stg[0:R, :], in_=dram_ap), "ld_" + name, writes=["stg_" + name])
            Sc.add("pe", lambda h: h.transpose(out=MISC[:, 0:R], in_=stg[0:R, :], identity=identf[0:R, 0:R]),
                   reads=["stg_" + name, "identf"], writes=["MISC"])
            Sc.add("dve", lambda h: h.tensor_copy(out=dst_ap, in_=MISC[:, 0:R]), reads=["MISC"], writes=[name])

        with ExitStack() as stA:
            hT = sb(stA, "hT", [128, 16, S], BF16)
            with ExitStack() as st:
                TB = ps(st, "TB_1", [128, 8, 128], BF16); TB2 = ps(st, "TB2_1", [128, 8, 128], BF16); MISC = ps(st, "MISC_1", [128, 512], F32)
                TBS = [TB, TB2]
                gT = sb(st, "gT", [128, 16], F32)
                load_T(st, "gT", attn_norm[:, :], 16, gT[:, :], MISC)
                xt = [sb(st, "xt%d" % i, [128, D], F32) for i in range(2)]
                xb = [sb(st, "xb%d" % i, [128, D], BF16) for i in range(2)]
                junk = sb(st, "junk", [128, D], BF16)
                ss = [sb(st, "ss%d" % i, [128, 1], F32) for i in range(2)]
                import os
                for i in range(int(os.environ.get("DBG_NT", NT))):
                    s = i % 2
                    Sc.dma("sp", lambda h, i=i, s=s: h.dma_start(out=xt[s][:], in_=x[i * 128:(i + 1) * 128, :]), "xt%d" % s, writes=["xt%d" % s])
                    Sc.add("act", lambda h, s=s: h.activation(out=junk[:], in_=xt[s][:], func=AF.Square, accum_out=ss[s][:]),
                           reads=["xt%d" % s], writes=["junk", "ss%d" % s])
                    Sc.add("act", lambda h, s=s: h.activation(out=ss[s][:], in_=ss[s][:], func=AF.Sqrt, scale=1.0 / D, bias=EPS),
                           reads=["ss%d" % s], writes=["ss%d" % s])
                    Sc.add("dve", lambda h, s=s: h.reciprocal(out=ss[s][:], in_=ss[s][:]), reads=["ss%d" % s], writes=["ss%d" % s])
                    Sc.add("dve", lambda h, s=s: h.tensor_scalar(out=xb[s][:], in0=xt[s][:], scalar1=ss[s][:, 0:1], scalar2=None, op0=ALU.mult),
                           reads=["xt%d" % s, "ss%d" % s], writes=["xb%d" % s])
                    for c4 in range(4):
                        for j in range(4):
                            c = 4 * c4 + j
                            Sc.add("pe", lambda h, s=s, c=c, j=j, c4=c4: h.transpose(out=TBS[c4 % 2][:, j, :], in_=xb[s][:, c * 128:(c + 1) * 128], identity=identb[:]),
                                   reads=["xb%d" % s, "identb"], writes=[["TB", "TB2"][c4 % 2]])
                        Sc.add("dve", lambda h, i=i, c4=c4: h.tensor_tensor(out=hT[:, 4 * c4:4 * c4 + 4, i * 128:(i + 1) * 128], in0=TBS[c4 % 2][:, 0:4, :],
                                                                   in1=gT[:, 4 * c4:4 * c4 + 4].unsqueeze(2).to_broadcast([128, 4, 128]), op=ALU.mult),
                               reads=[["TB", "TB2"][c4 % 2], "gT"], writes=["hT"])
                for kc in range(16):
                    Sc.dma("sp", lambda h, kc=kc: h.dma_start(out=HT[kc * 128:(kc + 1) * 128, :], in_=hT[:, kc, :]), "HTw%d" % kc, reads=["hT"], writes=["HT%d" % kc])
                Sc.flush()
            if stop_after == "P1":
                Sc.finish(); return nc

            with ExitStack() as st:
                B0 = ps(st, "B0_2", [128, 512], F32); B1 = ps(st, "B1_2", [128, 512], F32); MISC = ps(st, "MISC_2", [128, 512], F32)
                dwT = sb(st, "dwT", [128, 8, 32], F32)
                cb = sb(st, "cb", [128, 3, 8], F32)
                dws = sb(st, "dws", [32, CCH], F32)
                Sc.dma("sp", lambda h: h.dma_start(out=dws[0:TAPS, :], in_=conv_dw[:, :]), "dws", writes=["dws"])
                for cc in range(8):
                    Sc.add("pe", lambda h, cc=cc: h.transpose(out=MISC[:, 0:TAPS], in_=dws[0:TAPS, cc * 128:(cc + 1) * 128], identity=identf[0:TAPS, 0:TAPS]),
                           reads=["dws", "identf"], writes=["MISC"])
                    Sc.add("dve", lambda h, cc=cc: h.tensor_copy(out=dwT[:, cc, 0:TAPS], in_=MISC[:, 0:TAPS]), reads=["MISC"], writes=["dwT"])
                load_T(st, "cb0", conv_dw_b[:, :], 8, cb[:, 0, :], MISC)
                load_T(st, "cb1", conv_ln_g[:, :], 8, cb[:, 1, :], MISC)
                load_T(st, "cb2", conv_ln_b[:, :], 8, cb[:, 2, :], MISC)
                zt = sb(st, "zt", [128, 1, D], BF16)
                Sc.add("pool", lambda h: h.memset(zt[:], 0.0), writes=["zt"])
                for zi in range(NSLOT // 2048):
                    Sc.dma("sp", lambda h, zi=zi: h.dma_start(out=XS[zi * 2048:(zi + 1) * 2048, :].rearrange("(a p) d -> p a d", p=128), in_=zt[:, 0:1, :].to_broadcast([128, 16, D])), "xsz%d" % zi, reads=["zt"], writes=["XSz%d" % zi])
                y_all = sb(st, "y_all", [128, 8, S], BF16)
                w_in_v = w_in.rearrange("(k p) n -> p k n", p=128)
                with ExitStack() as sc:
                    CA = [ps(sc, "CA0_2", [128, 512], F32), ps(sc, "CA1_2", [128, 512], F32)]
                    wa = sb(sc, "wa", [128, 16, 512], BF16); wb = sb(sc, "wb", [128, 16, 512], BF16)
                    glu2 = [sb(sc, "glu%d" % i, [128, 30 + S], BF16) for i in range(2)]
                    diag2 = [sb(sc, "diag%d" % i, [128, TAPS, 128], BF16) for i in range(2)]
                    sig = sb(sc, "sig", [128, 512], F32)
                    for i in range(2):
                        Sc.add("pool", lambda h, i=i: h.memset(glu2[i][:, 0:30], 0.0), writes=["glu%d" % i])

                    def c_proj(cc, tt):
                        c4 = cc % 4
                        glu = glu2[cc % 2]; kg = "glu%d" % (cc % 2)
                        for k in range(16):
                            Sc.add("pe", lambda h, k=k: h.matmul(B0[:, :], lhsT=wa[:, k, c4 * 128:(c4 + 1) * 128], rhs=hT[:, k, tt * 512:(tt + 1) * 512], start=(k == 0), stop=(k == 15)),
                                   reads=["wa", "hT"], writes=["B0"])
                        for k in range(16):
                            Sc.add("pe", lambda h, k=k: h.matmul(B1[:, :], lhsT=wb[:, k, c4 * 128:(c4 + 1) * 128], rhs=hT[:, k, tt * 512:(tt + 1) * 512], start=(k == 0), stop=(k == 15)),
                                   reads=["wb", "hT"], writes=["B1"])
                        Sc.add("act", lambda h: h.activation(out=sig[:], in_=B1[:, :], func=AF.Sigmoid), reads=["B1"], writes=["sig"])
                        Sc.add("dve", lambda h: h.tensor_tensor(out=glu[:, 30 + tt * 512:30 + (tt + 1) * 512], in0=B0[:, :], in1=sig[:], op=ALU.mult),
                               reads=["B0", "sig"], writes=[kg])

                    def c_diag(cc):
                        dg = diag2[cc % 2]
                        for k in range(TAPS):
                            Sc.add("pool", lambda h, k=k: h.tensor_scalar(out=dg[:, k, :], in0=identb[:], scalar1=dwT[:, cc, k:k + 1], scalar2=None, op0=ALU.mult),
                                   reads=["identb", "dwT"], writes=["diag%d" % (cc % 2)])

                    def c_conv(cc, tt):
                        glu = glu2[cc % 2]; kg = "glu%d" % (cc % 2); dg = diag2[cc % 2]
                        bank = CA[tt % 2]; bkey = ["OA", "OA1"][tt % 2]
                        for k in range(TAPS):
                            Sc.add("pe", lambda h, k=k: h.matmul(bank[:, :], lhsT=dg[:, k, :], rhs=glu[:, k + tt * 512:k + (tt + 1) * 512], start=(k == 0), stop=(k == TAPS - 1)),
                                   reads=[kg, "diag%d" % (cc % 2)], writes=[bkey])
                        Sc.add("act", lambda h: h.activation(out=y_all[:, cc, tt * 512:(tt + 1) * 512], in_=bank[:, :], func=AF.Identity, bias=cb[:, 0, cc:cc + 1]),
                               reads=[bkey, "cb0"], writes=["y_all"])

                    def c_loadw(gi):
                        Sc.dma("pool", lambda h: h.dma_start(out=wa[:], in_=w_in_v[:, :, gi * 512:(gi + 1) * 512]), "wa", writes=["wa"])
                        Sc.dma("pool", lambda h: h.dma_start(out=wb[:], in_=w_in_v[:, :, CCH + gi * 512:CCH + (gi + 1) * 512]), "wb", writes=["wb"])

                    c_loadw(0)
                    c_diag(0); c_diag(1)
                    for tt in range(4):
                        c_proj(0, tt)
                    for cc in range(8):
                        if cc + 1 < 8 and (cc + 1) % 4 == 0:
                            c_loadw((cc + 1) // 4)
                            for tt in range(4):
                                c_conv(cc, tt)
                            for tt in range(4):
                                c_proj(cc + 1, tt)
                        else:
                            for tt in range(4):
                                if cc + 1 < 8:
                                    c_proj(cc + 1, tt)
                                c_conv(cc, tt)
                        if cc + 2 < 8:
                            c_diag(cc + 2)
                        bg(2)
                    Sc.flush()
                acc = sb(st, "acc", [128, S], F32)
                mean_b = sb(st, "mean_b", [128, S], F32); rstd_b = sb(st, "rstd_b", [128, S], F32)
                ysq = sb(st, "ysq", [128, 512], BF16); msq = sb(st, "msq", [128, 512], F32)
                for tt in range(4):
                    tsl = slice(tt * 512, (tt + 1) * 512)
                    for cc in range(8):
                        Sc.add("pe", lambda h, cc=cc, tsl=tsl: h.matmul(B0[:, :], lhsT=onesb[:, :], rhs=y_all[:, cc, tsl], start=(cc == 0), stop=(cc == 7)),
                               reads=["y_all", "onesb"], writes=["B0"])
                    for cc in range(8):
                        Sc.add("act", lambda h, cc=cc, tsl=tsl: h.activation(out=ysq[:], in_=y_all[:, cc, tsl], func=AF.Square), reads=["y_all"], writes=["ysq"])
                        Sc.add("pe", lambda h, cc=cc: h.matmul(B1[:, :], lhsT=onesb[:, :], rhs=ysq[:], start=(cc == 0), stop=(cc == 7)),
                               reads=["ysq", "onesb"], writes=["B1"])
                    Sc.add("dve", lambda h, tsl=tsl: h.tensor_scalar(out=mean_b[:, tsl], in0=B0[:, :], scalar1=1.0 / CCH, scalar2=None, op0=ALU.mult), reads=["B0"], writes=["mean_b"])
                    Sc.add("dve", lambda h, tsl=tsl: h.tensor_tensor(out=msq[:], in0=mean_b[:, tsl], in1=mean_b[:, tsl], op=ALU.mult), reads=["mean_b"], writes=["msq"])
                    Sc.add("dve", lambda h, tsl=tsl: h.scalar_tensor_tensor(out=rstd_b[:, tsl], in0=B1[:, :], scalar=1.0 / CCH, in1=msq[:], op0=ALU.mult, op1=ALU.subtract),
                           reads=["B1", "msq"], writes=["rstd_b"])
                    Sc.add("act", lambda h, tsl=tsl: h.activation(out=rstd_b[:, tsl], in_=rstd_b[:, tsl], func=AF.Sqrt, bias=EPS), reads=["rstd_b"], writes=["rstd_b"])
                    Sc.add("dve", lambda h, tsl=tsl: h.reciprocal(out=rstd_b[:, tsl], in_=rstd_b[:, tsl]), reads=["rstd_b"], writes=["rstd_b"])
                ub = [sb(st, "ub%d" % i, [128, S], BF16) for i in range(2)]
                for cc in range(8):
                    s = cc % 2
                    Sc.add("dve", lambda h, cc=cc: h.tensor_tensor(out=acc[:], in0=y_all[:, cc, :], in1=mean_b[:], op=ALU.subtract), reads=["y_all", "mean_b"], writes=["acc"])
                    Sc.add("dve", lambda h: h.tensor_tensor(out=acc[:], in0=acc[:], in1=rstd_b[:], op=ALU.mult), reads=["acc", "rstd_b"], writes=["acc"])
                    Sc.add("act", lambda h, cc=cc, s=s: h.activation(out=ub[s][:], in_=acc[:], func=AF.Silu, scale=cb[:, 1, cc:cc + 1], bias=cb[:, 2, cc:cc + 1]),
                           reads=["acc", "cb1", "cb2"], writes=["ub%d" % s])
                    Sc.dma("sp", lambda h, cc=cc, s=s: h.dma_start(out=UT[cc * 128:(cc + 1) * 128, :], in_=ub[s][:]), "UTw%d" % s, reads=["ub%d" % s], writes=["UT"])
                Sc.flush()
            if stop_after == "P2":
                Sc.finish(); return nc

            with ExitStack() as st:
                B0 = ps(st, "B0_3", [128, 512], F32); B1 = ps(st, "B1_3", [128, 512], F32); MISC = ps(st, "MISC_3", [128, 512], F32)
                OA0 = ps(st, "OA0_3", [128, 512], F32); OA1 = ps(st, "OA1_3", [128, 512], F32)
                OB0 = ps(st, "OB0_3", [128, 512], F32); OB1 = ps(st, "OB1_3", [128, 512], F32)
                TB = ps(st, "TB_3", [128, 8, 128], BF16)
                cs = sb(st, "cs", [128, NT, 32], F32)
                Sc.dma("sp", lambda h: h.dma_start(out=cs[:], in_=rope_cs.rearrange("(i p) c -> p i c", p=128)), "cs", writes=["cs"])
                cmpmask = sb(st, "cmpmask", [128, NT, 128], BF16)
                Sc.dma("pool", lambda h: h.dma_start(out=cmpmask[:], in_=cmpmask_d.rearrange("p (i q) -> p i q", q=128)), "cmpmask", writes=["cmpmask"])
                tri = sb(st, "tri", [128, 2, 128], BF16)
                Sc.dma("pool", lambda h: h.dma_start(out=tri[:], in_=tri_d.rearrange("p (i q) -> p i q", q=128)), "tri", writes=["tri"])
                esel = sb(st, "esel", [32, 16, 128], BF16)
                Sc.dma("pool", lambda h: h.dma_start(out=esel[:], in_=esel_d.rearrange("p (i q) -> p i q", q=128)), "esel", writes=["esel"])
                addm = sb(st, "addm", [128, 8, 32], F32)
                Sc.dma("sp", lambda h: h.dma_start(out=addm[:], in_=addm_d.rearrange("p (i q) -> p i q", q=32)), "addm", writes=["addm"])
                wg = sb(st, "wg", [128, 16, 48], BF16)
                w_in_v = w_in.rearrange("(k p) n -> p k n", p=128)
                Sc.dma("pool", lambda h: h.dma_start(out=wg[:], in_=w_in_v[:, :, 7168:7216]), "wg", writes=["wg"])
                w2 = [sb(st, "w2_%d" % i, [128, 2, 128], BF16) for i in range(2)]
                posT = [sb(st, "posT%d" % i, [128, 32], BF16) for i in range(2)]
                posf2 = [sb(st, "posf%d" % i, [128, 32], F32) for i in range(2)]
                for kv in range(2):
                    Sc.dma("pool", lambda h, kv=kv: h.dma_start(out=w2[kv][:], in_=cmp_w2[kv].rearrange("(b p) d -> p b d", p=128)), "w2_%d" % kv, writes=["w2_%d" % kv])
                    load_T(st, "posf%d" % kv, cmp_pos[kv][:, :], 32, posf2[kv][:, :], MISC)
                    Sc.add("dve", lambda h, kv=kv: h.tensor_copy(out=posT[kv][:], in_=posf2[kv][:]), reads=["posf%d" % kv], writes=["posT%d" % kv])
                gates = sb(st, "gates", [128, NT, 48], F32)
                arena = sb(st, "arena", [128, 20480], BF16)
                wq = arena[:, 0:8192].rearrange("p (k n) -> p k n", n=512)
                wkv = arena[:, 8192:20480].rearrange("p (k s n) -> p k s n", s=6, n=128)
                w1 = [arena[:, 0:8192].rearrange("p (j n) -> p j n", n=256), arena[:, 8192:16384].rearrange("p (j n) -> p j n", n=256)]
                qT = sb(st, "qT", [128, 4, S], BF16)
                kT = sb(st, "kT", [128, 3, S], BF16)
                vcT = sb(st, "vcT", [128, S], BF16)
                vslc = sb(st, "vslc", [128, NT, 132], BF16)
                vwin = sb(st, "vwin", [128, NT, 132], BF16)
                vcx = sb(st, "vcx", [128, 164], BF16)
                kcT = sb(st, "kcT", [128, 128], BF16)
                hid = [sb(st, "hid%d" % i, [128, 2, 128], BF16) for i in range(2)]
                qtok2 = [sb(st, "qtok%d" % i, [128, 4, 128], BF16) for i in range(2)]
                ktok2 = [sb(st, "ktok%d" % i, [128, 4, 128], BF16) for i in range(2)]
                rt = [sb(st, "rt%d" % i, [128, 4, 16], F32) for i in range(2)]
                cmpAf = sb(st, "cmpAf", [128, 32], F32)
                Sc.dma("sp", lambda h: h.dma_start(out=cmpAf[:], in_=cmpA[:, :]), "cmpAf", writes=["cmpAf"])
                Sc.add("pool", lambda h: h.memset(vcx[:], 0.0), writes=["vcx"])
                Sc.add("pool", lambda h: h.memset(vcx[:, 128:129], 1.0), reads=["vcx"], writes=["vcx"])
                Sc.add("dve", lambda h: h.tensor_copy(out=vcx[:, 129:161], in_=cmpAf[:]), reads=["cmpAf", "vcx"], writes=["vcx"])
                Sc.add("pool", lambda h: h.memset(vslc[:, :, 128:129], 1.0), writes=["vslc"])
                Sc.add("pool", lambda h: h.memset(vwin[:, :, 128:129], 1.0), writes=["vwin"])
                pT = [sb(st, "pT%d" % i, [128, 512], BF16) for i in range(3)]
                gsq = sb(st, "gsq", [128, 128], F32); gu = sb(st, "gu", [128, 128], F32)
                den = sb(st, "den", [128, 3, 4], F32); fac = sb(st, "fac", [128, 3, 4], F32)
                imp = sb(st, "imp", [128, 32], F32); impw = sb(st, "impw", [128, 32], F32); m8 = sb(st, "m8", [128, 16], F32)
                negsel = sb(st, "negsel", [128, 32], BF16); negselT = sb(st, "negselT", [32, 128], BF16)
                oacc = sb(st, "oacc", [128, 4, 128], F32); otok = sb(st, "otok", [128, 4, 128], BF16)
                oTb = [sb(st, "oTb%d" % i, [128, 4, 512], BF16) for i in range(2)]
                SB = [B0, B1, MISC]
                OAv = [OA0[:, :].rearrange("p (a b) -> p a b", b=256), OA1[:, :].rearrange("p (a b) -> p a b", b=256)]
                OBv = [OB0[:, :].rearrange("p (a b) -> p a b", b=256), OB1[:, :].rearrange("p (a b) -> p a b", b=256)]
                pcount = [0]
                A1K = ["arena1_%d" % i for i in range(6)]

                def rope(src, nh, dst, i, tag, wkey):
                    cosb = cs[:, i, 0:16].unsqueeze(1).to_broadcast([128, nh, 16])
                    sinb = cs[:, i, 16:32].unsqueeze(1).to_broadcast([128, nh, 16])
                    x1 = src[:, :, 0:16]; x2 = src[:, :, 16:32]
                    t0 = rt[0][:, 0:nh, :]; t1 = rt[1][:, 0:nh, :]
                    rd = [tag]
                    Sc.add("dve", lambda h: h.tensor_tensor(out=t0, in0=x1, in1=cosb, op=ALU.mult), reads=rd + ["cs"], writes=["rt0"])
                    Sc.add("dve", lambda h: h.tensor_tensor(out=t1, in0=x2, in1=sinb, op=ALU.mult), reads=rd + ["cs"], writes=["rt1"])
                    Sc.add("dve", lambda h: h.tensor_tensor(out=dst[:, :, 0:16], in0=t0, in1=t1, op=ALU.subtract), reads=["rt0", "rt1"], writes=[wkey])
                    Sc.add("dve", lambda h: h.tensor_tensor(out=t0, in0=x1, in1=sinb, op=ALU.mult), reads=rd + ["cs"], writes=["rt0"])
                    Sc.add("dve", lambda h: h.tensor_tensor(out=t1, in0=x2, in1=cosb, op=ALU.mult), reads=rd + ["cs"], writes=["rt1"])
                    Sc.add("dve", lambda h: h.tensor_tensor(out=dst[:, :, 16:32], in0=t0, in1=t1, op=ALU.add), reads=["rt0", "rt1"], writes=[wkey])

                for g in range(NG):
                    Sc.dma("pool", lambda h, g=g: h.dma_start(out=wq, in_=w_in_v[:, :, 2048 + g * 512:2048 + (g + 1) * 512]), "wq", writes=["arena0"])
                    for s6 in range(6):
                        c0 = 4096 + s6 * 512 + g * 128
                        Sc.dma("pool", lambda h, s6=s6, c0=c0: h.dma_start(out=wkv[:, :, s6, :], in_=w_in_v[:, :, c0:c0 + 128]), "wkv%d" % s6, writes=["arena1_%d" % s6])
                    def proj_mm(i):
                        p = i % 2
                        tsl = slice(i * 128, (i + 1) * 128)
                        bq = [B0, MISC][p]; kq = ["B0", "MISC"][p]
                        b1 = [B1, OB1][p]; k1 = ["B1", "OB1"][p]
                        b2 = [OA0, OA1][p]; k2 = ["OA", "OA1"][p]
                        for k in range(16):
                            Sc.add("pe", lambda h, k=k: h.matmul(bq[:, :], lhsT=hT[:, k, tsl], rhs=wq[:, k, :], start=(k == 0), stop=(k == 15)), reads=["hT", "arena0"], writes=[kq])
                        for k in range(16):
                            Sc.add("pe", lambda h, k=k: h.matmul(b1[:, :], lhsT=hT[:, k, tsl], rhs=wkv[:, k, 0:4, :], start=(k == 0), stop=(k == 15)), reads=["hT", "arena1_0", "arena1_1", "arena1_2", "arena1_3"], writes=[k1])
                        for k in range(16):
                            Sc.add("pe", lambda h, k=k: h.matmul(b2[:, 0:256], lhsT=hT[:, k, tsl], rhs=wkv[:, k, 4:6, :], start=(k == 0), stop=(k == 15)), reads=["hT", "arena1_4", "arena1_5"], writes=[k2])
                        if g == 0:
                            for k in range(16):
                                Sc.add("pe", lambda h, k=k: h.matmul(OB0[:, 0:48], lhsT=hT[:, k, tsl], rhs=wg[:, k, :], start=(k == 0), stop=(k == 15)), reads=["hT", "wg"], writes=["OB"])

                    def proj_evac(i):
                        p = i % 2
                        bq = [B0, MISC][p]; kq = ["B0", "MISC"][p]
                        b1 = [B1, OB1][p]; k1 = ["B1", "OB1"][p]
                        b2 = [OA0, OA1][p]; k2 = ["OA", "OA1"][p]
                        qtk = qtok2[p]; ktk = ktok2[p]; sq = "qtok%d" % p; sk = "ktok%d" % p
                        if g == 0:
                            Sc.add("act", lambda h: h.activation(out=gates[:, i, :], in_=OB0[:, 0:48], func=AF.Sigmoid), reads=["OB"], writes=["gates"])
                        B0v = bq[:, :].rearrange("p (a b) -> p a b", b=128)
                        B1v = b1[:, :].rearrange("p (a b) -> p a b", b=128)
                        OAp = b2[:, 0:256].rearrange("p (a b) -> p a b", b=128)
                        Sc.add("act", lambda h: h.activation(out=qtk[:, :, 32:128], in_=B0v[:, :, 32:128], func=AF.Copy), reads=[kq], writes=[sq + "a"])
                        rope(B0v, 4, qtk[:, :, :], i, kq, sq + "r")
                        Sc.add("act", lambda h: h.activation(out=ktk[:, 0:2, 32:128], in_=B1v[:, 0:4:2, 32:128], func=AF.Copy), reads=[k1], writes=[sk + "a"])
                        rope(B1v[:, 0:4:2, :], 2, ktk[:, 0:2, :], i, k1, sk + "r1")
                        Sc.add("act", lambda h: h.activation(out=ktk[:, 2:3, 32:128], in_=OAp[:, 0:1, 32:128], func=AF.Copy), reads=[k2], writes=[sk + "b"])
                        rope(OAp[:, 0:1, :], 1, ktk[:, 2:3, :], i, k2, sk + "r2")
                        Sc.add("act", lambda h: h.activation(out=ktk[:, 3, :], in_=B1v[:, 1, :], func=AF.Copy), reads=[k1], writes=[sk + "c"])
                        Sc.add("act", lambda h: h.activation(out=vslc[:, i, 0:128], in_=B1v[:, 3, :], func=AF.Copy), reads=[k1], writes=["vslc"])
                        Sc.add("act", lambda h: h.activation(out=vwin[:, i, 0:128], in_=OAp[:, 1, :], func=AF.Copy), reads=[k2], writes=["vwin"])

                    def proj_tr(i):
                        p = i % 2
                        tsl = slice(i * 128, (i + 1) * 128)
                        qtk = qtok2[p]; ktk = ktok2[p]; sq = "qtok%d" % p; sk = "ktok%d" % p
                        for r in range(4):
                            Sc.add("pe", lambda h, r=r: h.transpose(out=TB[:, r, :], in_=qtk[:, r, :], identity=identb[:]), reads=[sq + "a", sq + "r", "identb"], writes=["TB"])
                        for r in range(4):
                            Sc.add("pe", lambda h, r=r: h.transpose(out=TB[:, 4 + r, :], in_=ktk[:, r, :], identity=identb[:]),
                                   reads=[sk + "a", sk + "b", sk + "c", sk + "r1", sk + "r2", "identb"], writes=["TB"])
                        Sc.add("act", lambda h: h.activation(out=qT[:, :, tsl], in_=TB[:, 0:4, :], func=AF.Copy), reads=["TB"], writes=["qT"])
                        Sc.add("act", lambda h: h.activation(out=kT[:, :, tsl], in_=TB[:, 4:7, :], func=AF.Copy), reads=["TB"], writes=["kT"])
                        Sc.add("act", lambda h: h.activation(out=vcT[:, tsl], in_=TB[:, 7, :], func=AF.Copy), reads=["TB"], writes=["vcT"])

                    proj_mm(0); proj_evac(0)
                    for i in range(NT):
                        if i + 1 < NT:
                            proj_mm(i + 1); proj_evac(i + 1)
                        proj_tr(i)
                        if i % 2 == 1:
                            bg(1)
                    for kv in range(2):
                        Sc.dma("pool", lambda h, kv=kv: h.dma_start(out=w1[kv], in_=cmp_w1[kv].rearrange("(j d) n -> d j n", d=128)), "w1_%d" % kv, writes=(["arena0"] if kv == 0 else A1K))
                    for kv in range(2):
                        srcT = kT[:, 0, :] if kv == 0 else vcT[:, :]
                        skey = "kT" if kv == 0 else "vcT"
                        for blk in range(2):
                            for j in range(32):
                                rhs = bass.AP(srcT.tensor, srcT.offset + j, [[srcT.ap[0][0], 128], [16, 127]])
                                Sc.add("pe", lambda h, kv=kv, blk=blk, j=j, rhs=rhs: h.matmul(MISC[:, 0:127], lhsT=w1[kv][:, j, blk * 128:(blk + 1) * 128], rhs=rhs, start=(j == 0), stop=False),
                                       reads=(["arena0"] if kv == 0 else A1K) + [skey], writes=["MISC"])
                                Sc.add("pe", lambda h, kv=kv, blk=blk, j=j: h.matmul(MISC[:, 0:127], lhsT=w1[kv][:, j, blk * 128:(blk + 1) * 128], rhs=posT[kv][:, j:j + 1].to_broadcast([128, 127]), start=False, stop=(j == 31)),
                                       reads=(["arena0"] if kv == 0 else A1K) + ["posT%d" % kv], writes=["MISC"])
                            Sc.add("act", lambda h: h.activation(out=gsq[:, 0:127], in_=MISC[:, 0:127], func=AF.Square), reads=["MISC"], writes=["gsq"])
                            Sc.add("dve", lambda h: h.tensor_scalar(out=gsq[:, 0:127], in0=gsq[:, 0:127], scalar1=0.044715, scalar2=1.0, op0=ALU.mult, op1=ALU.add), reads=["gsq"], writes=["gsq"])
                            Sc.add("dve", lambda h: h.tensor_tensor(out=gu[:, 0:127], in0=gsq[:, 0:127], in1=MISC[:, 0:127], op=ALU.mult), reads=["gsq", "MISC"], writes=["gu"])
                            Sc.add("act", lambda h: h.activation(out=gu[:, 0:127], in_=gu[:, 0:127], func=AF.Tanh, scale=0.7978845608), reads=["gu"], writes=["gu"])
                            Sc.add("dve", lambda h: h.tensor_scalar(out=gu[:, 0:127], in0=gu[:, 0:127], scalar1=1.0, scalar2=0.5, op0=ALU.add, op1=ALU.mult), reads=["gu"], writes=["gu"])
                            Sc.add("dve", lambda h, kv=kv, blk=blk: h.tensor_tensor(out=hid[kv][:, blk, 0:127], in0=gu[:, 0:127], in1=MISC[:, 0:127], op=ALU.mult), reads=["gu", "MISC"], writes=["hid%d" % kv])
                    for blk in range(2):
                        Sc.add("pe", lambda h, blk=blk: h.matmul(MISC[:, 0:127], lhsT=w2[0][:, blk, :], rhs=hid[0][:, blk, 0:127], start=(blk == 0), stop=(blk == 1)), reads=["w2_0", "hid0"], writes=["MISC"])
                    Sc.add("dve", lambda h: h.tensor_copy(out=kcT[:, 0:127], in_=MISC[:, 0:127]), reads=["MISC"], writes=["kcT"])
                    for blk in range(2):
                        Sc.add("pe", lambda h, blk=blk: h.matmul(MISC[0:127, 0:128], lhsT=hid[1][:, blk, 0:127], rhs=w2[1][:, blk, :], start=(blk == 0), stop=(blk == 1)), reads=["w2_1", "hid1"], writes=["MISC"])
                    Sc.add("dve", lambda h: h.tensor_copy(out=vcx[0:127, 0:128], in_=MISC[0:127, 0:128]), reads=["MISC"], writes=["vcx"])

                    OAK = ["OA", "OA1"]; OBK = ["OB", "OB1"]
                    def emit_qk(job):
                        sidx = pcount[0] % 3
                        pcount[0] += 1
                        job["sidx"] = sidx
                        bank = SB[sidx]; bkey = ["B0", "B1", "MISC"][sidx]
                        i = job["i"]; npart = job["npart"]; lhsT = job["lhsT"]; extra = job["extra"]
                        qs = qT[:, :, i * 128:(i + 1) * 128]
                        n = len(extra)
                        Sc.add("pe", lambda h: h.matmul(bank[0:npart, :], lhsT=lhsT, rhs=qs, start=True, stop=(n == 0)), reads=["qT"] + job["lkeys"], writes=[bkey])
                        for xi, (l2, r2, k2) in enumerate(extra):
                            Sc.add("pe", lambda h, l2=l2, r2=r2, xi=xi: h.matmul(bank[0:npart, :], lhsT=l2, rhs=r2, start=False, stop=(xi == n - 1)), reads=k2, writes=[bkey])
                        Sc.add("act", lambda h: h.activation(out=pT[sidx][0:npart, :], in_=bank[0:npart, :], func=AF.Exp, scale=SCALE), reads=[bkey], writes=["pT%d" % sidx])

                    def bc4(ap2, npart):
                        return ap2.unsqueeze(1).to_broadcast([npart, 4, 128])

                    def denfac(branch, Ov, okey, i, g):
                        for b in range(2):
                            Sc.add("dve", lambda h, b=b: h.tensor_scalar(out=den[:, branch, 2 * b:2 * b + 2], in0=Ov[b][:, :, 128], scalar1=1e-30, scalar2=None, op0=ALU.max), reads=okey, writes=["den%d" % branch])
                        Sc.add("dve", lambda h: h.reciprocal(out=den[:, branch, :], in_=den[:, branch, :]), reads=["den%d" % branch], writes=["den%d" % branch])
                        gc0 = branch * 16 + g * 4
                        Sc.add("dve", lambda h: h.tensor_tensor(out=fac[:, branch, :], in0=den[:, branch, :], in1=gates[:, i, gc0:gc0 + 4], op=ALU.mult), reads=["den%d" % branch, "gates"], writes=["fac%d" % branch])

                    pending_pe = []

                    def make_jobs(i, g):
                        jobs = []

                        def pv_cmp(sidx):
                            for r in range(4):
                                Sc.add("pe", lambda h, r=r: h.matmul(OBv[r // 2][:, r % 2, 0:161], lhsT=pT[sidx][0:127, r * 128:(r + 1) * 128], rhs=vcx[0:127, 0:161], start=True, stop=True),
                                       reads=["pT%d" % sidx, "vcx"], writes=OBK)

                        def post_cmp():
                            denfac(0, OBv, OBK, i, g)
                            if i >= 8:
                                for r in range(4):
                                    if r == 0:
                                        Sc.add("dve", lambda h: h.tensor_scalar(out=imp[:], in0=OBv[0][:, 0, 129:161], scalar1=den[:, 0, 0:1], scalar2=None, op0=ALU.mult), reads=OBK + ["den0"], writes=["imp"])
                                    else:
                                        Sc.add("dve", lambda h, r=r: h.scalar_tensor_tensor(out=imp[:], in0=OBv[r // 2][:, r % 2, 129:161], scalar=den[:, 0, r:r + 1], in1=imp[:], op0=ALU.mult, op1=ALU.add), reads=OBK + ["den0", "imp"], writes=["imp"])
                            for r in range(4):
                                Sc.add("dve", lambda h, r=r: h.tensor_scalar(out=oacc[:, r, :], in0=OBv[r // 2][:, r % 2, 0:128], scalar1=fac[:, 0, r:r + 1], scalar2=None, op0=ALU.mult), reads=OBK + ["fac0"], writes=["oacc"])
                            if i >= 8:
                                Sc.add("dve", lambda h: h.tensor_tensor(out=imp[:], in0=imp[:], in1=addm[:, i - 8, :], op=ALU.add), reads=["imp", "addm"], writes=["imp"])
                                Sc.add("dve", lambda h: h.max(out=m8[:, 0:8], in_=imp[:]), reads=["imp"], writes=["m8"])
                                Sc.add("dve", lambda h: h.match_replace(out=impw[:], in_to_replace=m8[:, 0:8], in_values=imp[:], imm_value=-3e38), reads=["imp", "m8"], writes=["impw"])
                                Sc.add("dve", lambda h: h.max(out=m8[:, 8:16], in_=impw[:]), reads=["impw"], writes=["m8"])
                                Sc.add("dve", lambda h: h.tensor_scalar(out=impw[:], in0=imp[:], scalar1=m8[:, 15:16], scalar2=None, op0=ALU.is_ge), reads=["imp", "m8"], writes=["impw"])
                                Sc.add("dve", lambda h: h.tensor_scalar(out=negsel[:], in0=impw[:], scalar1=-1.0, scalar2=-NEG, op0=ALU.add, op1=ALU.mult), reads=["impw"], writes=["negsel"])
                                def pe_part():
                                    Sc.add("pe", lambda h: h.transpose(out=TB[0:32, 4, :], in_=negsel[:, :], identity=identb[:]), reads=["negsel", "identb"], writes=["TB"])
                                    Sc.add("dve", lambda h: h.tensor_copy(out=negselT[:, :], in_=TB[0:32, 4, :]), reads=["TB"], writes=["negselT"])
                                pending_pe.append(pe_part)

                        jobs.append(dict(i=i, lhsT=kcT[:, 0:127], lkeys=["kcT"], npart=127, pv=pv_cmp, post=post_cmp,
                                         extra=[(identb[0:127, 0:127], bc4(cmpmask[0:127, i, :], 127), ["identb", "cmpmask"])]))

                        j0 = max(0, i - 4)
                        for j in range(j0, i + 1):
                            extra = []
                            if j == i:
                                extra.append((identb[:, :], bc4(tri[:, 0, :], 128), ["identb", "tri"]))
                            if j == i - 4:
                                extra.append((identb[:, :], bc4(tri[:, 1, :], 128), ["identb", "tri"]))

                            def pv_win(sidx, j=j):
                                for r in range(4):
                                    Sc.add("pe", lambda h, r=r: h.matmul(OAv[r // 2][:, r % 2, 0:129], lhsT=pT[sidx][:, r * 128:(r + 1) * 128], rhs=vwin[:, j, 0:129], start=(j == j0 and r % 2 == 0), stop=(j == i), skip_group_check=True),
                                           reads=["pT%d" % sidx, "vwin"], writes=OAK)

                            jobs.append(dict(i=i, lhsT=kT[:, 2, j * 128:(j + 1) * 128], lkeys=["kT"], npart=128, pv=pv_win, post=None, extra=extra))

                        def post_combine():
                            denfac(1, OBv, OBK, i, g)
                            for r in range(4):
                                Sc.add("dve", lambda h, r=r: h.scalar_tensor_tensor(out=oacc[:, r, :], in0=OBv[r // 2][:, r % 2, 0:128], scalar=fac[:, 1, r:r + 1], in1=oacc[:, r, :], op0=ALU.mult, op1=ALU.add), reads=OBK + ["fac1", "oacc"], writes=["oacc"])
                            denfac(2, OAv, OAK, i, g)
                            for r in range(4):
                                Sc.add("dve", lambda h, r=r: h.scalar_tensor_tensor(out=otok[:, r, :], in0=OAv[r // 2][:, r % 2, 0:128], scalar=fac[:, 2, r:r + 1], in1=oacc[:, r, :], op0=ALU.mult, op1=ALU.add), reads=OAK + ["fac2", "oacc"], writes=["otok"])
                            bg(1)

                            def pe_part():
                                for r in range(4):
                                    Sc.add("pe", lambda h, r=r: h.transpose(out=TB[:, r, :], in_=otok[:, r, :], identity=identb[:]), reads=["otok", "identb"], writes=["TB"])
                                ob = (i // 4) % 2
                                Sc.add("act", lambda h: h.activation(out=oTb[ob][:, :, (i % 4) * 128:(i % 4 + 1) * 128], in_=TB[:, 0:4, :], func=AF.Copy), reads=["TB"], writes=["oTb%d" % ob])
                                if i % 4 == 3:
                                    t0 = (i // 4) * 512
                                    Sc.dma("sp", lambda h: h.dma_start(out=OT.rearrange("(hh d) t -> d hh t", d=128)[:, g * 4:(g + 1) * 4, t0:t0 + 512], in_=oTb[ob][:]), "OTw%d" % ob, reads=["oTb%d" % ob], writes=["OT"])
                            pending_pe.append(pe_part)

                        for j in range(0, i + 1):
                            extra = []
                            if j == i:
                                extra.append((identb[:, :], bc4(tri[:, 0, :], 128), ["identb", "tri"]))
                            if i >= 8:
                                extra.append((esel[0:32, j, :], bc4(negselT[0:32, :], 32), ["esel", "negselT"]))

                            def pv_slc(sidx, j=j):
                                for r in range(4):
                                    Sc.add("pe", lambda h, r=r: h.matmul(OBv[r // 2][:, r % 2, 0:129], lhsT=pT[sidx][:, r * 128:(r + 1) * 128], rhs=vslc[:, j, 0:129], start=(j == 0 and r % 2 == 0), stop=(j == i), skip_group_check=True),
                                           reads=["pT%d" % sidx, "vslc"], writes=OBK)

                            jobs.append(dict(i=i, lhsT=kT[:, 1, j * 128:(j + 1) * 128], lkeys=["kT"], npart=128, pv=pv_slc, post=(post_combine if j == i else None), extra=extra))
                        return jobs

                    jobs = []
                    for i in range(NT):
                        jobs += make_jobs(i, g)
                    LOOK = 2
                    for idx in range(min(LOOK, len(jobs))):
                        emit_qk(jobs[idx])
                    deferred = []
                    for idx, job in enumerate(jobs):
                        job["pv"](job["sidx"])
                        if job["post"] is not None:
                            job["post"]()
                        while pending_pe:
                            deferred.append((idx + 2, pending_pe.pop(0)))
                        while deferred and deferred[0][0] <= idx:
                            deferred.pop(0)[1]()
                        if idx + LOOK < len(jobs):
                            emit_qk(jobs[idx + LOOK])
                    while deferred:
                        deferred.pop(0)[1]()
                Sc.flush()
        if stop_after == "P3":
            Sc.finish(); return nc

        dest_i = sb(top, "dest_i", [128, NT, 2], I32)
        gate2 = sb(top, "gate2", [128, NT, 2], F32)
        with ExitStack() as st:
            B0 = ps(st, "B0_4", [128, 512], F32); B1 = ps(st, "B1_4", [128, 512], F32); MISC = ps(st, "MISC_4", [128, 512], F32)
            OA0 = ps(st, "OA0_4", [128, 512], F32); OB0 = ps(st, "OB0_4", [128, 512], F32)
            TB = ps(st, "TB_4", [128, 8, 128], BF16); TB2 = ps(st, "TB2_4", [128, 8, 128], BF16)
            TBS = [TB, TB2]
            bmT = sb(st, "bmT", [128, 32], F32)
            load_T(st, "bmT", b_merge[:, :], 32, bmT[:, :], MISC)
            fng = sb(st, "fng", [128, D], F32)
            Sc.dma("sp", lambda h: h.dma_start(out=fng[:], in_=ffn_norm[0:1, :].broadcast_to([128, D])), "fng", writes=["fng"])
            bgr = sb(st, "bgr", [128, 72], F32)
            Sc.dma("sp", lambda h: h.dma_start(out=bgr[:], in_=b_gr[0:1, :].broadcast_to([128, 72])), "bgr", writes=["bgr"])
            wgr = sb(st, "wgr", [128, 16, 72], BF16)
            Sc.dma("pool", lambda h: h.dma_start(out=wgr[:], in_=w_gr.rearrange("(k p) n -> p k n", p=128)), "wgr", writes=["wgr"])
            upper = sb(st, "upper", [128, 128], BF16)
            Sc.dma("pool", lambda h: h.dma_start(out=upper[:], in_=upper_d[:, :]), "upper", writes=["upper"])
            eoff = sb(st, "eoff", [128, 64], F32)
            Sc.dma("sp", lambda h: h.dma_start(out=eoff[:], in_=eoff_d[:, :]), "eoff", writes=["eoff"])
            base = sb(st, "base", [128, 64], F32)
            Sc.add("pool", lambda h: h.memset(base[:], 0.0), writes=["base"])

            HALF = 1024
            mT = sb(st, "mT", [128, 16, HALF], BF16)
            lg = sb(st, "lg", [128, 72], F32)
            r8 = sb(st, "r8", [128, 8], F32); r1 = sb(st, "r1", [128, 8], F32)
            oh = [sb(st, "oh%d" % i, [128, 64], F32) for i in range(2)]
            msk = sb(st, "msk", [128, 64], F32); Ab = sb(st, "Ab", [128, 64], BF16)
            cnt = sb(st, "cnt", [128, 64], F32); tmp64 = sb(st, "tmp64", [128, 64], F32)
            dst_f = sb(st, "dst_f", [128, 4], F32)
            ss = sb(st, "ss4", [128, 1], F32)
            w_merge_v = w_merge.rearrange("(k p) n -> p k n", p=128)
            w_pc_v = w_proj_conv.rearrange("(k p) n -> p k n", p=128)
            w_pn_v = w_proj_nsa.rearrange("(k p) n -> p k n", p=128)
            w_out_v = w_out.rearrange("(k p) n -> p k n", p=128)
            for hf in range(2):
              h0 = hf * HALF
              with ExitStack() as sa:
                hTh = sb(sa, "hTh%d" % hf, [128, 16, HALF], BF16)
                uTh = sb(sa, "uTh%d" % hf, [128, 8, HALF], BF16)
                oTh = sb(sa, "oTh%d" % hf, [128, 16, HALF], BF16)
                wm = [sb(sa, "wm%d_%d" % (hf, i), [128, 16, 2, 256], BF16) for i in range(2)]
                wpc = [sb(sa, "wpc%d_%d" % (hf, i), [128, 8, 256], BF16) for i in range(2)]
                wpn = [sb(sa, "wpn%d_%d" % (hf, i), [128, 16, 256], BF16) for i in range(2)]
                gcs = sb(sa, "gcs%d" % hf, [128, 512], F32); gns = sb(sa, "gns%d" % hf, [128, 512], F32); tmpm = sb(sa, "tmpm%d" % hf, [128, 512], F32)
                for kc in range(16):
                    Sc.dma("sp", lambda h, kc=kc, h0=h0: h.dma_start(out=hTh[:, kc, :], in_=HT[kc * 128:(kc + 1) * 128, h0:h0 + HALF]), "hTh%d" % kc, reads=["HT"], writes=["hTh%d" % kc])
                    Sc.dma("sp", lambda h, kc=kc, h0=h0: h.dma_start(out=oTh[:, kc, :], in_=OT[kc * 128:(kc + 1) * 128, h0:h0 + HALF]), "oTh%d" % kc, reads=["OT"], writes=["oTh%d" % kc])
                for kc in range(8):
                    Sc.dma("sp", lambda h, kc=kc, h0=h0: h.dma_start(out=uTh[:, kc, :], in_=UT[kc * 128:(kc + 1) * 128, h0:h0 + HALF]), "uTh%d" % kc, reads=["UT"], writes=["uTh%d" % kc])

                def load_w(d8):
                    wbi = d8 % 2
                    c0 = d8 * 256
                    Sc.dma("pool", lambda h: h.dma_start(out=wm[wbi][:, :, 0, :], in_=w_merge_v[:, :, c0:c0 + 256]), "wm%d" % wbi, writes=["wm%d" % wbi])
                    Sc.dma("pool", lambda h: h.dma_start(out=wm[wbi][:, :, 1, :], in_=w_merge_v[:, :, D + c0:D + c0 + 256]), "wm%d" % wbi, writes=["wm%d" % wbi])
                    Sc.dma("pool", lambda h: h.dma_start(out=wpc[wbi][:], in_=w_pc_v[:, :, c0:c0 + 256]), "wpc%d" % wbi, writes=["wpc%d" % wbi])
                    Sc.dma("pool", lambda h: h.dma_start(out=wpn[wbi][:], in_=w_pn_v[:, :, c0:c0 + 256]), "wpn%d" % wbi, writes=["wpn%d" % wbi])

                def stage_a(d8, db, t2):
                    wbi = d8 % 2
                    dblk = d8 * 2 + db
                    csl = slice(db * 128, (db + 1) * 128)
                    tsl = slice(t2 * 512, (t2 + 1) * 512)
                    kwm = "wm%d" % wbi; kpc = "wpc%d" % wbi; kpn = "wpn%d" % wbi
                    for k in range(16):
                        Sc.add("pe", lambda h, k=k: h.matmul(OA0[:, :], lhsT=wm[wbi][:, k, 0, csl], rhs=hTh[:, k, tsl], start=(k == 0), stop=(k == 15)), reads=[kwm, "hTh%d" % k], writes=["OA"])
                    for k in range(16):
                        Sc.add("pe", lambda h, k=k: h.matmul(OB0[:, :], lhsT=wm[wbi][:, k, 1, csl], rhs=hTh[:, k, tsl], start=(k == 0), stop=(k == 15)), reads=[kwm, "hTh%d" % k], writes=["OB"])
                    for k in range(8):
                        Sc.add("pe", lambda h, k=k: h.matmul(B0[:, :], lhsT=wpc[wbi][:, k, csl], rhs=uTh[:, k, tsl], start=(k == 0), stop=(k == 7)), reads=[kpc, "uTh%d" % k], writes=["B0"])
                    for k in range(16):
                        Sc.add("pe", lambda h, k=k: h.matmul(B1[:, :], lhsT=wpn[wbi][:, k, csl], rhs=oTh[:, k, tsl], start=(k == 0), stop=(k == 15)), reads=[kpn, "oTh%d" % k], writes=["B1"])
                    Sc.add("act", lambda h: h.activation(out=gcs[:], in_=OA0[:, :], func=AF.Sigmoid, bias=bmT[:, dblk:dblk + 1]), reads=["OA", "bmT"], writes=["gcs"])
                    Sc.add("act", lambda h: h.activation(out=gns[:], in_=OB0[:, :], func=AF.Sigmoid, bias=bmT[:, 16 + dblk:17 + dblk]), reads=["OB", "bmT"], writes=["gns"])
                    Sc.add("dve", lambda h: h.tensor_tensor(out=tmpm[:], in0=gcs[:], in1=B0[:, :], op=ALU.mult), reads=["gcs", "B0"], writes=["tmpm"])
                    Sc.add("dve", lambda h: h.tensor_tensor(out=gns[:], in0=gns[:], in1=B1[:, :], op=ALU.mult), reads=["gns", "B1"], writes=["gns"])
                    Sc.add("dve", lambda h: h.tensor_tensor(out=mT[:, dblk, tsl], in0=tmpm[:], in1=gns[:], op=ALU.add), reads=["tmpm", "gns"], writes=["mT"])

                load_w(0)
                for d8 in range(8):
                    if d8 + 1 < 8:
                        load_w(d8 + 1)
                    bg(1)
                    for db in range(2):
                        for t2 in range(2):
                            stage_a(d8, db, t2)
                Sc.flush()
              with ExitStack() as sk:
                wo4 = sb(sk, "wo4_%d" % hf, [128, 4, 16, 512], BF16)
                xin2 = [sb(sk, "xin%d_%d" % (hf, i), [128, D], F32) for i in range(2)]
                x1t2 = [sb(sk, "x1t%d_%d" % (hf, i), [128, D], F32) for i in range(2)]
                xnb = [sb(sk, "xnb%d_%d" % (i, hf), [128, D], BF16) for i in range(2)]
                junk = sb(sk, "junk4_%d" % hf, [128, D], BF16)
                xnT = sb(sk, "xnT%d" % hf, [128, 16, 128], BF16)
                for d4 in range(4):
                    Sc.dma("pool", lambda h, d4=d4: h.dma_start(out=wo4[:, d4, :, :], in_=w_out_v[:, :, d4 * 512:(d4 + 1) * 512]), "wo4_%d" % d4, writes=["wo4_%d" % d4])
                def sb_mm(t8):
                    tt = hf * 8 + t8
                    tsl = slice(t8 * 128, (t8 + 1) * 128)
                    xin = xin2[t8 % 2]; x1t = x1t2[t8 % 2]; kxin = "xin%d" % (t8 % 2); kx1t = "x1t%d" % (t8 % 2)
                    Sc.dma("sp", lambda h, tt=tt, xin=xin: h.dma_start(out=xin[:], in_=x[tt * 128:(tt + 1) * 128, :]), kxin, writes=[kxin])
                    for d4 in range(4):
                        dsl = slice(d4 * 512, (d4 + 1) * 512)
                        bank = [B0, B1, OA0, OB0][d4]; bkey = ["B0", "B1", "OA", "OB"][d4]
                        for k in range(16):
                            Sc.add("pe", lambda h, k=k, tsl=tsl, bank=bank, d4=d4: h.matmul(bank[:, :], lhsT=mT[:, k, tsl], rhs=wo4[:, d4, k, :], start=(k == 0), stop=(k == 15)), reads=["mT", "wo4_%d" % d4], writes=[bkey])
                        Sc.add("dve", lambda h, dsl=dsl, bank=bank, x1t=x1t, xin=xin: h.tensor_tensor(out=x1t[:, dsl], in0=bank[:, :], in1=xin[:, dsl], op=ALU.add), reads=[bkey, kxin], writes=[kx1t])

                def sb_post(t8):
                    tt = hf * 8 + t8
                    tsl = slice(t8 * 128, (t8 + 1) * 128)
                    xin = xin2[t8 % 2]; x1t = x1t2[t8 % 2]; kxin = "xin%d" % (t8 % 2); kx1t = "x1t%d" % (t8 % 2)
                    Sc.dma("sp", lambda h, tt=tt, x1t=x1t: h.dma_start(out=X1[tt * 128:(tt + 1) * 128, :], in_=x1t[:]), "X1w%d" % (t8 % 2), reads=[kx1t], writes=["X1"])
                    s = tt % 2
                    Sc.add("act", lambda h, x1t=x1t: h.activation(out=junk[:], in_=x1t[:], func=AF.Square, accum_out=ss[:]), reads=[kx1t], writes=["junk4", "ss4"])
                    Sc.add("act", lambda h: h.activation(out=ss[:], in_=ss[:], func=AF.Sqrt, scale=1.0 / D, bias=EPS), reads=["ss4"], writes=["ss4"])
                    Sc.add("dve", lambda h: h.reciprocal(out=ss[:], in_=ss[:]), reads=["ss4"], writes=["ss4"])
                    Sc.add("dve", lambda h, s=s, x1t=x1t: h.scalar_tensor_tensor(out=xnb[s][:], in0=x1t[:], scalar=ss[:, 0:1], in1=fng[:], op0=ALU.mult, op1=ALU.mult), reads=[kx1t, "ss4", "fng"], writes=["xnb%d" % s])
                    for c4 in range(4):
                        for j in range(4):
                            c = 4 * c4 + j
                            Sc.add("pe", lambda h, s=s, c=c, j=j, c4=c4: h.transpose(out=TBS[c4 % 2][:, j, :], in_=xnb[s][:, c * 128:(c + 1) * 128], identity=identb[:]), reads=["xnb%d" % s, "identb"], writes=[["TB", "TB2"][c4 % 2]])
                        Sc.add("act", lambda h, c4=c4: h.activation(out=xnT[:, 4 * c4:4 * c4 + 4, :], in_=TBS[c4 % 2][:, 0:4, :], func=AF.Copy), reads=[["TB", "TB2"][c4 % 2]], writes=["xnT"])
                    for k in range(16):
                        Sc.add("pe", lambda h, k=k: h.matmul(MISC[:, 0:72], lhsT=xnT[:, k, :], rhs=wgr[:, k, :], start=(k == 0), stop=(k == 15)), reads=["xnT", "wgr"], writes=["MISC"])
                    Sc.add("dve", lambda h: h.tensor_tensor(out=lg[:], in0=MISC[:, 0:72], in1=bgr[:], op=ALU.add), reads=["MISC", "bgr"], writes=["lg"])
                    Sc.add("dve", lambda h: h.max(out=r8[:], in_=lg[:, 0:8]), reads=["lg"], writes=["r8"])
                    Sc.add("dve", lambda h: h.tensor_scalar(out=r1[:, 0:1], in0=r8[:, 0:1], scalar1=-1.0, scalar2=None, op0=ALU.mult), reads=["r8"], writes=["r1a"])
                    Sc.add("act", lambda h: h.activation(out=r1[:, 0:8], in_=lg[:, 0:8], func=AF.Exp, bias=r1[:, 0:1], accum_out=r1[:, 1:2]) if False else h.activation(out=tmp64[:, 0:8], in_=lg[:, 0:8], func=AF.Exp, bias=r1[:, 0:1], accum_out=r1[:, 1:2]),
                           reads=["lg", "r1a"], writes=["tmp64", "r1b"])
                    Sc.add("dve", lambda h: h.reciprocal(out=r1[:, 2:3], in_=r1[:, 1:2]), reads=["r1b"], writes=["r1c"])
                    Sc.add("dve", lambda h: h.tensor_scalar(out=tmp64[:, 8:16], in0=lg[:, 0:8], scalar1=r8[:, 0:1], scalar2=None, op0=ALU.is_ge), reads=["lg", "r8", "tmp64"], writes=["tmp64"])
                    Sc.add("dve", lambda h: h.tensor_scalar(out=tmp64[:, 8:16], in0=tmp64[:, 8:16], scalar1=-1.0, scalar2=1e9, op0=ALU.add, op1=ALU.mult), reads=["tmp64"], writes=["tmp64"])
                    Sc.add("dve", lambda h: h.tensor_tensor(out=msk[:, :].rearrange("p (g e) -> p g e", e=8), in0=lg[:, 8:72].rearrange("p (g e) -> p g e", e=8),
                                                            in1=tmp64[:, 8:16].unsqueeze(2).to_broadcast([128, 8, 8]), op=ALU.add), reads=["lg", "tmp64"], writes=["msk"])
                    Sc.add("dve", lambda h: h.max(out=r8[:], in_=msk[:]), reads=["msk", "r8"], writes=["r8"])
                    Sc.add("dve", lambda h: h.tensor_scalar(out=oh[0][:], in0=msk[:], scalar1=r8[:, 0:1], scalar2=None, op0=ALU.is_equal), reads=["msk", "r8"], writes=["oh0"])
                    Sc.add("dve", lambda h: h.tensor_scalar(out=oh[1][:], in0=msk[:], scalar1=r8[:, 1:2], scalar2=None, op0=ALU.is_equal), reads=["msk", "r8"], writes=["oh1"])
                    Sc.add("dve", lambda h: h.tensor_tensor(out=r1[:, 3:4], in0=r8[:, 0:1], in1=r8[:, 1:2], op=ALU.subtract), reads=["r8"], writes=["r1d"])
                    Sc.add("act", lambda h: h.activation(out=r1[:, 4:5], in_=r1[:, 3:4], func=AF.Sigmoid), reads=["r1d"], writes=["r1e"])
                    Sc.add("dve", lambda h, tt=tt: h.tensor_tensor(out=gate2[:, tt, 0:1], in0=r1[:, 4:5], in1=r1[:, 2:3], op=ALU.mult), reads=["r1e", "r1c"], writes=["gate2"])
                    Sc.add("dve", lambda h, tt=tt: h.tensor_tensor(out=gate2[:, tt, 1:2], in0=r1[:, 2:3], in1=gate2[:, tt, 0:1], op=ALU.subtract), reads=["r1c", "gate2"], writes=["gate2"])
                    Sc.add("dve", lambda h: h.tensor_tensor(out=Ab[:], in0=oh[0][:], in1=oh[1][:], op=ALU.add), reads=["oh0", "oh1"], writes=["Ab"])
                    Sc.add("pe", lambda h: h.matmul(MISC[:, 128:192], lhsT=upper[:, :], rhs=Ab[:], start=True, stop=True), reads=["upper", "Ab"], writes=["MISC"])
                    Sc.add("pe", lambda h: h.matmul(MISC[:, 192:256], lhsT=onesb[:, :], rhs=Ab[:], start=True, stop=True), reads=["onesb", "Ab"], writes=["MISC"])
                    Sc.add("dve", lambda h: h.tensor_tensor(out=cnt[:], in0=MISC[:, 128:192], in1=base[:], op=ALU.add), reads=["MISC", "base"], writes=["cnt"])
                    Sc.add("dve", lambda h: h.tensor_tensor(out=base[:], in0=MISC[:, 192:256], in1=base[:], op=ALU.add), reads=["MISC", "base"], writes=["base"])
                    for kk in range(2):
                        Sc.add("dve", lambda h, kk=kk: h.tensor_tensor(out=tmp64[:], in0=oh[kk][:], in1=cnt[:], op=ALU.mult), reads=["oh%d" % kk, "cnt"], writes=["tmp64"])
                        Sc.add("dve", lambda h, kk=kk: h.reduce_sum(out=dst_f[:, kk:kk + 1], in_=tmp64[:], axis=mybir.AxisListType.X), reads=["tmp64"], writes=["dst_f"])
                        Sc.add("dve", lambda h, kk=kk: h.tensor_tensor(out=tmp64[:], in0=oh[kk][:], in1=eoff[:], op=ALU.mult), reads=["oh%d" % kk, "eoff"], writes=["tmp64"])
                        Sc.add("dve", lambda h, kk=kk: h.reduce_sum(out=dst_f[:, 2 + kk:3 + kk], in_=tmp64[:], axis=mybir.AxisListType.X), reads=["tmp64"], writes=["dst_f"])
                        Sc.add("dve", lambda h, kk=kk: h.tensor_scalar(out=tmp64[:, 0:1], in0=dst_f[:, kk:kk + 1], scalar1=float(CAP), scalar2=1e6, op0=ALU.is_ge, op1=ALU.mult), reads=["dst_f"], writes=["tmp64"])
                        Sc.add("dve", lambda h, kk=kk: h.tensor_tensor(out=dst_f[:, kk:kk + 1], in0=dst_f[:, kk:kk + 1], in1=dst_f[:, 2 + kk:3 + kk], op=ALU.add), reads=["dst_f"], writes=["dst_f"])
                        Sc.add("dve", lambda h, kk=kk: h.tensor_tensor(out=dst_f[:, kk:kk + 1], in0=dst_f[:, kk:kk + 1], in1=tmp64[:, 0:1], op=ALU.add), reads=["dst_f", "tmp64"], writes=["dst_f"])
                    Sc.add("dve", lambda h, tt=tt: h.tensor_copy(out=dest_i[:, tt, :], in_=dst_f[:, 0:2]), reads=["dst_f"], writes=["dest_i"])
                    for kk in range(2):
                        Sc.dma("pool", lambda h, tt=tt, kk=kk, s=s: h.indirect_dma_start(out=XS[:, :], out_offset=bass.IndirectOffsetOnAxis(ap=dest_i[:, tt, kk:kk + 1], axis=0), in_=xnb[s][:, :], in_offset=None,
                                                                                  bounds_check=Sc.bound_reg(h, NSLOT - 1), oob_is_err=False), "xs_sc%d" % s, reads=["xnb%d" % s, "dest_i"], writes=["XS"])
                    if dbg:
                        Sc.add("dve", lambda h, tt=tt: h.tensor_copy(out=dst_f[:, 2:4], in_=gate2[:, tt, :]), reads=["gate2", "dst_f"], writes=["dst_f"])
                        Sc.dma("sp", lambda h, tt=tt: h.dma_start(out=RT[tt * 128:(tt + 1) * 128, :], in_=dst_f[:]), "RTw", reads=["dst_f"], writes=["RT"])

                sb_mm(0)
                for t8 in range(8):
                    if t8 + 1 < 8:
                        sb_mm(t8 + 1)
                    sb_post(t8)
                    if t8 % 2 == 1:
                        bg(1)
                if hf == 1:
                    bg(len(bg_list))
                Sc.flush()
        if stop_after == "P4":
            Sc.finish(); return nc

        with ExitStack() as st:
            B0 = ps(st, "B0_5", [128, 512], F32); B1 = ps(st, "B1_5", [128, 512], F32)
            OA0 = ps(st, "OA0_5", [128, 512], F32); OA1 = ps(st, "OA1_5", [128, 512], F32)
            OB0 = ps(st, "OB0_5", [128, 512], F32); OB1 = ps(st, "OB1_5", [128, 512], F32)
            TB = ps(st, "TB_5", [128, 8, 128], BF16); TB2 = ps(st, "TB2_5", [128, 8, 128], BF16)
            TBS = [TB, TB2]
            ew1 = [sb(st, "ew1_%d" % i, [128, 16, FF], BF16) for i in range(2)]
            ew3 = [sb(st, "ew3_%d" % i, [128, 16, FF], BF16) for i in range(2)]
            ew2 = [sb(st, "ew2_%d" % i, [128, 4, D], BF16) for i in range(2)]
            xe = [sb(st, "xe%d" % i, [128, 2, D], BF16) for i in range(2)]
            xeT = sb(st, "xeT", [128, 16, 256], BF16)
            sg = sb(st, "sg", [128, 4, 256], F32)
            hTe = sb(st, "hTe", [128, 4, 256], BF16)
            ye = [sb(st, "ye%d" % i, [128, D], BF16) for i in range(4)]
            Av = [B0[:, :].rearrange("p (a b) -> p a b", b=256), B1[:, :].rearrange("p (a b) -> p a b", b=256)]
            Bv = [OA0[:, :].rearrange("p (a b) -> p a b", b=256), OA1[:, :].rearrange("p (a b) -> p a b", b=256)]
            akey = ["B0", "B1"]; bkeys = ["OA", "OA1"]
            ycount = [0]
            xeT2 = [xeT, sb(st, "xeTb", [128, 16, 256], BF16)]

            def e_ld13(e):
                s = e % 2
                if e in pre_idx:
                    j = pre_idx[e]
                    Sc.dma("pool", lambda h: h.dma_start(out=ew1[s][:], in_=EB1[j].rearrange("p (k n) -> p k n", n=FF)), "ew1_%d" % s, reads=["EB%d_0" % e], writes=["ew1_%d" % s])
                    Sc.dma("pool", lambda h: h.dma_start(out=ew3[s][:], in_=EB3[j].rearrange("p (k n) -> p k n", n=FF)), "ew3_%d" % s, reads=["EB%d_1" % e], writes=["ew3_%d" % s])
                else:
                    Sc.dma("pool", lambda h: h.dma_start(out=ew1[s][:], in_=exp_w1[e].rearrange("(k p) n -> p k n", p=128)), "ew1_%d" % s, writes=["ew1_%d" % s])
                    Sc.dma("pool", lambda h: h.dma_start(out=ew3[s][:], in_=exp_w3[e].rearrange("(k p) n -> p k n", p=128)), "ew3_%d" % s, writes=["ew3_%d" % s])

            def e_ld2(e):
                s = e % 2
                if e in pre_idx:
                    j = pre_idx[e]
                    Sc.dma("pool", lambda h: h.dma_start(out=ew2[s][:], in_=EB2[j].rearrange("p (k n) -> p k n", n=D)), "ew2_%d" % s, reads=["EB%d_2" % e], writes=["ew2_%d" % s])
                else:
                    Sc.dma("pool", lambda h: h.dma_start(out=ew2[s][:], in_=exp_w2[e].rearrange("(k p) n -> p k n", p=128)), "ew2_%d" % s, writes=["ew2_%d" % s])

            def e_ldx(e):
                s = e % 2
                Sc.dma("sp", lambda h: h.dma_start(out=xe[s][:, 0, :], in_=XS[e * CAP:e * CAP + 128, :]), "xe%da" % s, reads=["XS"], writes=["xe%da" % s])
                Sc.dma("sp", lambda h: h.dma_start(out=xe[s][0:R2, 1, :], in_=XS[e * CAP + 128:(e + 1) * CAP, :]), "xe%db" % s, reads=["XS"], writes=["xe%db" % s])

            def e_tr(e):
                s = e % 2
                xt = xeT2[s]; kx = "xeT%d" % s
                tcount = 0
                for a2 in range(2):
                    for c4 in range(4):
                        tb = tcount % 2; tcount += 1
                        for j in range(4):
                            c = 4 * c4 + j
                            nr = 128 if a2 == 0 else R2
                            Sc.add("pe", lambda h, c=c, j=j, tb=tb, a2=a2, nr=nr: h.transpose(out=TBS[tb][:, j, 0:nr], in_=xe[s][0:nr, a2, c * 128:(c + 1) * 128], identity=identb[0:nr, 0:nr]), reads=["xe%d%s" % (s, "ab"[a2]), "identb"], writes=[["TB", "TB2"][tb]])
                        nr = 128 if a2 == 0 else R2
                        if tb == 0:
                            Sc.add("act", lambda h, c4=c4, a2=a2, nr=nr: h.activation(out=xt[:, 4 * c4:4 * c4 + 4, a2 * 128:a2 * 128 + nr], in_=TB[:, 0:4, 0:nr], func=AF.Copy), reads=["TB"], writes=[kx])
                        else:
                            Sc.add("dve", lambda h, c4=c4, a2=a2, nr=nr: h.tensor_copy(out=xt[:, 4 * c4:4 * c4 + 4, a2 * 128:a2 * 128 + nr], in_=TB2[:, 0:4, 0:nr]), reads=["TB2"], writes=[kx])

            def e_s1(e):
                s = e % 2
                xt = xeT2[s]; kx = "xeT%d" % s
                for fb in range(4):
                    for k in range(16):
                        Sc.add("pe", lambda h, fb=fb, k=k: h.matmul(Av[fb // 2][:, fb % 2, 0:CAP], lhsT=ew1[s][:, k, fb * 128:(fb + 1) * 128], rhs=xt[:, k, 0:CAP], start=(k == 0), stop=(k == 15)), reads=["ew1_%d" % s, kx], writes=[akey[fb // 2]])
                for fb in range(4):
                    for k in range(16):
                        Sc.add("pe", lambda h, fb=fb, k=k: h.matmul(Bv[fb // 2][:, fb % 2, 0:CAP], lhsT=ew3[s][:, k, fb * 128:(fb + 1) * 128], rhs=xt[:, k, 0:CAP], start=(k == 0), stop=(k == 15)), reads=["ew3_%d" % s, kx], writes=[bkeys[fb // 2]])
                for hb in range(2):
                    Sc.add("act", lambda h, hb=hb: h.activation(out=sg[:, 2 * hb:2 * hb + 2, 0:CAP], in_=Av[hb][:, :, 0:CAP], func=AF.Silu), reads=[akey[hb]], writes=["sg%d" % hb])
                    Sc.add("dve", lambda h, hb=hb: h.tensor_tensor(out=hTe[:, 2 * hb:2 * hb + 2, 0:CAP], in0=sg[:, 2 * hb:2 * hb + 2, 0:CAP], in1=Bv[hb][:, :, 0:CAP], op=ALU.mult), reads=["sg%d" % hb, bkeys[hb]], writes=["hTe"])

            def e_s2(e):
                s = e % 2
                for a2 in range(2):
                    yb = ye[(e % 2) * 2 + a2]; ykey = "ye%d" % ((e % 2) * 2 + a2)
                    nr = 128 if a2 == 0 else R2
                    for d4 in range(4):
                        bank = [OB0, OB1][ycount[0] % 2]; bkey = ["OB", "OB1"][ycount[0] % 2]; ycount[0] += 1
                        for fb in range(4):
                            Sc.add("pe", lambda h, fb=fb, d4=d4, bank=bank, a2=a2, nr=nr: h.matmul(bank[0:nr, :], lhsT=hTe[:, fb, a2 * 128:a2 * 128 + nr], rhs=ew2[s][:, fb, d4 * 512:(d4 + 1) * 512], start=(fb == 0), stop=(fb == 3)), reads=["hTe", "ew2_%d" % s], writes=[bkey])
                        if d4 % 2 == 0:
                            Sc.add("act", lambda h, d4=d4, bank=bank, yb=yb, nr=nr: h.activation(out=yb[0:nr, d4 * 512:(d4 + 1) * 512], in_=bank[0:nr, :], func=AF.Copy), reads=[bkey], writes=[ykey])
                        else:
                            Sc.add("dve", lambda h, d4=d4, bank=bank, yb=yb, nr=nr: h.tensor_copy(out=yb[0:nr, d4 * 512:(d4 + 1) * 512], in_=bank[0:nr, :]), reads=[bkey], writes=[ykey])
                    Sc.dma("sp", lambda h, a2=a2, yb=yb, nr=nr: h.dma_start(out=YS[e * CAP + a2 * 128:e * CAP + a2 * 128 + nr, :], in_=yb[0:nr, :]), "w" + ykey, reads=[ykey], writes=["YS"])

            for e0 in range(min(2, nexp)):
                e_ld13(e0); e_ld2(e0); e_ldx(e0)
            e_tr(0)
            for e in range(nexp):
                e_s1(e)
                if e + 2 < nexp:
                    e_ld13(e + 2)
                if e + 1 < nexp:
                    e_tr(e + 1)
                if e + 2 < nexp:
                    e_ldx(e + 2)
                e_s2(e)
                if e + 2 < nexp:
                    e_ld2(e + 2)
            Sc.flush()
        if stop_after == "P5":
            Sc.finish(); return nc

        with ExitStack() as st:
            fin = sb(st, "fin", [128, D], F32)
            Sc.dma("sp", lambda h: h.dma_start(out=fin[:], in_=final_norm[0:1, :].broadcast_to([128, D])), "fin", writes=["fin"])
            x1b = [sb(st, "x1b%d" % i, [128, D], F32) for i in range(4)]
            y0 = [sb(st, "y0_%d" % i, [128, D], BF16) for i in range(4)]
            y1 = [sb(st, "y1_%d" % i, [128, D], BF16) for i in range(4)]
            junk = sb(st, "junk6", [128, D], BF16)
            ss = sb(st, "ss6", [128, 1], F32)
            for tt in range(NT):
                s = tt % 4
                Sc.dma("sp", lambda h, tt=tt, s=s: h.dma_start(out=x1b[s][:], in_=X1[tt * 128:(tt + 1) * 128, :]), "x1b%d" % s, reads=["X1"], writes=["x1b%d" % s])
                if tt < 4:
                    Sc.add("pool", lambda h, s=s: h.memset(y0[s][:], 0.0), writes=["y0_%d" % s])
                    Sc.add("pool", lambda h, s=s: h.memset(y1[s][:], 0.0), writes=["y1_%d" % s])
                for kk, yy in enumerate((y0, y1)):
                    Sc.dma("pool", lambda h, tt=tt, kk=kk, s=s, yy=yy: h.indirect_dma_start(out=yy[s][:, :], out_offset=None, in_=YS[:, :], in_offset=bass.IndirectOffsetOnAxis(ap=dest_i[:, tt, kk:kk + 1], axis=0),
                                                                                       bounds_check=Sc.bound_reg(h, NSLOT - 1), oob_is_err=False), "yg%d_%d" % (kk, s), reads=["YS", "dest_i"], writes=["y%d_%d" % (kk, s)])
                Sc.add("dve", lambda h, tt=tt, s=s: h.scalar_tensor_tensor(out=x1b[s][:], in0=y0[s][:], scalar=gate2[:, tt, 0:1], in1=x1b[s][:], op0=ALU.mult, op1=ALU.add), reads=["y0_%d" % s, "gate2", "x1b%d" % s], writes=["x1b%d" % s])
                Sc.add("dve", lambda h, tt=tt, s=s: h.scalar_tensor_tensor(out=x1b[s][:], in0=y1[s][:], scalar=gate2[:, tt, 1:2], in1=x1b[s][:], op0=ALU.mult, op1=ALU.add), reads=["y1_%d" % s, "gate2", "x1b%d" % s], writes=["x1b%d" % s])
                Sc.add("act", lambda h, s=s: h.activation(out=junk[:], in_=x1b[s][:], func=AF.Square, accum_out=ss[:]), reads=["x1b%d" % s], writes=["junk6", "ss6"])
                Sc.add("act", lambda h: h.activation(out=ss[:], in_=ss[:], func=AF.Sqrt, scale=1.0 / D, bias=EPS), reads=["ss6"], writes=["ss6"])
                Sc.add("dve", lambda h: h.reciprocal(out=ss[:], in_=ss[:]), reads=["ss6"], writes=["ss6"])
                Sc.add("dve", lambda h, s=s: h.scalar_tensor_tensor(out=x1b[s][:], in0=x1b[s][:], scalar=ss[:, 0:1], in1=fin[:], op0=ALU.mult, op1=ALU.mult), reads=["x1b%d" % s, "ss6", "fin"], writes=["x1b%d" % s])
                Sc.dma("sp", lambda h, tt=tt, s=s: h.dma_start(out=out[tt * 128:(tt + 1) * 128, :], in_=x1b[s][:]), "outw%d" % s, reads=["x1b%d" % s], writes=["out"])
            Sc.flush()
        Sc.finish()
    return nc


def make_in_maps(inputs, cores):
    c = _consts()
    g = lambda k: np.ascontiguousarray(np.asarray(inputs[k], dtype=np.float32))
    shared = {
        "attn_norm": g("attn_norm")[0].reshape(16, 128),
        "w_in": g("w_in")[0],
        "conv_dw": g("conv_dw")[0],
        "conv_dw_b": g("conv_dw_b")[0].reshape(8, 128),
        "conv_ln_g": g("conv_ln_g")[0].reshape(8, 128),
        "conv_ln_b": g("conv_ln_b")[0].reshape(8, 128),
        "cmp_pos_k": g("cmp_pos_k")[0], "cmp_pos_v": g("cmp_pos_v")[0],
        "cmp_k_w1": g("cmp_k_w1")[0], "cmp_v_w1": g("cmp_v_w1")[0],
        "cmp_k_w2": g("cmp_k_w2")[0], "cmp_v_w2": g("cmp_v_w2")[0],
        "w_proj_conv": g("w_proj_conv")[0], "w_proj_nsa": g("w_proj_nsa")[0],
        "w_merge": g("w_merge")[0], "b_merge": g("b_merge")[0].reshape(32, 128), "w_out": g("w_out")[0],
        "ffn_norm": g("ffn_norm")[0].reshape(1, D),
        "w_gr": np.ascontiguousarray(np.concatenate([g("w_grp")[0], g("w_exp")[0]], axis=1)),
        "b_gr": np.concatenate([g("b_grp")[0], g("b_exp")[0]]).reshape(1, 72),
        "exp_w1": g("exp_w1")[0], "exp_w3": g("exp_w3")[0], "exp_w2": g("exp_w2")[0],
        "final_norm": g("final_norm").reshape(1, D),
    }
    shared.update(c)
    xs = g("x")
    return [dict(shared, x=xs[b]) for b in cores]


def kernel(**inputs):
    nc = build()
    cores = list(range(8))
    in_maps = make_in_maps(inputs, cores)
    res = run_bass_kernel_spmd(nc, in_maps, core_ids=cores)
    return np.stack([res.results[i]["out"] for i in range(8)], axis=0).astype(np.float32)
```
